# Optimizing a Trainium2 kernel written in Bass

```python
import math
import jax
import jax.numpy as jnp
from jax import lax
import numpy as np


D_MODEL = 1024
BATCH = 16
SEQ = 2048
DEPTH = 1

GRID_W = 64
CTX_LEN = 256
DN_HEADS = 4
DN_DK = 128
DN_DV = 128
CONV_W = 5
GLA_HEADS = 4
GLA_DK = 64
GLA_DV = 128
GLA_LR = 16
GLA_TAU = 16.0
CHUNK = 64
PEER_HEADS = 8
PEER_NKEYS = 128
PEER_EXPERTS = PEER_NKEYS * PEER_NKEYS
PEER_DQ = 256
PEER_TOPK = 16
PEER_BLOCK = 128
EPS = 1e-6

D_MIX = DN_HEADS * DN_DV + GLA_HEADS * GLA_DV
DN_QKV = 2 * DN_HEADS * DN_DK + DN_HEADS * DN_DV
DN_COLS = DN_QKV + DN_HEADS * DN_DV + 4 * DN_HEADS
GLA_COLS = 2 * GLA_HEADS * GLA_DK + 2 * GLA_HEADS * GLA_DV + 2 * GLA_LR
IN_COLS = DN_COLS + GLA_COLS

kernel_name = "hybrid_gdn_gla_peer_prefix_dit"


def rmsnorm(x, g):
    xf = x.astype(jnp.float32)
    y = xf * lax.rsqrt(jnp.mean(xf * xf, axis=-1, keepdims=True) + EPS)
    return (y * g.astype(jnp.float32)).astype(x.dtype)


def modulate(h, shift, scale):
    return h * (1 + scale) + shift


def l2norm(t):
    tf = t.astype(jnp.float32)
    return tf * lax.rsqrt(jnp.sum(tf * tf, axis=-1, keepdims=True) + EPS)


def head_norm_gate(o, g, gate):
    y = o * lax.rsqrt(jnp.mean(o * o, axis=-1, keepdims=True) + EPS) * g.astype(jnp.float32)
    y = y * jax.nn.silu(gate.astype(jnp.float32))
    return y.reshape(y.shape[:2] + (-1,)).astype(gate.dtype)


def short_conv(t, w):
    pad = CONV_W // 2
    return lax.conv_general_dilated(
        t, w[:, None, :].astype(t.dtype), window_strides=(1,), padding=((pad, pad),),
        dimension_numbers=('NWC', 'WIO', 'NWC'), feature_group_count=t.shape[-1])


def _chunk(t, n):
    b, _, h = t.shape[:3]
    t = t.reshape((b, n, CHUNK, h) + t.shape[3:])
    return t.transpose((1, 0, 3, 2) + tuple(range(4, t.ndim)))


def _unchunk(t):
    n, b, h, c, d = t.shape
    return t.transpose(1, 0, 3, 2, 4).reshape(b, n * c, h, d)


def _tril(strict):
    i = jnp.arange(CHUNK)
    return (i[:, None] > i[None, :]) if strict else (i[:, None] >= i[None, :])


def gated_delta_scan(q, k, v, g, beta, s0, with_output):
    f32 = jnp.float32
    n = q.shape[1] // CHUNK
    dv = v.shape[-1]
    q, k, v, g, beta = (_chunk(t.astype(f32), n) for t in (q, k, v, g, beta))
    gc = jnp.cumsum(g, axis=-1)
    decay = jnp.exp(jnp.where(_tril(False), gc[..., :, None] - gc[..., None, :], -jnp.inf))
    kb = k * beta[..., None]
    lower = jnp.where(_tril(True), jnp.einsum('nbhtd,nbhsd->nbhts', kb, k) * decay, 0.0)
    rhs = jnp.concatenate([v * beta[..., None], kb * jnp.exp(gc)[..., None]], axis=-1)
    sol = lax.linalg.triangular_solve(lower, rhs, left_side=True, lower=True, unit_diagonal=True)
    u, w = sol[..., :dv], sol[..., dv:]
    k_dec = k * jnp.exp(gc[..., -1:] - gc)[..., None]
    g_last = jnp.exp(gc[..., -1])

    def step(S, xs):
        u_c, w_c, kd_c, gl_c = xs[:4]
        v_new = u_c - jnp.einsum('bhck,bhkv->bhcv', w_c, S)
        S_next = S * gl_c[..., None, None] + jnp.einsum('bhck,bhcv->bhkv', kd_c, v_new)
        if not with_output:
            return S_next, None
        a_c, qd_c = xs[4:]
        o = jnp.einsum('bhck,bhkv->bhcv', qd_c, S) + jnp.einsum('bhts,bhsv->bhtv', a_c, v_new)
        return S_next, o

    if with_output:
        a_qk = jnp.einsum('nbhtd,nbhsd->nbhts', q, k) * decay
        xs = (u, w, k_dec, g_last, a_qk, q * jnp.exp(gc)[..., None])
    else:
        xs = (u, w, k_dec, g_last)
    S, o = lax.scan(step, s0.astype(f32), xs)
    return (_unchunk(o) if with_output else None), S


def gla_scan(q, k, v, log_a, s0, with_output):
    f32 = jnp.float32
    n = q.shape[1] // CHUNK
    q, k, v, la = (_chunk(t.astype(f32), n) for t in (q, k, v, log_a))
    b = jnp.cumsum(la, axis=-2)
    b_last = b[..., -1, :]
    k_dec = k * jnp.exp(b_last[..., None, :] - b)
    causal = _tril(False)

    def step(S, xs):
        v_c, kd_c, bl_c = xs[:3]
        S_next = S * jnp.exp(bl_c)[..., :, None] + jnp.einsum('bhck,bhcv->bhkv', kd_c, v_c)
        if not with_output:
            return S_next, None
        q_c, k_c, b_c = xs[3:]
        diff = jnp.where(causal[:, :, None], b_c[..., :, None, :] - b_c[..., None, :, :], -jnp.inf)
        att = jnp.einsum('bhtk,bhsk,bhtsk->bhts', q_c, k_c, jnp.exp(diff))
        o = (jnp.einsum('bhck,bhkv->bhcv', q_c * jnp.exp(b_c), S)
             + jnp.einsum('bhts,bhsv->bhtv', att, v_c))
        return S_next, o

    xs = (v, k_dec, b_last, q, k, b) if with_output else (v, k_dec, b_last)
    S, o = lax.scan(step, s0.astype(f32), xs)
    return (_unchunk(o) if with_output else None), S


def bidirectional(scan_fn, ctx_f, ctx_b, lat_f, lat_b, s0, ctx_out):
    rev = lambda args: tuple(jnp.flip(a, axis=1) for a in args)
    oc_f, sc_f = scan_fn(*ctx_f, s0, ctx_out)
    oc_b, sc_b = scan_fn(*rev(ctx_b), s0, ctx_out)
    ol_f, _ = scan_fn(*lat_f, sc_f, True)
    ol_b, _ = scan_fn(*rev(lat_b), sc_b, True)
    o_lat = ol_f + jnp.flip(ol_b, axis=1)
    o_ctx = (oc_f + jnp.flip(oc_b, axis=1)) if ctx_out else None
    return o_ctx, o_lat


def dn_features(p, conv_w, a_log, dt_bias):
    b, l, _ = p.shape
    qk, vd, h = DN_HEADS * DN_DK, DN_HEADS * DN_DV, DN_HEADS
    qkv = jax.nn.silu(short_conv(p[..., :DN_QKV], conv_w))
    q = l2norm(qkv[..., :qk].reshape(b, l, h, DN_DK)) * DN_DK ** -0.5
    k = l2norm(qkv[..., qk:2 * qk].reshape(b, l, h, DN_DK))
    v = qkv[..., 2 * qk:].reshape(b, l, h, DN_DV)
    z = p[..., DN_QKV:DN_QKV + vd].reshape(b, l, h, DN_DV)
    o = DN_QKV + vd
    beta = jax.nn.sigmoid(p[..., o:o + 2 * h].astype(jnp.float32)).reshape(b, l, 2, h)
    a = p[..., o + 2 * h:o + 4 * h].astype(jnp.float32).reshape(b, l, 2, h)
    g = -jnp.exp(a_log.astype(jnp.float32)) * jax.nn.softplus(a + dt_bias.astype(jnp.float32))
    return q, k, v, z, g, beta


def gla_features(p, wa2, ba):
    b, l, _ = p.shape
    qk, vd = GLA_HEADS * GLA_DK, GLA_HEADS * GLA_DV
    q = p[..., :qk].reshape(b, l, GLA_HEADS, GLA_DK) * GLA_DK ** -0.5
    k = p[..., qk:2 * qk].reshape(b, l, GLA_HEADS, GLA_DK)
    v = p[..., 2 * qk:2 * qk + vd].reshape(b, l, GLA_HEADS, GLA_DV)
    r = p[..., 2 * qk + vd:2 * qk + 2 * vd].reshape(b, l, GLA_HEADS, GLA_DV)
    lr = p[..., 2 * qk + 2 * vd:].reshape(b, l, 2, GLA_LR)
    pre = jnp.einsum('bldr,drk->bldk', lr, wa2) + ba
    log_a = jax.nn.log_sigmoid(pre.astype(jnp.float32)) / GLA_TAU
    return q, k, v, r, log_a.reshape(b, l, 2, GLA_HEADS, GLA_DK)


def to_col_major(t, rows):
    b = t.shape[0]
    return t.reshape((b, rows, GRID_W) + t.shape[2:]).swapaxes(1, 2).reshape(t.shape)


def from_col_major(t, rows):
    b = t.shape[0]
    return t.reshape((b, GRID_W, rows) + t.shape[2:]).swapaxes(1, 2).reshape(t.shape)


def token_mixer(h_ctx, h_lat, rows, w_in, conv_w, dn_a_log, dn_dt_bias, dn_norm_g,
                gla_wa2, gla_ba, gla_norm_g, w_out, ctx_out):
    b = h_lat.shape[0]
    p_c = h_ctx @ w_in
    p_l = h_lat @ w_in
    qc, kc, vc, zc, gc, bc = dn_features(p_c[..., :DN_COLS], conv_w, dn_a_log, dn_dt_bias)
    ql, kl, vl, zl, gl, bl = dn_features(p_l[..., :DN_COLS], conv_w, dn_a_log, dn_dt_bias)
    s0_dn = jnp.zeros((b, DN_HEADS, DN_DK, DN_DV), jnp.float32)
    dn_c, dn_l = bidirectional(
        gated_delta_scan,
        (qc, kc, vc, gc[:, :, 0], bc[:, :, 0]), (qc, kc, vc, gc[:, :, 1], bc[:, :, 1]),
        (ql, kl, vl, gl[:, :, 0], bl[:, :, 0]), (ql, kl, vl, gl[:, :, 1], bl[:, :, 1]),
        s0_dn, ctx_out)
    gqc, gkc, gvc, grc, lac = gla_features(p_c[..., DN_COLS:], gla_wa2, gla_ba)
    gql, gkl, gvl, grl, lal = gla_features(p_l[..., DN_COLS:], gla_wa2, gla_ba)
    col = lambda t: to_col_major(t, rows)
    gql, gkl, gvl, lal = col(gql), col(gkl), col(gvl), col(lal)
    s0_gla = jnp.zeros((b, GLA_HEADS, GLA_DK, GLA_DV), jnp.float32)
    gla_c, gla_l = bidirectional(
        gla_scan,
        (gqc, gkc, gvc, lac[:, :, 0]), (gqc, gkc, gvc, lac[:, :, 1]),
        (gql, gkl, gvl, lal[:, :, 0]), (gql, gkl, gvl, lal[:, :, 1]),
        s0_gla, ctx_out)
    gla_l = from_col_major(gla_l, rows)
    y_lat = jnp.concatenate([head_norm_gate(dn_l, dn_norm_g, zl),
                             head_norm_gate(gla_l, gla_norm_g, grl)], axis=-1) @ w_out
    y_ctx = None
    if ctx_out:
        y_ctx = jnp.concatenate([head_norm_gate(dn_c, dn_norm_g, zc),
                                 head_norm_gate(gla_c, gla_norm_g, grc)], axis=-1) @ w_out
    return y_ctx, y_lat


def peer(h, wq, keys, u_tab, v_tab):
    b, l, d = h.shape

    def block(hb):
        t = hb.shape[0]
        q = (hb @ wq).reshape(t, PEER_HEADS, 2, PEER_DQ // 2)
        s = jnp.einsum('thpd,hpkd->thpk', q, keys)
        top_s, top_i = lax.top_k(s, PEER_TOPK)
        cand_s = (top_s[:, :, 0, :, None] + top_s[:, :, 1, None, :]).reshape(t, PEER_HEADS, -1)
        cand_i = (top_i[:, :, 0, :, None] * PEER_NKEYS + top_i[:, :, 1, None, :]).reshape(t, PEER_HEADS, -1)
        best_s, pos = lax.top_k(cand_s, PEER_TOPK)
        idx = jnp.take_along_axis(cand_i, pos, axis=-1)
        gate = jax.nn.softmax(best_s.astype(jnp.float32), axis=-1)
        act = jax.nn.gelu(jnp.einsum('thkd,td->thk', u_tab[idx], hb), approximate=False)
        return jnp.einsum('thk,thkd->td', gate * act, v_tab[idx]).astype(hb.dtype)

    y = lax.map(block, h.reshape(-1, PEER_BLOCK, d))
    return y.reshape(b, l, d)


def setup_inputs(seed: int = 0) -> dict:
    key = jax.random.key(seed)
    ks = jax.random.split(key, 22)
    f32 = jnp.float32
    nrm = lambda k, shape, scale: jax.random.normal(k, shape, f32) * scale
    x = nrm(ks[0], (BATCH, SEQ, D_MODEL), 1.0)
    c = nrm(ks[1], (BATCH, D_MODEL), 1.0)
    ctx = nrm(ks[2], (BATCH, CTX_LEN, D_MODEL), 1.0)
    c_ctx = nrm(ks[3], (D_MODEL,), 1.0)
    w_ada = nrm(ks[4], (DEPTH, D_MODEL, 6 * D_MODEL), 0.5 * D_MODEL ** -0.5)
    b_ada = nrm(ks[5], (DEPTH, 6 * D_MODEL), 0.02)
    norm1_g = 1.0 + nrm(ks[6], (DEPTH, D_MODEL), 0.02)
    norm2_g = 1.0 + nrm(ks[7], (DEPTH, D_MODEL), 0.02)
    w_in = nrm(ks[8], (DEPTH, D_MODEL, IN_COLS), D_MODEL ** -0.5)
    conv_w = nrm(ks[9], (DEPTH, CONV_W, DN_QKV), CONV_W ** -0.5)
    dn_a_log = jnp.log(jax.random.uniform(ks[10], (DEPTH, 2, DN_HEADS), f32, 1.0, 16.0))
    dt = jnp.exp(jax.random.uniform(ks[11], (DEPTH, 2, DN_HEADS), f32, math.log(1e-3), math.log(1e-1)))
    dn_dt_bias = dt + jnp.log(-jnp.expm1(-dt))
    dn_norm_g = 1.0 + nrm(ks[12], (DEPTH, DN_DV), 0.02)
    gla_wa2 = nrm(ks[13], (DEPTH, 2, GLA_LR, GLA_HEADS * GLA_DK), GLA_LR ** -0.5)
    gla_ba = nrm(ks[14], (DEPTH, 2, GLA_HEADS * GLA_DK), 0.1)
    gla_norm_g = 1.0 + nrm(ks[15], (DEPTH, GLA_DV), 0.02)
    w_out = nrm(ks[16], (DEPTH, D_MIX, D_MODEL), D_MIX ** -0.5)
    peer_wq = nrm(ks[17], (DEPTH, D_MODEL, PEER_HEADS * PEER_DQ), D_MODEL ** -0.5)
    peer_keys = nrm(ks[18], (DEPTH, PEER_HEADS, 2, PEER_NKEYS, PEER_DQ // 2), (PEER_DQ // 2) ** -0.5)
    peer_u = nrm(ks[19], (DEPTH, PEER_EXPERTS, D_MODEL), D_MODEL ** -0.5)
    peer_v = nrm(ks[20], (DEPTH, PEER_EXPERTS, D_MODEL), 0.5)
    final_g = 1.0 + nrm(ks[21], (D_MODEL,), 0.02)
    return {"x": x, "c": c, "ctx": ctx, "c_ctx": c_ctx, "w_ada": w_ada, "b_ada": b_ada,
            "norm1_g": norm1_g, "norm2_g": norm2_g, "w_in": w_in, "conv_w": conv_w,
            "dn_a_log": dn_a_log, "dn_dt_bias": dn_dt_bias, "dn_norm_g": dn_norm_g,
            "gla_wa2": gla_wa2, "gla_ba": gla_ba, "gla_norm_g": gla_norm_g, "w_out": w_out,
            "peer_wq": peer_wq, "peer_keys": peer_keys, "peer_u": peer_u, "peer_v": peer_v,
            "final_g": final_g}


def reference(x, c, ctx, c_ctx, w_ada, b_ada, norm1_g, norm2_g, w_in, conv_w, dn_a_log,
              dn_dt_bias, dn_norm_g, gla_wa2, gla_ba, gla_norm_g, w_out, peer_wq, peer_keys,
              peer_u, peer_v, final_g):
    rows = x.shape[1] // GRID_W
    for l in range(DEPTH):
        last = l == DEPTH - 1
        mod_l = (jax.nn.silu(c) @ w_ada[l] + b_ada[l])[:, None, :]
        mod_c = jax.nn.silu(c_ctx) @ w_ada[l] + b_ada[l]
        sh1, sc1, g1, sh2, sc2, g2 = jnp.split(mod_l, 6, axis=-1)
        csh1, csc1, cg1, csh2, csc2, cg2 = jnp.split(mod_c, 6, axis=-1)
        h_lat = modulate(rmsnorm(x, norm1_g[l]), sh1, sc1)
        h_ctx = modulate(rmsnorm(ctx, norm1_g[l]), csh1, csc1)
        y_ctx, y_lat = token_mixer(h_ctx, h_lat, rows, w_in[l], conv_w[l], dn_a_log[l],
                                   dn_dt_bias[l], dn_norm_g[l], gla_wa2[l], gla_ba[l],
                                   gla_norm_g[l], w_out[l], not last)
        x = x + g1 * y_lat
        x = x + g2 * peer(modulate(rmsnorm(x, norm2_g[l]), sh2, sc2),
                          peer_wq[l], peer_keys[l], peer_u[l], peer_v[l])
        if not last:
            ctx = ctx + cg1 * y_ctx
            ctx = ctx + cg2 * peer(modulate(rmsnorm(ctx, norm2_g[l]), csh2, csc2),
                                   peer_wq[l], peer_keys[l], peer_u[l], peer_v[l])
    return rmsnorm(x, final_g)
```

```python
import contextlib
import numpy as np
import concourse.bass as bass
import concourse.mybir as mybir
from concourse.bass_utils import run_bass_kernel_spmd

F32 = mybir.dt.float32
BF16 = mybir.dt.bfloat16
ALU = mybir.AluOpType
AF = mybir.ActivationFunctionType
AX = mybir.AxisListType

NEG = -30000.0
EPS = 1e-6


class Buf:
    __slots__ = ("name", "t", "wev", "revs", "dsem", "dcnt", "pre")

    def __init__(self, name, t=None):
        self.name = name
        self.t = t
        self.wev = []
        self.revs = []
        self.dsem = None
        self.dcnt = 0
        self.pre = []

    def __getitem__(self, k):
        return self.t[k]


class Eng:
    def __init__(self, name, h, sem):
        self.name = name
        self.h = h
        self.sem = sem
        self.cnt = 0
        self.known = {}


class FW:
    def __init__(self, nc, stack):
        self.nc = nc
        self.top = stack
        self.stack = stack
        self.engs = {}
        self.dsems = []
        for name, h in (("pe", nc.tensor), ("act", nc.scalar), ("dve", nc.vector),
                        ("pool", nc.gpsimd), ("sp", nc.sync)):
            sem = stack.enter_context(nc.semaphore("s_" + name))
            self.engs[name] = Eng(name, h, sem)
        self.ninst = 0

    @contextlib.contextmanager
    def scope(self):
        old = self.stack
        with contextlib.ExitStack() as st:
            self.stack = st
            try:
                yield
            finally:
                self.barrier()
                self.stack = old

    def sb(self, name, shape, dt=F32):
        self.nsb = getattr(self, "nsb", 0) + 1
        name = "sb%d_%s" % (self.nsb, name)
        t = self.stack.enter_context(self.nc.sbuf_tensor(name, list(shape), dt))
        return Buf(name, t)

    def view(self, name, t=None):
        return Buf(name, t)

    def _waits(self, e, reads, writes, skip=None):
        need = {}
        for b in reads:
            for (s, v) in b.wev:
                if need.get(s, 0) < v:
                    need[s] = v
        for b in writes:
            for (s, v) in b.wev:
                if need.get(s, 0) < v:
                    need[s] = v
            for (s, v) in b.revs:
                if need.get(s, 0) < v:
                    need[s] = v
        for s, v in need.items():
            if s is skip or (e.name == "pe" and s is e.sem):
                continue
            if e.known.get(s, 0) < v:
                e.h.wait_ge(s, v)
                e.known[s] = v

    def op(self, eng, fn, reads=(), writes=()):
        if getattr(self, "dead", False):
            return None
        e = self.engs[eng]
        self._waits(e, reads, writes)
        ins = fn(e.h)
        e.cnt += 1
        ins.then_inc(e.sem, 1)
        self.ninst += 1
        ev = (e.sem, e.cnt)
        for b in reads:
            if len(b.revs) > 24:
                b.revs = b.revs[-12:] + self._maxev(b.revs[:-12])
            b.revs.append(ev)
        for b in writes:
            b.wev = [ev]
            b.revs = []
        return ins

    @staticmethod
    def _maxev(evs):
        d = {}
        for (s, v) in evs:
            if d.get(s, (None, 0))[1] < v:
                d[s] = (s, v)
        return list(d.values())

    def dma(self, q, out_ap, in_ap, dst, src, **kw):
        if getattr(self, "dead", False):
            return None
        e = self.engs[q]
        reads = [src] if src is not None else []
        if dst.dsem is None:
            dst.dsem = self.top.enter_context(self.nc.semaphore("d%d_%s" % (len(self.dsems), dst.name)))
            self.dsems.append(dst)
        evs = [ev for ev in dst.wev if ev[0] is not dst.dsem] + list(dst.revs)
        if evs:
            dst.pre = evs
        else:
            evs = dst.pre
        for (sm, v) in evs:
            if e.known.get(sm, 0) < v:
                e.h.wait_ge(sm, v)
                e.known[sm] = v
        self._waits(e, reads, [], skip=dst.dsem)
        ins = e.h.dma_start(out=out_ap, in_=in_ap, **kw)
        self.ninst += 1
        dst.dcnt += 16
        ins.then_inc(dst.dsem, 16)
        ev = (dst.dsem, dst.dcnt)
        if src is not None:
            src.revs.append(ev)
        dst.wev = [ev]
        dst.revs = []
        return ins

    def barrier(self):
        for e in self.engs.values():
            for f in self.engs.values():
                if f is e or f.cnt == 0:
                    continue
                if e.known.get(f.sem, 0) < f.cnt:
                    e.h.wait_ge(f.sem, f.cnt)
                    e.known[f.sem] = f.cnt
            for b in self.dsems:
                if b.dcnt and e.known.get(b.dsem, 0) < b.dcnt:
                    e.h.wait_ge(b.dsem, b.dcnt)
                    e.known[b.dsem] = b.dcnt

    def finish(self, bufs, eng="sp"):
        self._waits(self.engs[eng], bufs, [])


def make_consts():
    p = np.arange(128)[:, None]
    f = np.arange(128)[None, :]
    c = np.zeros((128, 12, 128), np.float32)
    c[:, 0] = (p == f)
    c[:, 1] = (p <= f)
    c[:, 2] = (p >= f)
    c[:, 3] = np.where(p > f, 0.0, NEG)
    c[:, 4] = np.where(p < f, 0.0, NEG)
    c[:, 5] = np.where(p <= f, 0.0, NEG)
    c[:, 6] = np.where(p >= f, 0.0, NEG)
    c[:, 7] = (p <= f)
    c[:, 8] = (p >= f)
    c[:, 9] = 1.0
    c[:, 10] = -(p <= f).astype(np.float32) / 16.0
    c[:, 11] = -(p >= f).astype(np.float32) / 16.0
    return c


def dn_scan(fw, nc, PS, PB, pbf, pbb, cst, cstb, Pq, CO, BET, NBET, GG, oacc):
    V = lambda fn, r, w: fw.op("dve", fn, r, w)
    A = lambda fn, r, w: fw.op("act", fn, r, w)
    T = lambda fn, r, w: fw.op("pe", fn, r, w)
    ident_b = cstb[:, 0, :]
    ones_f = cst[:, 9, :]
    r4 = lambda ap: ap.rearrange("p (h t) -> p h t", t=128)
    S = fw.sb("dnS", [128, 4, 128])
    Sbf = fw.sb("dnSbf", [128, 4, 128], BF16)
    kv = fw.sb("dkv", [128, 8, 128], BF16)
    Gb = fw.sb("dGb", [128, 4, 128])
    nGb = fw.sb("dnGb", [128, 4, 128])
    sml = fw.sb("dsml", [128, 4, 4])
    gct = fw.sb("dgct", [128, 8])
    E1 = fw.sb("dE1", [128, 4, 128])
    E2 = fw.sb("dE2", [128, 4, 128])
    E3 = fw.sb("dE3", [128, 4, 128])
    Y = fw.sb("dY", [128, 4, 128])
    Z = fw.sb("dZ", [128, 4, 128])
    Rf = fw.sb("dRf", [128, 4, 128])
    R = fw.sb("dR", [128, 4, 128], BF16)
    At = fw.sb("dAt", [128, 4, 128], BF16)
    qg = fw.sb("dqg", [128, 4, 128], BF16)
    kbg = fw.sb("dkbg", [128, 4, 128], BF16)
    kd = fw.sb("dkd", [128, 4, 128], BF16)
    vb = fw.sb("dvb", [128, 4, 128], BF16)
    Usb = fw.sb("dU", [128, 4, 128])
    WT = fw.sb("dWT", [128, 4, 128], BF16)
    vnew = fw.sb("dvn", [128, 4, 128], BF16)
    bc = lambda ap: ap.unsqueeze(2).to_broadcast([128, 4, 128])
    for d in (0, 1):
        Cum = cst[:, 1 + d, :]
        NM1 = cst[:, 3 + d, :].unsqueeze(1).to_broadcast([128, 4, 128])
        NM2 = cst[:, 5 + d, :].unsqueeze(1).to_broadcast([128, 4, 128])
        V(lambda e: e.memset(S[:], 0.0), [], [S])
        V(lambda e: e.memset(Sbf[:], 0.0), [], [Sbf])
        order = [(0, i) for i in ((0, 1) if d == 0 else (1, 0))] + \
                [(1, i) for i in (range(16) if d == 0 else range(15, -1, -1))]
        for (seg, i) in order:
            fw.ntile = getattr(fw, "ntile", 0) + 1
            if fw.ntile > getattr(fw, "dn_limit", 10 ** 9):
                fw.dead = True
            gi = i if seg == 0 else 2 + i
            c0 = CO[seg] + i * 128
            g4 = GG[:, gi, d * 4:d * 4 + 4]
            b4 = BET[:, gi, d * 4:d * 4 + 4]
            nb4 = NBET[:, gi, d * 4:d * 4 + 4]
            T(lambda e: e.matmul(out=pbf(6)[:, 0:4], lhsT=Cum, rhs=g4, start=True, stop=True), [cst, GG], [PB[6]])
            T(lambda e: e.matmul(out=pbf(6)[:, 4:8], lhsT=ones_f, rhs=g4, start=True, stop=True), [cst, GG], [PB[6]])
            A(lambda e: e.copy(out=gct[:], in_=pbf(6)[:, 0:8]), [PB[6]], [gct])
            A(lambda e: e.activation(out=sml[:, 0, :], in_=gct[:, 0:4], func=AF.Exp), [gct], [sml])
            V(lambda e: e.tensor_tensor(out=sml[:, 1, :], in0=gct[:, 4:8], in1=gct[:, 0:4], op=ALU.subtract), [gct], [sml])
            A(lambda e: e.activation(out=sml[:, 1, :], in_=sml[:, 1, :], func=AF.Exp), [sml], [sml])
            A(lambda e: e.activation(out=sml[:, 2, :], in_=gct[:, 4:8], func=AF.Exp), [gct], [sml])
            V(lambda e: e.tensor_tensor(out=sml[:, 3, :], in0=sml[:, 0, :], in1=b4, op=ALU.mult), [sml, BET], [sml])
            V(lambda e: e.tensor_copy(out=Gb[:], in_=bc(g4)), [GG], [Gb])
            A(lambda e: e.mul(out=nGb[:], in_=Gb[:], mul=-1.0), [Gb], [nGb])
            for h in range(4):
                T(lambda e, h=h: e.transpose(out=pbb(0)[:, h * 128:(h + 1) * 128], in_=Pq[:, 4 + h, c0:c0 + 128], identity=ident_b),
                  [Pq, cstb], [PB[0]])
                T(lambda e, h=h: e.transpose(out=pbb(0)[:, 512 + h * 128:512 + (h + 1) * 128], in_=Pq[:, 8 + h, c0:c0 + 128],
                                             identity=ident_b), [Pq, cstb], [PB[0]])
            A(lambda e: e.copy(out=kv[:].rearrange("p a t -> p (a t)"), in_=pbb(0)), [PB[0]], [kv])
            for h in range(4):
                kT = Pq[:, 4 + h, c0:c0 + 128]
                qT = Pq[:, h, c0:c0 + 128]
                hs = slice(h * 128, (h + 1) * 128)
                T(lambda e, kT=kT, hs=hs: e.matmul(out=pbf(1)[:, hs], lhsT=kT, rhs=kT, start=True, stop=True), [Pq], [PB[1]])
                T(lambda e, kT=kT, qT=qT, hs=hs: e.matmul(out=pbf(2)[:, hs], lhsT=kT, rhs=qT, start=True, stop=True), [Pq], [PB[2]])
                T(lambda e, h=h, hs=hs: e.matmul(out=pbf(3)[:, hs], lhsT=Cum, rhs=Gb[:, h, :], start=True, stop=False), [cst, Gb], [PB[3]])
                T(lambda e, h=h, hs=hs: e.matmul(out=pbf(3)[:, hs], lhsT=nGb[:, h, :], rhs=Cum, start=False, stop=True), [cst, nGb], [PB[3]])
                T(lambda e, h=h, hs=hs: e.matmul(out=pbf(4)[:, hs], lhsT=Gb[:, h, :], rhs=Cum, start=True, stop=False), [cst, Gb], [PB[4]])
                T(lambda e, h=h, hs=hs: e.matmul(out=pbf(4)[:, hs], lhsT=Cum, rhs=nGb[:, h, :], start=False, stop=True), [cst, nGb], [PB[4]])
                T(lambda e, h=h, hs=hs: e.matmul(out=pbf(5)[:, hs], lhsT=Gb[:, h, :], rhs=Cum, start=True, stop=True), [cst, Gb], [PB[5]])
            V(lambda e: e.scalar_tensor_tensor(out=E1[:], in0=r4(pbf(3)), scalar=0.0, in1=NM1, op0=ALU.min, op1=ALU.add),
              [PB[3], cst], [E1])
            A(lambda e: e.activation(out=E1[:], in_=E1[:], func=AF.Exp), [E1], [E1])
            V(lambda e: e.tensor_tensor(out=E1[:], in0=r4(pbf(1)), in1=E1[:], op=ALU.mult), [PB[1], E1], [E1])
            V(lambda e: e.tensor_tensor(out=Y[:], in0=E1[:], in1=bc(nb4), op=ALU.mult), [E1, NBET], [Y])
            V(lambda e: e.scalar_tensor_tensor(out=E2[:], in0=r4(pbf(4)), scalar=0.0, in1=NM2, op0=ALU.min, op1=ALU.add),
              [PB[4], cst], [E2])
            A(lambda e: e.activation(out=E2[:], in_=E2[:], func=AF.Exp), [E2], [E2])
            V(lambda e: e.tensor_tensor(out=At[:], in0=r4(pbf(2)), in1=E2[:], op=ALU.mult), [PB[2], E2], [At])
            A(lambda e: e.activation(out=E3[:], in_=r4(pbf(5)), func=AF.Exp), [PB[5]], [E3])
            V(lambda e: e.tensor_tensor(out=qg[:], in0=Pq[:, 0:4, c0:c0 + 128], in1=E3[:], op=ALU.mult), [Pq, E3], [qg])
            for h in range(4):
                T(lambda e, h=h: e.transpose(out=pbf(6)[:, h * 128:(h + 1) * 128], in_=Y[:, h, :], identity=cst[:, 0, :]), [Y, cst], [PB[6]])
            A(lambda e: e.copy(out=Z[:], in_=r4(pbf(6))), [PB[6]], [Z])
            V(lambda e: e.tensor_tensor(out=Rf[:], in0=Z[:], in1=cst[:, 0, :].unsqueeze(1).to_broadcast([128, 4, 128]), op=ALU.add),
              [Z, cst], [Rf])
            for lvl in range(1, 7):
                for h in range(4):
                    hs = slice(h * 128, (h + 1) * 128)
                    T(lambda e, h=h, hs=hs: e.matmul(out=pbf(3)[:, hs], lhsT=Z[:, h, :], rhs=Y[:, h, :], start=True, stop=True), [Y, Z], [PB[3]])
                    if lvl < 6:
                        T(lambda e, h=h, hs=hs: e.matmul(out=pbf(4)[:, hs], lhsT=Y[:, h, :], rhs=Z[:, h, :], start=True, stop=True), [Y, Z], [PB[4]])
                A(lambda e: e.copy(out=Y[:], in_=r4(pbf(3))), [PB[3]], [Y])
                if lvl < 6:
                    V(lambda e: e.tensor_copy(out=Z[:], in_=r4(pbf(4))), [PB[4]], [Z])
                for h in range(4):
                    hs = slice(h * 128, (h + 1) * 128)
                    T(lambda e, h=h, hs=hs: e.matmul(out=pbf(5)[:, hs], lhsT=Y[:, h, :], rhs=Rf[:, h, :], start=True, stop=True), [Y, Rf], [PB[5]])
                V(lambda e: e.tensor_tensor(out=Rf[:], in0=r4(pbf(5)), in1=Rf[:], op=ALU.add), [PB[5], Rf], [Rf])
            A(lambda e: e.copy(out=R[:], in_=Rf[:]), [Rf], [R])
            V(lambda e: e.tensor_tensor(out=kbg[:], in0=kv[:, 0:4, :], in1=bc(sml[:, 3, :]), op=ALU.mult), [kv, sml], [kbg])
            V(lambda e: e.tensor_tensor(out=kd[:], in0=kv[:, 0:4, :], in1=bc(sml[:, 1, :]), op=ALU.mult), [kv, sml], [kd])
            V(lambda e: e.tensor_tensor(out=vb[:], in0=kv[:, 4:8, :], in1=bc(b4), op=ALU.mult), [kv, BET], [vb])
            for h in range(4):
                hs = slice(h * 128, (h + 1) * 128)
                T(lambda e, h=h, hs=hs: e.matmul(out=pbf(1)[:, hs], lhsT=R[:, h, :], rhs=vb[:, h, :], start=True, stop=True), [R, vb], [PB[1]])
                T(lambda e, h=h, hs=hs: e.matmul(out=pbf(2)[:, hs], lhsT=kbg[:, h, :], rhs=R[:, h, :], start=True, stop=True), [R, kbg], [PB[2]])
            A(lambda e: e.copy(out=Usb[:], in_=r4(pbf(1))), [PB[1]], [Usb])
            V(lambda e: e.tensor_copy(out=WT[:], in_=r4(pbf(2))), [PB[2]], [WT])
            for h in range(4):
                hs = slice(h * 128, (h + 1) * 128)
                T(lambda e, h=h, hs=hs: e.matmul(out=pbf(6)[:, hs], lhsT=WT[:, h, :], rhs=Sbf[:, h, :], start=True, stop=True), [WT, Sbf], [PB[6]])
            V(lambda e: e.tensor_tensor(out=vnew[:], in0=Usb[:], in1=r4(pbf(6)), op=ALU.subtract), [Usb, PB[6]], [vnew])
            if seg == 1:
                for h in range(4):
                    hs = slice(h * 128, (h + 1) * 128)
                    T(lambda e, h=h, hs=hs: e.matmul(out=pbf(7)[:, hs], lhsT=qg[:, h, :], rhs=Sbf[:, h, :], start=True, stop=False), [qg, Sbf], [PB[7]])
                    T(lambda e, h=h, hs=hs: e.matmul(out=pbf(7)[:, hs], lhsT=At[:, h, :], rhs=vnew[:, h, :], start=False, stop=True), [At, vnew], [PB[7]])
                if d == 0:
                    A(lambda e: e.copy(out=oacc[:, i], in_=r4(pbf(7))), [PB[7]], [oacc])
                else:
                    V(lambda e: e.tensor_tensor(out=oacc[:, i], in0=r4(pbf(7)), in1=oacc[:, i], op=ALU.add), [PB[7], oacc], [oacc])
            for h in range(4):
                hs = slice(h * 128, (h + 1) * 128)
                T(lambda e, h=h, hs=hs: e.matmul(out=pbf(0)[:, hs], lhsT=kd[:, h, :], rhs=vnew[:, h, :], start=True, stop=True), [kd, vnew], [PB[0]])
            V(lambda e: e.tensor_tensor(out=S[:], in0=S[:], in1=bc(sml[:, 2, :]), op=ALU.mult), [S, sml], [S])
            V(lambda e: e.tensor_tensor(out=S[:], in0=r4(pbf(0)), in1=S[:], op=ALU.add), [PB[0], S], [S])
            A(lambda e: e.copy(out=Sbf[:], in_=S[:]), [S], [Sbf])


def gla_scan(fw, PB, pbf, pbb, cst, cstb, gqk, gv, LRT, wa2_d, ba_d, oacc):
    V = lambda fn, r, w: fw.op("dve", fn, r, w)
    A = lambda fn, r, w: fw.op("act", fn, r, w)
    T = lambda fn, r, w: fw.op("pe", fn, r, w)
    ident_b = cstb[:, 0, :]
    r4 = lambda ap: ap.rearrange("p (h t) -> p h t", t=128)
    wa2f = fw.sb("wa2f", [16, 2, 256])
    baf = fw.sb("baf", [1, 2, 256])
    wa2b = fw.sb("wa2b", [16, 2, 256], BF16)
    bab = fw.sb("bab", [1, 2, 256], BF16)
    fw.dma("sp", wa2f[:], wa2_d.rearrange("d r c -> r d c"), wa2f, None)
    fw.dma("sp", baf[:], ba_d.rearrange("d o c -> o d c"), baf, None)
    V(lambda e: e.tensor_copy(out=wa2b[:], in_=wa2f[:]), [wa2f], [wa2b])
    V(lambda e: e.tensor_copy(out=bab[:], in_=baf[:]), [baf], [bab])
    S = fw.sb("gS", [64, 4, 128])
    Sbf = fw.sb("gSbf", [64, 4, 128], BF16)
    sp = fw.sb("gsp", [128, 256])
    bT = fw.sb("gbT", [64, 4, 128])
    eb = fw.sb("geb", [64, 4, 128])
    enb = fw.sb("genb", [64, 4, 128])
    ekd = fw.sb("gekd", [64, 4, 128])
    Qp = fw.sb("gQp", [64, 4, 128], BF16)
    Kp = fw.sb("gKp", [64, 4, 128], BF16)
    KdT = fw.sb("gKdT", [64, 4, 128], BF16)
    Kd = fw.sb("gKd", [128, 4, 64], BF16)
    att = fw.sb("gatt", [128, 4, 128], BF16)
    for d in (0, 1):
        CumS = cst[:, 10 + d, :]
        MK = cst[:, 7 + d, :].unsqueeze(1).to_broadcast([128, 4, 128])
        tl = 127 if d == 0 else 0
        V(lambda e: e.memset(S[:], 0.0), [], [S])
        V(lambda e: e.memset(Sbf[:], 0.0), [], [Sbf])
        order = [(0, i) for i in ((0, 1) if d == 0 else (1, 0))] + \
                [(1, i) for i in (range(16) if d == 0 else range(15, -1, -1))]
        for (seg, i) in order:
            gi = i if seg == 0 else 2 + i
            t0 = gi * 128
            T(lambda e: e.matmul(out=pbf(0)[:, 0:256], lhsT=LRT[:, d, t0:t0 + 128], rhs=wa2b[:, d, :], start=True, stop=False),
              [LRT, wa2b], [PB[0]])
            T(lambda e: e.matmul(out=pbf(0)[:, 0:256], lhsT=cstb[0:1, 9, :], rhs=bab[0:1, d, :], start=False, stop=True),
              [cstb, bab], [PB[0]])
            A(lambda e: e.activation(out=sp[:], in_=pbf(0)[:, 0:256], func=AF.Exp, scale=-1.0), [PB[0]], [sp])
            A(lambda e: e.activation(out=sp[:], in_=sp[:], func=AF.Ln, bias=1.0), [sp], [sp])
            for h in range(4):
                T(lambda e, h=h: e.matmul(out=pbf(1)[0:64, h * 128:(h + 1) * 128], lhsT=sp[:, h * 64:(h + 1) * 64], rhs=CumS,
                                          start=True, stop=True), [sp, cst], [PB[1]])
            A(lambda e: e.copy(out=bT[:], in_=r4(pbf(1)[0:64, :])), [PB[1]], [bT])
            A(lambda e: e.activation(out=eb[:], in_=bT[:], func=AF.Exp), [bT], [eb])
            A(lambda e: e.activation(out=enb[:], in_=bT[:], func=AF.Exp, scale=-1.0), [bT], [enb])
            V(lambda e: e.tensor_tensor(out=ekd[:], in0=bT[:], in1=bT[:, :, tl:tl + 1].to_broadcast([64, 4, 128]), op=ALU.subtract),
              [bT], [ekd])
            A(lambda e: e.activation(out=ekd[:], in_=ekd[:], func=AF.Exp, scale=-1.0), [ekd], [ekd])
            V(lambda e: e.tensor_tensor(out=Qp[:], in0=gqk[:, 0:4, t0:t0 + 128], in1=eb[:], op=ALU.mult), [gqk, eb], [Qp])
            V(lambda e: e.tensor_tensor(out=Kp[:], in0=gqk[:, 4:8, t0:t0 + 128], in1=enb[:], op=ALU.mult), [gqk, enb], [Kp])
            V(lambda e: e.tensor_tensor(out=KdT[:], in0=gqk[:, 4:8, t0:t0 + 128], in1=ekd[:], op=ALU.mult), [gqk, ekd], [KdT])
            for h in range(4):
                T(lambda e, h=h: e.transpose(out=pbb(2)[:, h * 64:(h + 1) * 64], in_=KdT[:, h, :], identity=cstb[0:64, 0, 0:64]),
                  [KdT, cstb], [PB[2]])
            A(lambda e: e.copy(out=Kd[:].rearrange("p h k -> p (h k)"), in_=pbb(2)[:, 0:256]), [PB[2]], [Kd])
            if seg == 1:
                for h in range(4):
                    T(lambda e, h=h: e.matmul(out=pbf(3)[:, h * 128:(h + 1) * 128], lhsT=Kp[:, h, :], rhs=Qp[:, h, :], start=True, stop=True),
                      [Kp, Qp], [PB[3]])
                V(lambda e: e.tensor_tensor(out=att[:], in0=r4(pbf(3)), in1=MK, op=ALU.mult), [PB[3], cst], [att])
                for h in range(4):
                    hs = slice(h * 128, (h + 1) * 128)
                    T(lambda e, h=h, hs=hs: e.matmul(out=pbf(4)[:, hs], lhsT=Qp[:, h, :], rhs=Sbf[:, h, :], start=True, stop=False),
                      [Qp, Sbf], [PB[4]])
                    T(lambda e, h=h, hs=hs: e.matmul(out=pbf(4)[:, hs], lhsT=att[:, h, :], rhs=gv[:, gi, hs], start=False, stop=True),
                      [att, gv], [PB[4]])
                if d == 0:
                    A(lambda e: e.copy(out=oacc[:, i], in_=r4(pbf(4))), [PB[4]], [oacc])
                else:
                    V(lambda e: e.tensor_tensor(out=oacc[:, i], in0=r4(pbf(4)), in1=oacc[:, i], op=ALU.add), [PB[4], oacc], [oacc])
            for h in range(4):
                hs = slice(h * 128, (h + 1) * 128)
                T(lambda e, h=h, hs=hs: e.matmul(out=pbf(5)[0:64, hs], lhsT=Kd[:, h, :], rhs=gv[:, gi, hs], start=True, stop=True),
                  [Kd, gv], [PB[5]])
            V(lambda e: e.tensor_tensor(out=S[:], in0=S[:], in1=eb[:, :, tl:tl + 1].to_broadcast([64, 4, 128]), op=ALU.mult), [S, eb], [S])
            V(lambda e: e.tensor_tensor(out=S[:], in0=r4(pbf(5)[0:64, :]), in1=S[:], op=ALU.add), [PB[5], S], [S])
            A(lambda e: e.copy(out=Sbf[:], in_=S[:]), [S], [Sbf])


def head_norm_gate(fw, PB, pbb, cstb, oacc, gate, gnorm, mixT, chunk0, permute):
    V = lambda fn, r, w: fw.op("dve", fn, r, w)
    A = lambda fn, r, w: fw.op("act", fn, r, w)
    T = lambda fn, r, w: fw.op("pe", fn, r, w)
    ident_b = cstb[:, 0, :]
    sq = fw.sb("hsq", [128, 4, 128])
    ss = fw.sb("hss", [128, 4])
    mix = fw.sb("hmix", [128, 4, 128], BF16)
    for i in range(NT_L):
        o = oacc[:, i]
        V(lambda e: e.tensor_tensor(out=sq[:], in0=o, in1=o, op=ALU.mult), [oacc], [sq])
        V(lambda e: e.tensor_reduce(out=ss[:], in_=sq[:], axis=AX.X, op=ALU.add), [sq], [ss])
        A(lambda e: e.activation(out=ss[:], in_=ss[:], func=AF.Sqrt, scale=1.0 / 128, bias=EPS), [ss], [ss])
        V(lambda e: e.reciprocal(out=ss[:], in_=ss[:]), [ss], [ss])
        V(lambda e: e.tensor_tensor(out=sq[:], in0=o, in1=ss[:].unsqueeze(2).to_broadcast([128, 4, 128]), op=ALU.mult), [oacc, ss], [sq])
        V(lambda e: e.tensor_tensor(out=sq[:], in0=sq[:], in1=gnorm[:].unsqueeze(1).to_broadcast([128, 4, 128]), op=ALU.mult), [sq, gnorm], [sq])
        V(lambda e: e.tensor_tensor(out=mix[:], in0=sq[:], in1=gate[:, i, :].rearrange("p (h t) -> p h t", t=128), op=ALU.mult),
          [sq, gate], [mix])
        bk = i % 2
        for h in range(4):
            T(lambda e, h=h: e.transpose(out=pbb(bk)[:, h * 128:(h + 1) * 128], in_=mix[:, h, :], identity=ident_b), [mix, cstb], [PB[bk]])
        if not permute:
            A(lambda e: e.copy(out=mixT[:, chunk0:chunk0 + 4, i * 128:(i + 1) * 128],
                               in_=pbb(bk)[:, 0:512].rearrange("p (h t) -> p h t", t=128)), [PB[bk]], [mixT])
        else:
            for h in range(4):
                dst = mixT[:, chunk0 + h, :].rearrange("p (r c) -> p c r", c=64)[:, 4 * i:4 * i + 4, :]
                src = pbb(bk)[:, h * 128:(h + 1) * 128].rearrange("p (c r) -> p c r", r=32)
                if h % 2:
                    A(lambda e, dst=dst, src=src: e.copy(out=dst, in_=src), [PB[bk]], [mixT])
                else:
                    V(lambda e, dst=dst, src=src: e.tensor_copy(out=dst, in_=src), [PB[bk]], [mixT])


class StopBuild(Exception):
    pass


NSEQ = 2
TL = 2048
TC = 256
NT_L = 16
NT_C = 2
WCOLS = 3632


def build_program(stage=9):
    nc = bass.Bass("TRN2", target_bir_lowering=False)
    din = lambda n, s: nc.dram_tensor(n, list(s), F32, kind="ExternalInput").ap()
    x_d = din("x", [NSEQ, TL, 1024])
    ctx_d = din("ctx", [NSEQ, TC, 1024])
    cT_d = din("cT", [128, 8, 3])
    wada_d = din("w_ada", [1024, 6144])
    badaT_d = din("b_adaT", [128, 48])
    n1g_d = din("n1gT", [128, 8])
    n2g_d = din("n2gT", [128, 8])
    fing_d = din("final_g", [1, 1024])
    win_d = din("w_in", [1024, WCOLS])
    convT_d = din("convT", [128, 12, 5])
    alog_d = din("a_log", [1, 8])
    dtb_d = din("dt_bias", [1, 8])
    dng_d = din("dn_g", [1, 128])
    glg_d = din("gla_g", [1, 128])
    wa2_d = din("wa2", [2, 16, 256])
    ba_d = din("ba", [2, 1, 256])
    wout_d = din("w_out", [1024, 1024])
    wq_d = din("wq", [1024, 2048])
    keysT_d = din("keysT", [128, 16, 128])
    u_d = din("peer_u", [16384, 1024])
    v_d = din("peer_v", [16384, 1024])
    consts_d = din("consts", [128, 12, 128])
    out_d = nc.dram_tensor("out", [NSEQ, TL, 1024], F32, kind="ExternalOutput").ap()
    dbg_d = nc.dram_tensor("dbg", [128, 8, TL], F32, kind="ExternalOutput").ap() if stage < 2 else None
    PDBG = (stage == 3.5)
    X1OUT = (stage == 8)
    if stage == 8:
        stage = 9
    if PDBG:
        dps_d = nc.dram_tensor("dps", [128, 2048], F32, kind="ExternalOutput").ap()
        dptop_d = nc.dram_tensor("dptop", [128, 256], F32, kind="ExternalOutput").ap()
        dpcv_d = nc.dram_tensor("dpcv", [128, 128], F32, kind="ExternalOutput").ap()
        dpt4_d = nc.dram_tensor("dpt4", [128, 512], F32, kind="ExternalOutput").ap()
        dpt4t_d = nc.dram_tensor("dpt4t", [128, 512], F32, kind="ExternalOutput").ap()
        dpw_d = nc.dram_tensor("dpw", [128, 128, 8], F32, kind="ExternalOutput").ap()
        dppo_d = nc.dram_tensor("dppo", [128, 1024], F32, kind="ExternalOutput").ap()
        dpsr_d = nc.dram_tensor("dpsr", [128, 2, 2048], F32, kind="ExternalOutput").ap()
        dpab_d = nc.dram_tensor("dpab", [128, 2, 512], F32, kind="ExternalOutput").ap()
    modrow_d = nc.dram_tensor("modrow", [3, 6144], F32, kind="Internal").ap()
    x1_d = nc.dram_tensor("x1s", [NSEQ * TL, 1024], F32, kind=("ExternalOutput" if X1OUT else "Internal")).ap()
    ss_d = nc.dram_tensor("sscr", [2, 8, NSEQ * TL, 128], F32, kind="Internal").ap()
    ut_d = nc.dram_tensor("utscr", [128, 128, 8, 128], BF16, kind="Internal").ap()
    vb_d = nc.dram_tensor("vbscr", [128, 128, 1024], BF16, kind="Internal").ap()

    with contextlib.ExitStack() as top:
        fw = FW(nc, top)
        V = lambda fn, r, w: fw.op("dve", fn, r, w)
        A = lambda fn, r, w: fw.op("act", fn, r, w)
        G = lambda fn, r, w: fw.op("pool", fn, r, w)
        T = lambda fn, r, w: fw.op("pe", fn, r, w)

        PS = top.enter_context(nc.psum_tensor("ps", [128, 4096], F32))
        PB = [Buf("pb%d" % i, PS[:, i * 512:(i + 1) * 512]) for i in range(8)]

        def pbf(i, n=1):
            return PS[:, i * 512:(i + n) * 512]

        def pbb(i, n=1):
            return PS[:, i * 512:(i + n) * 512].bitcast(BF16)

        outb = fw.view("outb")
        cst = fw.sb("cst", [128, 12, 128])
        fw.dma("sp", cst[:], consts_d, cst, None)
        cstb = fw.sb("cstb", [128, 12, 128], BF16)
        V(lambda e: e.tensor_copy(out=cstb[:], in_=cst[:]), [cst], [cstb])
        ident_f = cst[:, 0, :]
        ident_b = cstb[:, 0, :]
        ones_b = cstb[:, 9, :]
        ones_f = cst[:, 9, :]

        modT = fw.sb("modT", [128, 48, 3])
        G1 = fw.sb("G1", [128, 8, 3])
        G2 = fw.sb("G2", [128, 8, 3])
        modrow = fw.view("modrow")
        x1buf = fw.view("x1buf")

        with fw.scope():
            cT = fw.sb("cT", [128, 8, 3])
            fw.dma("sp", cT[:], cT_d, cT, None)
            siluT = fw.sb("siluT", [128, 8, 3])
            A(lambda e: e.activation(out=siluT[:], in_=cT[:], func=AF.Silu), [cT], [siluT])
            badaT = fw.sb("badaT", [128, 48])
            fw.dma("sp", badaT[:], badaT_d, badaT, None)
            n1g = fw.sb("n1g", [128, 8])
            n2g = fw.sb("n2g", [128, 8])
            fw.dma("sp", n1g[:], n1g_d, n1g, None)
            fw.dma("sp", n2g[:], n2g_d, n2g, None)
            slabs = [fw.sb("wada%d" % i, [128, 8, 1024]) for i in range(2)]
            wv = wada_d.rearrange("(k p) c -> p k c", p=128)
            pm = pbf(0)[:, 0:144].rearrange("p (j b) -> p j b", b=3)
            for v in range(6):
                sl = slabs[v % 2]
                fw.dma("sp" if v % 2 == 0 else "act", sl[:], wv[:, :, v * 1024:(v + 1) * 1024], sl, None)
                for ch in range(8):
                    j = v * 8 + ch
                    for k in range(8):
                        T(lambda e, sl=sl, ch=ch, k=k, j=j: e.matmul(
                            out=pm[:, j, :], lhsT=sl[:, k, ch * 128:(ch + 1) * 128], rhs=siluT[:, k, :],
                            start=(k == 0), stop=(k == 7)), [sl, siluT], [PB[0]])
            V(lambda e: e.tensor_tensor(out=modT[:], in0=pm[:, 0:48, :],
                                        in1=badaT[:].unsqueeze(2).to_broadcast([128, 48, 3]), op=ALU.add),
              [PB[0], badaT], [modT])
            for (Gt, ng, o) in ((G1, n1g, 8), (G2, n2g, 32)):
                V(lambda e, Gt=Gt, o=o: e.tensor_scalar(out=Gt[:], in0=modT[:, o:o + 8, :], scalar1=1.0, scalar2=None,
                                                        op0=ALU.add), [modT], [Gt])
                V(lambda e, Gt=Gt, ng=ng: e.tensor_tensor(out=Gt[:], in0=Gt[:],
                                                          in1=ng[:].unsqueeze(2).to_broadcast([128, 8, 3]), op=ALU.mult),
                  [Gt, ng], [Gt])
            for b in range(3):
                fw.dma("pool", modrow_d[b].rearrange("(j p) -> p j", p=128), modT[:, :, b], modrow, modT,
                       allow_slow_non_contiguous=True)

        def make_hT(hT, col0, src_ap, ntiles, Gt, SHo, mcol, xt_bufs, wk, perm=False, srcbuf=None):
            for i in range(ntiles):
                xt = xt_bufs[i % 2]
                fw.dma("sp", xt[:], src_ap[i * 128:(i + 1) * 128, :], xt, srcbuf)
                junk, ssq, xn = wk
                A(lambda e, xt=xt: e.activation(out=junk[:], in_=xt[:], func=AF.Square, accum_out=ssq[:]),
                  [xt], [junk, ssq])
                A(lambda e: e.activation(out=ssq[:], in_=ssq[:], func=AF.Sqrt, scale=1.0 / 1024, bias=EPS), [ssq], [ssq])
                V(lambda e: e.reciprocal(out=ssq[:], in_=ssq[:]), [ssq], [ssq])
                V(lambda e, xt=xt: e.tensor_scalar(out=xn[:], in0=xt[:], scalar1=ssq[:, 0:1], scalar2=None, op0=ALU.mult),
                  [xt, ssq], [xn])
                pt = pbb(7).rearrange("p (k t) -> p k t", t=128)
                for k in range(8):
                    T(lambda e, k=k: e.transpose(out=pt[:, k, :], in_=xn[:, k * 128:(k + 1) * 128], identity=ident_b),
                      [xn, cstb], [PB[7]])
                if not perm:
                    dst = hT[:, :, col0 + i * 128: col0 + (i + 1) * 128]
                    src = pt[:, 0:8, :]
                    g_b = Gt[:, :, mcol:mcol + 1].to_broadcast([128, 8, 128])
                    s_b = modT[:, SHo:SHo + 8, mcol:mcol + 1].to_broadcast([128, 8, 128])
                else:
                    dst = hT[:, :, col0:col0 + TL].rearrange("p k (c r) -> p k r c", r=32)[:, :, 2 * i:2 * i + 2, :]
                    src = pt[:, 0:8, :].rearrange("p k (r c) -> p k r c", c=64)
                    g_b = Gt[:, :, mcol:mcol + 1].unsqueeze(3).to_broadcast([128, 8, 2, 64])
                    s_b = modT[:, SHo:SHo + 8, mcol:mcol + 1].unsqueeze(3).to_broadcast([128, 8, 2, 64])
                V(lambda e, dst=dst, src=src, g_b=g_b: e.tensor_tensor(out=dst, in0=src, in1=g_b, op=ALU.mult),
                  [PB[7], Gt], [hT])
                V(lambda e, dst=dst, s_b=s_b: e.tensor_tensor(out=dst, in0=dst, in1=s_b, op=ALU.add), [hT, modT], [hT])

        def load_w_bf16(dst, src_view, ncols, stage_bufs, step=512, d0=0):
            n = 0
            for c0 in range(0, ncols, step):
                c1 = min(ncols, c0 + step)
                sg = stage_bufs[n % 2]
                fw.dma("act" if n % 2 else "sp", sg[:, :, 0:c1 - c0], src_view[:, :, c0:c1], sg, None)
                if n % 2:
                    A(lambda e, sg=sg, c0=c0, c1=c1: e.copy(out=dst[:, :, d0 + c0:d0 + c1], in_=sg[:, :, 0:c1 - c0]), [sg], [dst])
                else:
                    V(lambda e, sg=sg, c0=c0, c1=c1: e.tensor_copy(out=dst[:, :, d0 + c0:d0 + c1], in_=sg[:, :, 0:c1 - c0]), [sg], [dst])
                n += 1

        win_v = win_d.rearrange("(k p) c -> p k c", p=128)

        if 0.6 < stage < 0.7:
            fw.dn_limit = int(round((stage - 0.6) * 1000))

        def ck(x):
            if stage <= x:
                fw.dead = True

        for s in range(NSEQ if stage >= 0.1 else 0):
          try:
            with fw.scope():
                mixT = fw.sb("mixT", [128, 8, TL], BF16)
                with fw.scope():
                    Pq = fw.sb("Pq", [128, 12, TC + TL + 8], BF16)
                    CO = (2, 262)
                    Zg = fw.sb("Zg", [128, NT_L, 512], BF16)
                    SM = fw.sb("SM", [128, 18, 16])
                    with fw.scope():
                        hT = fw.sb("hT", [128, 8, TC + TL], BF16)
                        xts = [fw.sb("xt%d" % i, [128, 1024]) for i in range(2)]
                        wk = (fw.sb("junk", [128, 1024], BF16), fw.sb("ssq", [128, 1]), fw.sb("xn", [128, 1024], BF16))
                        make_hT(hT, 0, ctx_d[s], NT_C, G1, 0, 2, xts, wk)
                        make_hT(hT, TC, x_d[s], NT_L, G1, 0, s, xts, wk)
                        ck(0.2)
                        wdn = fw.sb("wdn", [128, 8, 2064], BF16)
                        stg = [fw.sb("stg%d" % i, [128, 8, 128]) for i in range(2)]
                        load_w_bf16(wdn, win_v[:, :, 0:2048], 2048, stg, 128)
                        load_w_bf16(wdn, win_v[:, :, 3584:3600], 16, stg, 128, 2048)
                        V(lambda e: e.memset(Pq[:], 0.0), [], [Pq])
                        ck(0.3)
                        nb = 0
                        for (t0, tn, seg) in [(0, 256, 0)] + [(TC + b * 512, 512, 1) for b in range(4)]:
                            for c in range(12):
                                bk = nb % 2
                                nb += 1
                                for k in range(8):
                                    T(lambda e, bk=bk, c=c, k=k, t0=t0, tn=tn: e.matmul(
                                        out=pbf(bk)[:, 0:tn], lhsT=wdn[:, k, c * 128:(c + 1) * 128], rhs=hT[:, k, t0:t0 + tn],
                                        start=(k == 0), stop=(k == 7)), [wdn, hT], [PB[bk]])
                                d0 = CO[seg] + (t0 - (TC if seg else 0))
                                if bk:
                                    A(lambda e, bk=bk, c=c, d0=d0, tn=tn: e.copy(out=Pq[:, c, d0:d0 + tn], in_=pbf(bk)[:, 0:tn]),
                                      [PB[bk]], [Pq])
                                else:
                                    V(lambda e, bk=bk, c=c, d0=d0, tn=tn: e.tensor_copy(out=Pq[:, c, d0:d0 + tn], in_=pbf(bk)[:, 0:tn]),
                                      [PB[bk]], [Pq])
                        for i in range(18):
                            t0 = i * 128
                            if i >= 2:
                                for k in range(8):
                                    T(lambda e, k=k, t0=t0: e.matmul(out=pbf(2), lhsT=hT[:, k, t0:t0 + 128], rhs=wdn[:, k, 1536:2048],
                                                                     start=(k == 0), stop=(k == 7)), [wdn, hT], [PB[2]])
                                A(lambda e, i=i: e.activation(out=Zg[:, i - 2, :], in_=pbf(2), func=AF.Silu), [PB[2]], [Zg])
                            for k in range(8):
                                T(lambda e, k=k, t0=t0: e.matmul(out=pbf(3)[:, 0:16], lhsT=hT[:, k, t0:t0 + 128], rhs=wdn[:, k, 2048:2064],
                                                                 start=(k == 0), stop=(k == 7)), [wdn, hT], [PB[3]])
                            V(lambda e, i=i: e.tensor_copy(out=SM[:, i, :], in_=pbf(3)[:, 0:16]), [PB[3]], [SM])
                    ck(0.4)
                    BET = fw.sb("BET", [128, 18, 8])
                    NBET = fw.sb("NBET", [128, 18, 8])
                    GG = fw.sb("GG", [128, 18, 8])
                    alog = fw.sb("alog", [128, 8])
                    dtb = fw.sb("dtb", [128, 8])
                    fw.dma("sp", alog[:], alog_d.partition_broadcast(128), alog, None)
                    fw.dma("sp", dtb[:], dtb_d.partition_broadcast(128), dtb, None)
                    A(lambda e: e.activation(out=BET[:], in_=SM[:, :, 0:8], func=AF.Sigmoid), [SM], [BET])
                    V(lambda e: e.tensor_scalar(out=NBET[:], in0=BET[:], scalar1=-1.0, scalar2=None, op0=ALU.mult), [BET], [NBET])
                    V(lambda e: e.tensor_tensor(out=GG[:], in0=SM[:, :, 8:16], in1=dtb[:].unsqueeze(1).to_broadcast([128, 18, 8]),
                                                op=ALU.add), [SM, dtb], [GG])
                    A(lambda e: e.activation(out=GG[:], in_=GG[:], func=AF.Exp), [GG], [GG])
                    A(lambda e: e.activation(out=GG[:], in_=GG[:], func=AF.Ln, bias=1.0), [GG], [GG])
                    A(lambda e: e.activation(out=alog[:], in_=alog[:], func=AF.Exp), [alog], [alog])
                    V(lambda e: e.tensor_scalar(out=alog[:], in0=alog[:], scalar1=-1.0, scalar2=None, op0=ALU.mult), [alog], [alog])
                    V(lambda e: e.tensor_tensor(out=GG[:], in0=GG[:], in1=alog[:].unsqueeze(1).to_broadcast([128, 18, 8]),
                                                op=ALU.mult), [GG, alog], [GG])
                    ck(0.5)
                    cw = fw.sb("cw", [128, 12, 5])
                    fw.dma("sp", cw[:], convT_d, cw, None)
                    with fw.scope():
                        acc = fw.sb("acc", [128, TL])
                        sq = fw.sb("sq", [128, 512], BF16)
                        rin = fw.sb("rin", [128, 512])
                        for c in range(12):
                            for seg, n in ((0, TC), (1, TL)):
                                o = CO[seg]
                                V(lambda e, c=c, o=o, n=n: e.tensor_scalar(out=acc[:, 0:n], in0=Pq[:, c, o - 2:o - 2 + n],
                                                                           scalar1=cw[:, c, 0:1], scalar2=None, op0=ALU.mult),
                                  [Pq, cw], [acc])
                                for j in range(1, 5):
                                    V(lambda e, c=c, o=o, n=n, j=j: e.scalar_tensor_tensor(
                                        out=acc[:, 0:n], in0=Pq[:, c, o - 2 + j:o - 2 + j + n], scalar=cw[:, c, j:j + 1],
                                        in1=acc[:, 0:n], op0=ALU.mult, op1=ALU.add), [Pq, cw, acc], [acc])
                                if c >= 8:
                                    A(lambda e, c=c, o=o, n=n: e.activation(out=Pq[:, c, o:o + n], in_=acc[:, 0:n], func=AF.Silu),
                                      [acc], [Pq])
                                    continue
                                A(lambda e, n=n: e.activation(out=acc[:, 0:n], in_=acc[:, 0:n], func=AF.Silu), [acc], [acc])
                                for b0 in range(0, n, 512):
                                    bn = min(512, n - b0)
                                    V(lambda e, b0=b0, bn=bn: e.tensor_tensor(out=sq[:, 0:bn], in0=acc[:, b0:b0 + bn],
                                                                              in1=acc[:, b0:b0 + bn], op=ALU.mult), [acc], [sq])
                                    T(lambda e, bn=bn: e.matmul(out=pbf(0)[:, 0:bn], lhsT=ones_b, rhs=sq[:, 0:bn], start=True, stop=True),
                                      [sq, cstb], [PB[0]])
                                    A(lambda e, bn=bn: e.activation(out=rin[:, 0:bn], in_=pbf(0)[:, 0:bn], func=AF.Sqrt, bias=EPS),
                                      [PB[0]], [rin])
                                    V(lambda e, bn=bn: e.reciprocal(out=rin[:, 0:bn], in_=rin[:, 0:bn]), [rin], [rin])
                                    sc = (128.0 ** -0.5) if c < 4 else 1.0
                                    V(lambda e, c=c, o=o, b0=b0, bn=bn, sc=sc: e.scalar_tensor_tensor(
                                        out=Pq[:, c, o + b0:o + b0 + bn], in0=acc[:, b0:b0 + bn], scalar=sc, in1=rin[:, 0:bn],
                                        op0=ALU.mult, op1=ALU.mult), [acc, rin], [Pq])
                    ck(0.6)
                    oacc = fw.sb("oacc", [128, NT_L, 4, 128])
                    with fw.scope():
                        dn_scan(fw, nc, PS, PB, pbf, pbb, cst, cstb, Pq, CO, BET, NBET, GG, oacc)
                    ck(0.7)
                    dng = fw.sb("dng", [128, 128])
                    fw.dma("sp", dng[:], dng_d.partition_broadcast(128), dng, None)
                    with fw.scope():
                        head_norm_gate(fw, PB, pbb, cstb, oacc, Zg, dng, mixT, 0, False)

                with fw.scope():
                    gqk = fw.sb("gqk", [64, 8, TC + TL], BF16)
                    gv = fw.sb("gv", [128, 18, 512], BF16)
                    Rg = fw.sb("Rg", [128, NT_L, 512], BF16)
                    LRT = fw.sb("LRT", [16, 2, TC + TL], BF16)
                    with fw.scope():
                        hT = fw.sb("hTg", [128, 8, TC + TL], BF16)
                        xts = [fw.sb("xtg%d" % i, [128, 1024]) for i in range(2)]
                        wk = (fw.sb("junkg", [128, 1024], BF16), fw.sb("ssqg", [128, 1]), fw.sb("xng", [128, 1024], BF16))
                        make_hT(hT, 0, ctx_d[s], NT_C, G1, 0, 2, xts, wk)
                        make_hT(hT, TC, x_d[s], NT_L, G1, 0, s, xts, wk, perm=True)
                        wgl = fw.sb("wgl", [128, 8, 1568], BF16)
                        stg = [fw.sb("stgg%d" % i, [128, 8, 128]) for i in range(2)]
                        load_w_bf16(wgl, win_v[:, :, 2048:3584], 1536, stg, 128)
                        load_w_bf16(wgl, win_v[:, :, 3600:3632], 32, stg, 128, 1536)
                        nb = 0
                        for (t0, tn) in [(0, 256)] + [(TC + b * 512, 512) for b in range(4)]:
                            for g in range(10):
                                bk = nb % 2
                                nb += 1
                                if g < 8:
                                    cs, m = slice(g * 64, (g + 1) * 64), 64
                                else:
                                    cs, m = slice(1536 + (g - 8) * 16, 1536 + (g - 7) * 16), 16
                                for k in range(8):
                                    T(lambda e, bk=bk, k=k, cs=cs, m=m, t0=t0, tn=tn: e.matmul(
                                        out=pbf(bk)[0:m, 0:tn], lhsT=wgl[:, k, cs], rhs=hT[:, k, t0:t0 + tn],
                                        start=(k == 0), stop=(k == 7)), [wgl, hT], [PB[bk]])
                                if g < 4:
                                    A(lambda e, bk=bk, g=g, t0=t0, tn=tn: e.mul(out=gqk[:, g, t0:t0 + tn], in_=pbf(bk)[0:64, 0:tn], mul=0.125),
                                      [PB[bk]], [gqk])
                                elif g < 8:
                                    V(lambda e, bk=bk, g=g, t0=t0, tn=tn: e.tensor_copy(out=gqk[:, g, t0:t0 + tn], in_=pbf(bk)[0:64, 0:tn]),
                                      [PB[bk]], [gqk])
                                else:
                                    V(lambda e, bk=bk, g=g, t0=t0, tn=tn: e.tensor_copy(out=LRT[:, g - 8, t0:t0 + tn], in_=pbf(bk)[0:16, 0:tn]),
                                      [PB[bk]], [LRT])
                        for i in range(18):
                            t0 = i * 128
                            for k in range(8):
                                T(lambda e, k=k, t0=t0: e.matmul(out=pbf(2), lhsT=hT[:, k, t0:t0 + 128], rhs=wgl[:, k, 512:1024],
                                                                 start=(k == 0), stop=(k == 7)), [wgl, hT], [PB[2]])
                            V(lambda e, i=i: e.tensor_copy(out=gv[:, i, :], in_=pbf(2)), [PB[2]], [gv])
                            if i >= 2:
                                for k in range(8):
                                    T(lambda e, k=k, t0=t0: e.matmul(out=pbf(3), lhsT=hT[:, k, t0:t0 + 128], rhs=wgl[:, k, 1024:1536],
                                                                     start=(k == 0), stop=(k == 7)), [wgl, hT], [PB[3]])
                                A(lambda e, i=i: e.activation(out=Rg[:, i - 2, :], in_=pbf(3), func=AF.Silu), [PB[3]], [Rg])
                    ck(1.2)
                    oacc = fw.sb("oaccg", [128, NT_L, 4, 128])
                    with fw.scope():
                        gla_scan(fw, PB, pbf, pbb, cst, cstb, gqk, gv, LRT, wa2_d, ba_d, oacc)
                    ck(1.3)
                    glg = fw.sb("glg", [128, 128])
                    fw.dma("sp", glg[:], glg_d.partition_broadcast(128), glg, None)
                    with fw.scope():
                        head_norm_gate(fw, PB, pbb, cstb, oacc, Rg, glg, mixT, 4, True)
                ck(1.4)
                with fw.scope():
                    wo = fw.sb("wo", [128, 8, 1024], BF16)
                    stg = [fw.sb("stgo%d" % i, [128, 8, 128]) for i in range(2)]
                    load_w_bf16(wo, wout_d.rearrange("(k p) c -> p k c", p=128), 1024, stg, 128)
                    g1B = fw.sb("g1B", [128, 1024])
                    fw.dma("sp", g1B[:], modrow_d[s:s + 1, 2048:3072].partition_broadcast(128), g1B, modrow)
                    xts = [fw.sb("xto%d" % i, [128, 1024]) for i in range(2)]
                    x1ts = [fw.sb("x1t%d" % i, [128, 1024]) for i in range(2)]
                    for i in range(NT_L):
                        xt = xts[i % 2]
                        x1t = x1ts[i % 2]
                        fw.dma("sp", xt[:], x_d[s][i * 128:(i + 1) * 128, :], xt, None)
                        for hf in range(2):
                            bk = 2 * (i % 2) + hf
                            for k in range(8):
                                T(lambda e, k=k, i=i, hf=hf, bk=bk: e.matmul(out=pbf(bk), lhsT=mixT[:, k, i * 128:(i + 1) * 128],
                                                                             rhs=wo[:, k, hf * 512:(hf + 1) * 512],
                                                                             start=(k == 0), stop=(k == 7)), [mixT, wo], [PB[bk]])
                            hs = slice(hf * 512, (hf + 1) * 512)
                            V(lambda e, bk=bk, hs=hs, x1t=x1t: e.tensor_tensor(out=x1t[:, hs], in0=pbf(bk), in1=g1B[:, hs], op=ALU.mult),
                              [PB[bk], g1B], [x1t])
                            V(lambda e, hs=hs, x1t=x1t, xt=xt: e.tensor_tensor(out=x1t[:, hs], in0=x1t[:, hs], in1=xt[:, hs], op=ALU.add),
                              [x1t, xt], [x1t])
                        fw.dma("pool", x1_d[s * TL + i * 128:s * TL + (i + 1) * 128, :], x1t[:], x1buf, x1t)
                ck(1.45)
                if stage < 2:
                    if s == 0:
                        with fw.scope():
                            dts = [fw.sb("dtmp%d" % i, [128, TL]) for i in range(2)]
                            for ch in range(8):
                                dt_ = dts[ch % 2]
                                V(lambda e, ch=ch, dt_=dt_: e.tensor_copy(out=dt_[:], in_=mixT[:, ch, :]), [mixT], [dt_])
                                fw.dma("sp", dbg_d[:, ch, :], dt_[:], outb, dt_)
                        fw.dead = True
                    continue
          except StopBuild:
            break
        fw.dead = False
        if stage >= 3:
            utscr = fw.view("utscr")
            vbscr = fw.view("vbscr")
            with fw.scope():
                uview = u_d.rearrange("(i j) d -> j i d", j=128)
                vview = v_d.rearrange("(i j) d -> j i d", j=128)
                ubs = [fw.sb("ub%d" % i, [128, 1024]) for i in range(2)]
                vbs = [fw.sb("vb%d" % i, [128, 1024]) for i in range(2)]
                ubb = [fw.sb("ubb%d" % i, [128, 1024], BF16) for i in range(2)]
                vbb = [fw.sb("vbb%d" % i, [128, 1024], BF16) for i in range(2)]
                utb = [fw.sb("utb%d" % i, [128, 8, 128], BF16) for i in range(2)]
                for j in range(128):
                    q = j % 2
                    fw.dma("sp", ubs[q][:], uview[j], ubs[q], None)
                    fw.dma("act", vbs[q][:], vview[j], vbs[q], None)
                    A(lambda e, q=q: e.copy(out=ubb[q][:], in_=ubs[q][:]), [ubs[q]], [ubb[q]])
                    V(lambda e, q=q: e.tensor_copy(out=vbb[q][:], in_=vbs[q][:]), [vbs[q]], [vbb[q]])
                    for k in range(8):
                        T(lambda e, q=q, k=k: e.transpose(out=pbb(q)[:, k * 128:(k + 1) * 128], in_=ubb[q][:, k * 128:(k + 1) * 128],
                                                          identity=ident_b), [ubb[q], cstb], [PB[q]])
                    V(lambda e, q=q: e.tensor_copy(out=utb[q][:].rearrange("p k i -> p (k i)"), in_=pbb(q)), [PB[q]], [utb[q]])
                    fw.dma("pool", ut_d[j], utb[q][:], utscr, utb[q])
                    fw.dma("pool", vb_d[j], vbb[q][:], vbscr, vbb[q])
            with fw.scope():
                wqb = fw.sb("wqb", [128, 8, 2048], BF16)
                stg = [fw.sb("stgp%d" % i, [128, 8, 128]) for i in range(2)]
                load_w_bf16(wqb, wq_d.rearrange("(k p) c -> p k c", p=128), 2048, stg, 128)
                keysf = fw.sb("keysf", [128, 16, 128])
                keysb = fw.sb("keysb", [128, 16, 128], BF16)
                fw.dma("sp", keysf[:], keysT_d, keysf, None)
                V(lambda e: e.tensor_copy(out=keysb[:], in_=keysf[:]), [keysf], [keysb])
                g2B = fw.sb("g2B", [128, 2, 1024])
                for sq in range(NSEQ):
                    fw.dma("sp", g2B[:, sq, :], modrow_d[sq:sq + 1, 5120:6144].partition_broadcast(128), g2B, modrow)
                fingB = fw.sb("fingB", [128, 1024])
                fw.dma("sp", fingB[:], fing_d.partition_broadcast(128), fingB, None)
                h2T = fw.sb("h2T", [128, 8, 128], BF16)
                xts = [fw.sb("xtp%d" % i, [128, 1024]) for i in range(2)]
                wk = (fw.sb("junkp", [128, 1024], BF16), fw.sb("ssqp", [128, 1]), fw.sb("xnp", [128, 1024], BF16))
                qT = fw.sb("qT", [128, 16, 128], BF16)
                ssb = fw.sb("ssb", [128, 16, 128])
                top = fw.sb("top", [128, 16, 16])
                wkk = fw.sb("wkk", [128, 128])
                cand = fw.sb("cand", [128, 8, 256])
                wk2 = fw.sb("wk2", [128, 256])
                cv = fw.sb("cv", [128, 8, 16])
                T4s = fw.sb("T4s", [128, 4, 128])
                T4 = fw.sb("T4", [128, 4, 128])
                zz = fw.sb("zz", [128, 8, 16])
                rZ = fw.sb("rZ", [128, 8])
                Srep = [fw.sb("Srep%d" % i, [128, 16, 128]) for i in range(2)]
                A0 = fw.sb("A0", [128, 16, 128], BF16)
                Ab = fw.sb("Ab", [128, 16, 128], BF16)
                cmpb = fw.sb("cmpb", [128, 16, 128], BF16)
                Bb = fw.sb("Bb", [128, 16, 128], BF16)
                Wsb = fw.sb("Wsb", [128, 128, 128], BF16)
                utl = [fw.sb("utl%d" % i, [128, 8, 128], BF16) for i in range(3)]
                vtl = [fw.sb("vtl%d" % i, [128, 1024], BF16) for i in range(3)]
                gef = [fw.sb("gef%d" % i, [128, 128]) for i in range(2)]
                gw = [fw.sb("gw%d" % i, [128, 128], BF16) for i in range(2)]
                x2 = fw.sb("x2", [128, 1024])
                ssbuf = [fw.view("ssbuf%d" % i) for i in range(2)]
                tv = top[:].rearrange("p (h q) k -> p h q k", q=2)
                bk3 = lambda ap: ap.unsqueeze(2).to_broadcast([128, 16, 128])
                for blk in range(NSEQ * NT_L):
                    sq = blk // NT_L
                    make_hT(h2T, 0, x1_d[blk * 128:(blk + 1) * 128, :], 1, G2, 24, sq, xts, wk, srcbuf=x1buf)
                    xt = xts[0]
                    for hp in range(16):
                        bk = hp % 2
                        for k in range(8):
                            T(lambda e, bk=bk, hp=hp, k=k: e.matmul(out=pbf(bk)[:, 0:128], lhsT=wqb[:, k, hp * 128:(hp + 1) * 128],
                                                                    rhs=h2T[:, k, :], start=(k == 0), stop=(k == 7)), [wqb, h2T], [PB[bk]])
                        if bk:
                            A(lambda e, bk=bk, hp=hp: e.copy(out=qT[:, hp, :], in_=pbf(bk)[:, 0:128]), [PB[bk]], [qT])
                        else:
                            V(lambda e, bk=bk, hp=hp: e.tensor_copy(out=qT[:, hp, :], in_=pbf(bk)[:, 0:128]), [PB[bk]], [qT])
                    for g in range(4):
                        bk = 2 + g % 2
                        for q in range(4):
                            hp = g * 4 + q
                            T(lambda e, bk=bk, hp=hp, q=q: e.matmul(out=pbf(bk)[:, q * 128:(q + 1) * 128], lhsT=qT[:, hp, :],
                                                                    rhs=keysb[:, hp, :], start=True, stop=True), [qT, keysb], [PB[bk]])
                        A(lambda e, bk=bk, g=g: e.copy(out=ssb[:, g * 4:(g + 1) * 4, :].rearrange("p a i -> p (a i)"), in_=pbf(bk)),
                          [PB[bk]], [ssb])
                    sc = ssbuf[blk % 2]
                    for p in range(2):
                        fw.dma("pool", ss_d[p, :, blk * 128:(blk + 1) * 128, :].rearrange("h t i -> t h i"),
                               ssb[:].rearrange("p (h q) i -> p h q i", q=2)[:, :, p, :], sc, ssb)
                    for hp in range(16):
                        V(lambda e, hp=hp: e.max(out=top[:, hp, 0:8], in_=ssb[:, hp, :]), [ssb], [top])
                        V(lambda e, hp=hp: e.match_replace(out=wkk[:], in_to_replace=top[:, hp, 0:8], in_values=ssb[:, hp, :],
                                                           imm_value=-1e30), [ssb, top], [wkk])
                        V(lambda e, hp=hp: e.max(out=top[:, hp, 8:16], in_=wkk[:]), [wkk], [top])
                    V(lambda e: e.tensor_tensor(out=cand[:].rearrange("p h (a b) -> p h a b", b=16),
                                                in0=tv[:, :, 0, :].unsqueeze(3).to_broadcast([128, 8, 16, 16]),
                                                in1=tv[:, :, 1, :].unsqueeze(2).to_broadcast([128, 8, 16, 16]), op=ALU.add), [top], [cand])
                    for h in range(8):
                        V(lambda e, h=h: e.max(out=cv[:, h, 0:8], in_=cand[:, h, :]), [cand], [cv])
                        V(lambda e, h=h: e.match_replace(out=wk2[:], in_to_replace=cv[:, h, 0:8], in_values=cand[:, h, :],
                                                         imm_value=-1e30), [cand, cv], [wk2])
                        V(lambda e, h=h: e.max(out=cv[:, h, 8:16], in_=wk2[:]), [wk2], [cv])
                    t4 = lambda q: T4s[:, q, :].rearrange("p (h k) -> p h k", k=16)
                    b16 = lambda ap: ap.to_broadcast([128, 8, 16])
                    V(lambda e: e.tensor_copy(out=t4(0), in_=tv[:, :, 0, :]), [top], [T4s])
                    V(lambda e: e.tensor_scalar(out=t4(1), in0=tv[:, :, 0, :], scalar1=-1.0, scalar2=-1e-5, op0=ALU.mult, op1=ALU.add),
                      [top], [T4s])
                    V(lambda e: e.tensor_tensor(out=t4(1), in0=t4(1), in1=b16(cv[:, :, 15:16]), op=ALU.add), [T4s, cv], [T4s])
                    V(lambda e: e.tensor_tensor(out=zz[:], in0=cv[:], in1=b16(cv[:, :, 0:1]), op=ALU.subtract), [cv], [zz])
                    A(lambda e: e.activation(out=zz[:], in_=zz[:], func=AF.Exp), [zz], [zz])
                    V(lambda e: e.tensor_reduce(out=rZ[:], in_=zz[:], axis=AX.X, op=ALU.add), [zz], [rZ])
                    V(lambda e: e.reciprocal(out=rZ[:], in_=rZ[:]), [rZ], [rZ])
                    V(lambda e: e.tensor_tensor(out=t4(2), in0=tv[:, :, 0, :], in1=b16(tv[:, :, 0, 0:1]), op=ALU.subtract), [top], [T4s])
                    A(lambda e: e.activation(out=t4(2), in_=t4(2), func=AF.Exp), [T4s], [T4s])
                    V(lambda e: e.tensor_tensor(out=t4(2), in0=t4(2), in1=b16(rZ[:].unsqueeze(2)), op=ALU.mult), [T4s, rZ], [T4s])
                    V(lambda e: e.tensor_copy(out=t4(3), in_=b16(tv[:, :, 1, 0:1])), [top], [T4s])
                    for q in range(4):
                        T(lambda e, q=q: e.transpose(out=pbf(4)[:, q * 128:(q + 1) * 128], in_=T4s[:, q, :], identity=ident_f),
                          [T4s, cst], [PB[4]])
                    A(lambda e: e.copy(out=T4[:].rearrange("p q t -> p (q t)"), in_=pbf(4)), [PB[4]], [T4])
                    if PDBG and blk == 0:
                        fw.dma("sp", dps_d, ssb[:].rearrange("p a i -> p (a i)"), outb, ssb)
                        fw.dma("sp", dptop_d, top[:].rearrange("p a i -> p (a i)"), outb, top)
                        fw.dma("sp", dpcv_d, cv[:].rearrange("p a i -> p (a i)"), outb, cv)
                        fw.dma("sp", dpt4_d, T4s[:].rearrange("p a i -> p (a i)"), outb, T4s)
                        fw.dma("sp", dpt4t_d, T4[:].rearrange("p a i -> p (a i)"), outb, T4)
                    for sbk in range(8):
                        ta = blk * 128 + sbk * 16
                        tsl = slice(sbk * 16, (sbk + 1) * 16)
                        for p in range(2):
                            for h in range(8):
                                fw.dma("sp" if h % 2 else "act", Srep[p][h * 16:(h + 1) * 16, :, :].rearrange("q t i -> q (t i)"),
                                       ss_d[p, h:h + 1, ta:ta + 16, :].rearrange("o t i -> o (t i)").partition_broadcast(16),
                                       Srep[p], sc)
                        if PDBG and blk == 0 and sbk == 0:
                            for p in range(2):
                                fw.dma("sp", dpsr_d[:, p, :], Srep[p][:].rearrange("p a i -> p (a i)"), outb, Srep[p])
                        V(lambda e, tsl=tsl: e.tensor_tensor(out=A0[:], in0=Srep[0][:], in1=bk3(T4[:, 0, tsl]), op=ALU.is_equal),
                          [Srep[0], T4], [A0])
                        V(lambda e, tsl=tsl: e.tensor_tensor(out=Ab[:], in0=A0[:], in1=bk3(T4[:, 2, tsl]), op=ALU.mult), [A0, T4], [Ab])
                        V(lambda e, tsl=tsl: e.tensor_tensor(out=cmpb[:], in0=Srep[1][:], in1=bk3(T4[:, 1, tsl]), op=ALU.is_ge),
                          [Srep[1], T4], [cmpb])
                        V(lambda e, tsl=tsl: e.tensor_tensor(out=Srep[1][:], in0=Srep[1][:], in1=bk3(T4[:, 3, tsl]), op=ALU.subtract),
                          [Srep[1], T4], [Srep[1]])
                        A(lambda e: e.activation(out=Srep[1][:], in_=Srep[1][:], func=AF.Exp), [Srep[1]], [Srep[1]])
                        V(lambda e: e.tensor_tensor(out=Bb[:], in0=cmpb[:], in1=Srep[1][:], op=ALU.mult), [cmpb, Srep[1]], [Bb])
                        if PDBG and blk == 0 and sbk == 0:
                            abd = fw.sb("abd", [128, 2, 512])
                            V(lambda e: e.tensor_copy(out=abd[:, 0, :], in_=Ab[:, 0:4, :].rearrange("p a i -> p (a i)")), [Ab], [abd])
                            V(lambda e: e.tensor_copy(out=abd[:, 1, :], in_=Bb[:, 0:4, :].rearrange("p a i -> p (a i)")), [Bb], [abd])
                            fw.dma("sp", dpab_d, abd[:], outb, abd)
                        for t in range(16):
                            bk = 2 + (t // 4) % 2
                            T(lambda e, bk=bk, t=t: e.matmul(out=pbf(bk)[:, (t % 4) * 128:(t % 4 + 1) * 128], lhsT=Ab[:, t, :], rhs=Bb[:, t, :],
                                                             start=True, stop=True), [Ab, Bb], [PB[bk]])
                            if t % 4 == 3:
                                t0 = sbk * 16 + t - 3
                                dst = Wsb[:, :, t0:t0 + 4].rearrange("p j t -> p t j")
                                src = pbf(bk).rearrange("p (t j) -> p t j", j=128)
                                if (t // 4) % 2:
                                    A(lambda e, dst=dst, src=src: e.copy(out=dst, in_=src), [PB[bk]], [Wsb])
                                else:
                                    V(lambda e, dst=dst, src=src: e.tensor_copy(out=dst, in_=src), [PB[bk]], [Wsb])
                    if PDBG and blk == 0:
                        wdb = fw.sb("wdb", [128, 128, 8])
                        V(lambda e: e.tensor_copy(out=wdb[:], in_=Wsb[:, :, 0:8]), [Wsb], [wdb])
                        fw.dma("sp", dpw_d, wdb[:], outb, wdb)
                    for j in range(128):
                        ut, vt = utl[j % 3], vtl[j % 3]
                        fw.dma("sp", ut[:], ut_d[j], ut, utscr)
                        fw.dma("act", vt[:], vb_d[j], vt, vbscr)
                        bk = 4 + j % 2
                        for k in range(8):
                            T(lambda e, bk=bk, k=k, ut=ut: e.matmul(out=pbf(bk)[:, 0:128], lhsT=ut[:, k, :], rhs=h2T[:, k, :],
                                                                    start=(k == 0), stop=(k == 7)), [ut, h2T], [PB[bk]])
                        ge_, gw_ = gef[j % 2], gw[j % 2]
                        A(lambda e, bk=bk, ge_=ge_: e.activation(out=ge_[:], in_=pbf(bk)[:, 0:128], func=AF.Gelu), [PB[bk]], [ge_])
                        V(lambda e, ge_=ge_, gw_=gw_, j=j: e.tensor_tensor(out=gw_[:], in0=ge_[:], in1=Wsb[:, j, :], op=ALU.mult),
                          [ge_, Wsb], [gw_])
                        for hf in range(2):
                            T(lambda e, hf=hf, gw_=gw_, vt=vt, j=j: e.matmul(out=pbf(6 + hf), lhsT=gw_[:], rhs=vt[:, hf * 512:(hf + 1) * 512],
                                                                             start=(j == 0), stop=(j == 127)), [gw_, vt], [PB[6 + hf]])
                    if PDBG and blk == 0:
                        V(lambda e: e.tensor_copy(out=x2[:], in_=pbf(6, 2)), [PB[6], PB[7]], [x2])
                        fw.dma("sp", dppo_d, x2[:], outb, x2)
                    for hf in range(2):
                        hs = slice(hf * 512, (hf + 1) * 512)
                        V(lambda e, hf=hf, hs=hs: e.tensor_tensor(out=x2[:, hs], in0=pbf(6 + hf), in1=g2B[:, sq, hs], op=ALU.mult),
                          [PB[6 + hf], g2B], [x2])
                    V(lambda e: e.tensor_tensor(out=x2[:], in0=x2[:], in1=xt[:], op=ALU.add), [x2, xt], [x2])
                    junk, ssq, xn = wk
                    A(lambda e: e.activation(out=junk[:], in_=x2[:], func=AF.Square, accum_out=ssq[:]), [x2], [junk, ssq])
                    A(lambda e: e.activation(out=ssq[:], in_=ssq[:], func=AF.Sqrt, scale=1.0 / 1024, bias=EPS), [ssq], [ssq])
                    V(lambda e: e.reciprocal(out=ssq[:], in_=ssq[:]), [ssq], [ssq])
                    V(lambda e: e.scalar_tensor_tensor(out=x2[:], in0=x2[:], scalar=ssq[:, 0:1], in1=fingB[:], op0=ALU.mult, op1=ALU.mult),
                      [x2, ssq, fingB], [x2])
                    ti = blk % NT_L
                    fw.dma("pool", out_d[sq][ti * 128:(ti + 1) * 128, :], x2[:], outb, x2)
                    if PDBG:
                        fw.dead = True
        fw.dead = False
        fw.finish([outb], "sp")
        fw.barrier()
    return nc


def _layout(inp, core):
    b0 = 2 * core
    f = lambda a: np.ascontiguousarray(a, dtype=np.float32)
    csel = np.stack([inp["c"][b0], inp["c"][b0 + 1], inp["c_ctx"]])
    w_in = inp["w_in"][0]
    w_in_r = np.concatenate([w_in[:, 0:2048], w_in[:, 2064:3600], w_in[:, 2048:2064], w_in[:, 3600:3632]], axis=1)
    return {
        "x": f(inp["x"][b0:b0 + 2]), "ctx": f(inp["ctx"][b0:b0 + 2]),
        "cT": f(csel.reshape(3, 8, 128).transpose(2, 1, 0)),
        "w_ada": f(inp["w_ada"][0]), "b_adaT": f(inp["b_ada"][0].reshape(48, 128).T),
        "n1gT": f(inp["norm1_g"][0].reshape(8, 128).T), "n2gT": f(inp["norm2_g"][0].reshape(8, 128).T),
        "final_g": f(inp["final_g"].reshape(1, 1024)), "w_in": f(w_in_r),
        "convT": f(inp["conv_w"][0].T.reshape(12, 128, 5).transpose(1, 0, 2)),
        "a_log": f(inp["dn_a_log"][0].reshape(1, 8)), "dt_bias": f(inp["dn_dt_bias"][0].reshape(1, 8)),
        "dn_g": f(inp["dn_norm_g"][0].reshape(1, 128)), "gla_g": f(inp["gla_norm_g"][0].reshape(1, 128)),
        "wa2": f(inp["gla_wa2"][0]), "ba": f(inp["gla_ba"][0].reshape(2, 1, 256)),
        "w_out": f(inp["w_out"][0]), "wq": f(inp["peer_wq"][0]),
        "keysT": f(inp["peer_keys"][0].reshape(16, 128, 128).transpose(2, 0, 1)),
        "peer_u": f(inp["peer_u"][0]), "peer_v": f(inp["peer_v"][0]),
        "consts": make_consts(),
    }


def kernel(**inputs):
    inp = {k: np.asarray(v) for k, v in inputs.items()}
    nc = build_program(9)
    in_maps = [_layout(inp, c) for c in range(8)]
    res = run_bass_kernel_spmd(nc, in_maps, core_ids=list(range(8)))
    return np.concatenate([r["out"] for r in res.results], axis=0).astype(np.float32)
```

```python
import contextlib
import numpy as np
import concourse.bass as bass
import concourse.mybir as mybir
from concourse.bass_utils import run_bass_kernel_spmd

F32 = mybir.dt.float32
BF16 = mybir.dt.bfloat16
ALU = mybir.AluOpType
AF = mybir.ActivationFunctionType
AX = mybir.AxisListType

NEG = -30000.0
EPS = 1e-6


class Buf:
    __slots__ = ("name", "t", "wev", "revs", "dsem", "dcnt", "pre")

    def __init__(self, name, t=None):
        self.name = name
        self.t = t
        self.wev = []
        self.revs = []
        self.dsem = None
        self.dcnt = 0
        self.pre = []

    def __getitem__(self, k):
        return self.t[k]


class Eng:
    def __init__(self, name, h, sem):
        self.name = name
        self.h = h
        self.sem = sem
        self.cnt = 0
        self.known = {}


class FW:
    def __init__(self, nc, stack):
        self.nc = nc
        self.top = stack
        self.stack = stack
        self.engs = {}
        self.dsems = []
        for name, h in (("pe", nc.tensor), ("act", nc.scalar), ("dve", nc.vector),
                        ("pool", nc.gpsimd), ("sp", nc.sync)):
            sem = stack.enter_context(nc.semaphore("s_" + name))
            self.engs[name] = Eng(name, h, sem)
        self.ninst = 0

    @contextlib.contextmanager
    def scope(self):
        old = self.stack
        with contextlib.ExitStack() as st:
            self.stack = st
            try:
                yield
            finally:
                self.barrier()
                self.stack = old

    def sb(self, name, shape, dt=F32):
        self.nsb = getattr(self, "nsb", 0) + 1
        name = "sb%d_%s" % (self.nsb, name)
        t = self.stack.enter_context(self.nc.sbuf_tensor(name, list(shape), dt))
        return Buf(name, t)

    def view(self, name, t=None):
        return Buf(name, t)

    def _waits(self, e, reads, writes, skip=None):
        need = {}
        for b in reads:
            for (s, v) in b.wev:
                if need.get(s, 0) < v:
                    need[s] = v
        for b in writes:
            for (s, v) in b.wev:
                if need.get(s, 0) < v:
                    need[s] = v
            for (s, v) in b.revs:
                if need.get(s, 0) < v:
                    need[s] = v
        for s, v in need.items():
            if s is skip or (e.name == "pe" and s is e.sem):
                continue
            if e.known.get(s, 0) < v:
                e.h.wait_ge(s, v)
                e.known[s] = v

    def op(self, eng, fn, reads=(), writes=()):
        if getattr(self, "dead", False):
            return None
        e = self.engs[eng]
        self._waits(e, reads, writes)
        ins = fn(e.h)
        e.cnt += 1
        ins.then_inc(e.sem, 1)
        self.ninst += 1
        ev = (e.sem, e.cnt)
        for b in reads:
            if len(b.revs) > 24:
                b.revs = b.revs[-12:] + self._maxev(b.revs[:-12])
            b.revs.append(ev)
        for b in writes:
            b.wev = [ev]
            b.revs = []
        return ins

    @staticmethod
    def _maxev(evs):
        d = {}
        for (s, v) in evs:
            if d.get(s, (None, 0))[1] < v:
                d[s] = (s, v)
        return list(d.values())

    def dma(self, q, out_ap, in_ap, dst, src, **kw):
        if getattr(self, "dead", False):
            return None
        e = self.engs[q]
        reads = [src] if src is not None else []
        if dst.dsem is None:
            dst.dsem = self.top.enter_context(self.nc.semaphore("d%d_%s" % (len(self.dsems), dst.name)))
            self.dsems.append(dst)
        evs = [ev for ev in dst.wev if ev[0] is not dst.dsem] + list(dst.revs)
        if evs:
            dst.pre = evs
        else:
            evs = dst.pre
        for (sm, v) in evs:
            if e.known.get(sm, 0) < v:
                e.h.wait_ge(sm, v)
                e.known[sm] = v
        self._waits(e, reads, [], skip=dst.dsem)
        ins = e.h.dma_start(out=out_ap, in_=in_ap, **kw)
        self.ninst += 1
        dst.dcnt += 16
        ins.then_inc(dst.dsem, 16)
        ev = (dst.dsem, dst.dcnt)
        if src is not None:
            src.revs.append(ev)
        dst.wev = [ev]
        dst.revs = []
        return ins

    def barrier(self):
        for e in self.engs.values():
            for f in self.engs.values():
                if f is e or f.cnt == 0:
                    continue
                if e.known.get(f.sem, 0) < f.cnt:
                    e.h.wait_ge(f.sem, f.cnt)
                    e.known[f.sem] = f.cnt
            for b in self.dsems:
                if b.dcnt and e.known.get(b.dsem, 0) < b.dcnt:
                    e.h.wait_ge(b.dsem, b.dcnt)
                    e.known[b.dsem] = b.dcnt

    def finish(self, bufs, eng="sp"):
        self._waits(self.engs[eng], bufs, [])


def make_consts():
    p = np.arange(128)[:, None]
    f = np.arange(128)[None, :]
    c = np.zeros((128, 13, 128), np.float32)
    c[:, 0] = (p == f)
    c[:, 1] = (p <= f)
    c[:, 2] = (p >= f)
    c[:, 3] = np.where(p > f, 0.0, NEG)
    c[:, 4] = np.where(p < f, 0.0, NEG)
    c[:, 5] = np.where(p <= f, 0.0, NEG)
    c[:, 6] = np.where(p >= f, 0.0, NEG)
    c[:, 7] = (p <= f)
    c[:, 8] = (p >= f)
    c[:, 9] = 1.0
    c[:, 10] = -(p <= f).astype(np.float32) / 16.0
    c[:, 11] = -(p >= f).astype(np.float32) / 16.0
    c[:, 12] = f + 0.0 * p
    return c


def dn_scan(fw, nc, PS, PB, pbf, pbb, cst, cstb, Pq, CO, BET, NBET, GG, oacc):
    V = lambda fn, r, w: fw.op("dve", fn, r, w)
    A = lambda fn, r, w: fw.op("act", fn, r, w)
    T = lambda fn, r, w: fw.op("pe", fn, r, w)
    ident_b = cstb[:, 0, :]
    ones_f = cst[:, 9, :]
    r4 = lambda ap: ap.rearrange("p (h t) -> p h t", t=128)
    S = fw.sb("dnS", [128, 4, 128])
    Sbf = fw.sb("dnSbf", [128, 4, 128], BF16)
    kv = fw.sb("dkv", [128, 8, 128], BF16)
    Gb = fw.sb("dGb", [128, 4, 128])
    nGb = fw.sb("dnGb", [128, 4, 128])
    sml = fw.sb("dsml", [128, 4, 4])
    gct = fw.sb("dgct", [128, 8])
    E1 = fw.sb("dE1", [128, 4, 128])
    E2 = fw.sb("dE2", [128, 4, 128])
    E3 = fw.sb("dE3", [128, 4, 128])
    Y = fw.sb("dY", [128, 4, 128])
    Z = fw.sb("dZ", [128, 4, 128])
    Rf = fw.sb("dRf", [128, 4, 128])
    R = fw.sb("dR", [128, 4, 128], BF16)
    At = fw.sb("dAt", [128, 4, 128], BF16)
    qg = fw.sb("dqg", [128, 4, 128], BF16)
    kbg = fw.sb("dkbg", [128, 4, 128], BF16)
    kd = fw.sb("dkd", [128, 4, 128], BF16)
    vb = fw.sb("dvb", [128, 4, 128], BF16)
    Usb = fw.sb("dU", [128, 4, 128])
    WT = fw.sb("dWT", [128, 4, 128], BF16)
    vnew = fw.sb("dvn", [128, 4, 128], BF16)
    bc = lambda ap: ap.unsqueeze(2).to_broadcast([128, 4, 128])
    for d in (0, 1):
        Cum = cst[:, 1 + d, :]
        NM1 = cst[:, 3 + d, :].unsqueeze(1).to_broadcast([128, 4, 128])
        NM2 = cst[:, 5 + d, :].unsqueeze(1).to_broadcast([128, 4, 128])
        V(lambda e: e.memset(S[:], 0.0), [], [S])
        V(lambda e: e.memset(Sbf[:], 0.0), [], [Sbf])
        order = [(0, i) for i in ((0, 1) if d == 0 else (1, 0))] + \
                [(1, i) for i in (range(16) if d == 0 else range(15, -1, -1))]
        for (seg, i) in order:
            fw.ntile = getattr(fw, "ntile", 0) + 1
            if fw.ntile > getattr(fw, "dn_limit", 10 ** 9):
                fw.dead = True
            gi = i if seg == 0 else 2 + i
            c0 = CO[seg] + i * 128
            g4 = GG[:, gi, d * 4:d * 4 + 4]
            b4 = BET[:, gi, d * 4:d * 4 + 4]
            nb4 = NBET[:, gi, d * 4:d * 4 + 4]
            T(lambda e: e.matmul(out=pbf(6)[:, 0:4], lhsT=Cum, rhs=g4, start=True, stop=True), [cst, GG], [PB[6]])
            T(lambda e: e.matmul(out=pbf(6)[:, 4:8], lhsT=ones_f, rhs=g4, start=True, stop=True), [cst, GG], [PB[6]])
            A(lambda e: e.copy(out=gct[:], in_=pbf(6)[:, 0:8]), [PB[6]], [gct])
            A(lambda e: e.activation(out=sml[:, 0, :], in_=gct[:, 0:4], func=AF.Exp), [gct], [sml])
            V(lambda e: e.tensor_tensor(out=sml[:, 1, :], in0=gct[:, 4:8], in1=gct[:, 0:4], op=ALU.subtract), [gct], [sml])
            A(lambda e: e.activation(out=sml[:, 1, :], in_=sml[:, 1, :], func=AF.Exp), [sml], [sml])
            A(lambda e: e.activation(out=sml[:, 2, :], in_=gct[:, 4:8], func=AF.Exp), [gct], [sml])
            V(lambda e: e.tensor_tensor(out=sml[:, 3, :], in0=sml[:, 0, :], in1=b4, op=ALU.mult), [sml, BET], [sml])
            V(lambda e: e.tensor_copy(out=Gb[:], in_=bc(g4)), [GG], [Gb])
            A(lambda e: e.mul(out=nGb[:], in_=Gb[:], mul=-1.0), [Gb], [nGb])
            for h in range(4):
                T(lambda e, h=h: e.transpose(out=pbb(0)[:, h * 128:(h + 1) * 128], in_=Pq[:, 4 + h, c0:c0 + 128], identity=ident_b),
                  [Pq, cstb], [PB[0]])
                T(lambda e, h=h: e.transpose(out=pbb(0)[:, 512 + h * 128:512 + (h + 1) * 128], in_=Pq[:, 8 + h, c0:c0 + 128],
                                             identity=ident_b), [Pq, cstb], [PB[0]])
            A(lambda e: e.copy(out=kv[:].rearrange("p a t -> p (a t)"), in_=pbb(0)), [PB[0]], [kv])
            for h in range(4):
                kT = Pq[:, 4 + h, c0:c0 + 128]
                qT = Pq[:, h, c0:c0 + 128]
                hs = slice(h * 128, (h + 1) * 128)
                T(lambda e, kT=kT, hs=hs: e.matmul(out=pbf(1)[:, hs], lhsT=kT, rhs=kT, start=True, stop=True), [Pq], [PB[1]])
                T(lambda e, kT=kT, qT=qT, hs=hs: e.matmul(out=pbf(2)[:, hs], lhsT=kT, rhs=qT, start=True, stop=True), [Pq], [PB[2]])
                T(lambda e, h=h, hs=hs: e.matmul(out=pbf(3)[:, hs], lhsT=Cum, rhs=Gb[:, h, :], start=True, stop=False), [cst, Gb], [PB[3]])
                T(lambda e, h=h, hs=hs: e.matmul(out=pbf(3)[:, hs], lhsT=nGb[:, h, :], rhs=Cum, start=False, stop=True), [cst, nGb], [PB[3]])
                T(lambda e, h=h, hs=hs: e.matmul(out=pbf(4)[:, hs], lhsT=Gb[:, h, :], rhs=Cum, start=True, stop=False), [cst, Gb], [PB[4]])
                T(lambda e, h=h, hs=hs: e.matmul(out=pbf(4)[:, hs], lhsT=Cum, rhs=nGb[:, h, :], start=False, stop=True), [cst, nGb], [PB[4]])
                T(lambda e, h=h, hs=hs: e.matmul(out=pbf(5)[:, hs], lhsT=Gb[:, h, :], rhs=Cum, start=True, stop=True), [cst, Gb], [PB[5]])
            V(lambda e: e.scalar_tensor_tensor(out=E1[:], in0=r4(pbf(3)), scalar=0.0, in1=NM1, op0=ALU.min, op1=ALU.add),
              [PB[3], cst], [E1])
            A(lambda e: e.activation(out=E1[:], in_=E1[:], func=AF.Exp), [E1], [E1])
            V(lambda e: e.tensor_tensor(out=E1[:], in0=r4(pbf(1)), in1=E1[:], op=ALU.mult), [PB[1], E1], [E1])
            V(lambda e: e.tensor_tensor(out=Y[:], in0=E1[:], in1=bc(nb4), op=ALU.mult), [E1, NBET], [Y])
            V(lambda e: e.scalar_tensor_tensor(out=E2[:], in0=r4(pbf(4)), scalar=0.0, in1=NM2, op0=ALU.min, op1=ALU.add),
              [PB[4], cst], [E2])
            A(lambda e: e.activation(out=E2[:], in_=E2[:], func=AF.Exp), [E2], [E2])
            V(lambda e: e.tensor_tensor(out=At[:], in0=r4(pbf(2)), in1=E2[:], op=ALU.mult), [PB[2], E2], [At])
            A(lambda e: e.activation(out=E3[:], in_=r4(pbf(5)), func=AF.Exp), [PB[5]], [E3])
            V(lambda e: e.tensor_tensor(out=qg[:], in0=Pq[:, 0:4, c0:c0 + 128], in1=E3[:], op=ALU.mult), [Pq, E3], [qg])
            for h in range(4):
                T(lambda e, h=h: e.transpose(out=pbf(6)[:, h * 128:(h + 1) * 128], in_=Y[:, h, :], identity=cst[:, 0, :]), [Y, cst], [PB[6]])
            A(lambda e: e.copy(out=Z[:], in_=r4(pbf(6))), [PB[6]], [Z])
            V(lambda e: e.tensor_tensor(out=Rf[:], in0=Z[:], in1=cst[:, 0, :].unsqueeze(1).to_broadcast([128, 4, 128]), op=ALU.add),
              [Z, cst], [Rf])
            for lvl in range(1, 7):
                for h in range(4):
                    hs = slice(h * 128, (h + 1) * 128)
                    T(lambda e, h=h, hs=hs: e.matmul(out=pbf(3)[:, hs], lhsT=Z[:, h, :], rhs=Y[:, h, :], start=True, stop=True), [Y, Z], [PB[3]])
                    if lvl < 6:
                        T(lambda e, h=h, hs=hs: e.matmul(out=pbf(4)[:, hs], lhsT=Y[:, h, :], rhs=Z[:, h, :], start=True, stop=True), [Y, Z], [PB[4]])
                A(lambda e: e.copy(out=Y[:], in_=r4(pbf(3))), [PB[3]], [Y])
                if lvl < 6:
                    V(lambda e: e.tensor_copy(out=Z[:], in_=r4(pbf(4))), [PB[4]], [Z])
                for h in range(4):
                    hs = slice(h * 128, (h + 1) * 128)
                    T(lambda e, h=h, hs=hs: e.matmul(out=pbf(5)[:, hs], lhsT=Y[:, h, :], rhs=Rf[:, h, :], start=True, stop=True), [Y, Rf], [PB[5]])
                V(lambda e: e.tensor_tensor(out=Rf[:], in0=r4(pbf(5)), in1=Rf[:], op=ALU.add), [PB[5], Rf], [Rf])
            A(lambda e: e.copy(out=R[:], in_=Rf[:]), [Rf], [R])
            V(lambda e: e.tensor_tensor(out=kbg[:], in0=kv[:, 0:4, :], in1=bc(sml[:, 3, :]), op=ALU.mult), [kv, sml], [kbg])
            V(lambda e: e.tensor_tensor(out=kd[:], in0=kv[:, 0:4, :], in1=bc(sml[:, 1, :]), op=ALU.mult), [kv, sml], [kd])
            V(lambda e: e.tensor_tensor(out=vb[:], in0=kv[:, 4:8, :], in1=bc(b4), op=ALU.mult), [kv, BET], [vb])
            for h in range(4):
                hs = slice(h * 128, (h + 1) * 128)
                T(lambda e, h=h, hs=hs: e.matmul(out=pbf(1)[:, hs], lhsT=R[:, h, :], rhs=vb[:, h, :], start=True, stop=True), [R, vb], [PB[1]])
                T(lambda e, h=h, hs=hs: e.matmul(out=pbf(2)[:, hs], lhsT=kbg[:, h, :], rhs=R[:, h, :], start=True, stop=True), [R, kbg], [PB[2]])
            A(lambda e: e.copy(out=Usb[:], in_=r4(pbf(1))), [PB[1]], [Usb])
            V(lambda e: e.tensor_copy(out=WT[:], in_=r4(pbf(2))), [PB[2]], [WT])
            for h in range(4):
                hs = slice(h * 128, (h + 1) * 128)
                T(lambda e, h=h, hs=hs: e.matmul(out=pbf(6)[:, hs], lhsT=WT[:, h, :], rhs=Sbf[:, h, :], start=True, stop=True), [WT, Sbf], [PB[6]])
            V(lambda e: e.tensor_tensor(out=vnew[:], in0=Usb[:], in1=r4(pbf(6)), op=ALU.subtract), [Usb, PB[6]], [vnew])
            if seg == 1:
                for h in range(4):
                    hs = slice(h * 128, (h + 1) * 128)
                    T(lambda e, h=h, hs=hs: e.matmul(out=pbf(7)[:, hs], lhsT=qg[:, h, :], rhs=Sbf[:, h, :], start=True, stop=False), [qg, Sbf], [PB[7]])
                    T(lambda e, h=h, hs=hs: e.matmul(out=pbf(7)[:, hs], lhsT=At[:, h, :], rhs=vnew[:, h, :], start=False, stop=True), [At, vnew], [PB[7]])
                if d == 0:
                    A(lambda e: e.copy(out=oacc[:, i], in_=r4(pbf(7))), [PB[7]], [oacc])
                else:
                    V(lambda e: e.tensor_tensor(out=oacc[:, i], in0=r4(pbf(7)), in1=oacc[:, i], op=ALU.add), [PB[7], oacc], [oacc])
            for h in range(4):
                hs = slice(h * 128, (h + 1) * 128)
                T(lambda e, h=h, hs=hs: e.matmul(out=pbf(0)[:, hs], lhsT=kd[:, h, :], rhs=vnew[:, h, :], start=True, stop=True), [kd, vnew], [PB[0]])
            V(lambda e: e.tensor_tensor(out=S[:], in0=S[:], in1=bc(sml[:, 2, :]), op=ALU.mult), [S, sml], [S])
            V(lambda e: e.tensor_tensor(out=S[:], in0=r4(pbf(0)), in1=S[:], op=ALU.add), [PB[0], S], [S])
            A(lambda e: e.copy(out=Sbf[:], in_=S[:]), [S], [Sbf])


def gla_scan(fw, PB, pbf, pbb, cst, cstb, gqk, gv, LRT, wa2_d, ba_d, oacc):
    V = lambda fn, r, w: fw.op("dve", fn, r, w)
    A = lambda fn, r, w: fw.op("act", fn, r, w)
    T = lambda fn, r, w: fw.op("pe", fn, r, w)
    ident_b = cstb[:, 0, :]
    r4 = lambda ap: ap.rearrange("p (h t) -> p h t", t=128)
    wa2f = fw.sb("wa2f", [16, 2, 256])
    baf = fw.sb("baf", [1, 2, 256])
    wa2b = fw.sb("wa2b", [16, 2, 256], BF16)
    bab = fw.sb("bab", [1, 2, 256], BF16)
    fw.dma("sp", wa2f[:], wa2_d.rearrange("d r c -> r d c"), wa2f, None)
    fw.dma("sp", baf[:], ba_d.rearrange("d o c -> o d c"), baf, None)
    V(lambda e: e.tensor_copy(out=wa2b[:], in_=wa2f[:]), [wa2f], [wa2b])
    V(lambda e: e.tensor_copy(out=bab[:], in_=baf[:]), [baf], [bab])
    S = fw.sb("gS", [64, 4, 128])
    Sbf = fw.sb("gSbf", [64, 4, 128], BF16)
    sp = fw.sb("gsp", [128, 256])
    bT = fw.sb("gbT", [64, 4, 128])
    eb = fw.sb("geb", [64, 4, 128])
    enb = fw.sb("genb", [64, 4, 128])
    ekd = fw.sb("gekd", [64, 4, 128])
    Qp = fw.sb("gQp", [64, 4, 128], BF16)
    Kp = fw.sb("gKp", [64, 4, 128], BF16)
    KdT = fw.sb("gKdT", [64, 4, 128], BF16)
    Kd = fw.sb("gKd", [128, 4, 64], BF16)
    att = fw.sb("gatt", [128, 4, 128], BF16)
    for d in (0, 1):
        CumS = cst[:, 10 + d, :]
        MK = cst[:, 7 + d, :].unsqueeze(1).to_broadcast([128, 4, 128])
        tl = 127 if d == 0 else 0
        V(lambda e: e.memset(S[:], 0.0), [], [S])
        V(lambda e: e.memset(Sbf[:], 0.0), [], [Sbf])
        order = [(0, i) for i in ((0, 1) if d == 0 else (1, 0))] + \
                [(1, i) for i in (range(16) if d == 0 else range(15, -1, -1))]
        for (seg, i) in order:
            gi = i if seg == 0 else 2 + i
            t0 = gi * 128
            T(lambda e: e.matmul(out=pbf(0)[:, 0:256], lhsT=LRT[:, d, t0:t0 + 128], rhs=wa2b[:, d, :], start=True, stop=False),
              [LRT, wa2b], [PB[0]])
            T(lambda e: e.matmul(out=pbf(0)[:, 0:256], lhsT=cstb[0:1, 9, :], rhs=bab[0:1, d, :], start=False, stop=True),
              [cstb, bab], [PB[0]])
            A(lambda e: e.activation(out=sp[:], in_=pbf(0)[:, 0:256], func=AF.Exp, scale=-1.0), [PB[0]], [sp])
            A(lambda e: e.activation(out=sp[:], in_=sp[:], func=AF.Ln, bias=1.0), [sp], [sp])
            for h in range(4):
                T(lambda e, h=h: e.matmul(out=pbf(1)[0:64, h * 128:(h + 1) * 128], lhsT=sp[:, h * 64:(h + 1) * 64], rhs=CumS,
                                          start=True, stop=True), [sp, cst], [PB[1]])
            A(lambda e: e.copy(out=bT[:], in_=r4(pbf(1)[0:64, :])), [PB[1]], [bT])
            A(lambda e: e.activation(out=eb[:], in_=bT[:], func=AF.Exp), [bT], [eb])
            A(lambda e: e.activation(out=enb[:], in_=bT[:], func=AF.Exp, scale=-1.0), [bT], [enb])
            V(lambda e: e.tensor_tensor(out=ekd[:], in0=bT[:], in1=bT[:, :, tl:tl + 1].to_broadcast([64, 4, 128]), op=ALU.subtract),
              [bT], [ekd])
            A(lambda e: e.activation(out=ekd[:], in_=ekd[:], func=AF.Exp, scale=-1.0), [ekd], [ekd])
            V(lambda e: e.tensor_tensor(out=Qp[:], in0=gqk[:, 0:4, t0:t0 + 128], in1=eb[:], op=ALU.mult), [gqk, eb], [Qp])
            V(lambda e: e.tensor_tensor(out=Kp[:], in0=gqk[:, 4:8, t0:t0 + 128], in1=enb[:], op=ALU.mult), [gqk, enb], [Kp])
            V(lambda e: e.tensor_tensor(out=KdT[:], in0=gqk[:, 4:8, t0:t0 + 128], in1=ekd[:], op=ALU.mult), [gqk, ekd], [KdT])
            for h in range(4):
                T(lambda e, h=h: e.transpose(out=pbb(2)[:, h * 64:(h + 1) * 64], in_=KdT[:, h, :], identity=cstb[0:64, 0, 0:64]),
                  [KdT, cstb], [PB[2]])
            A(lambda e: e.copy(out=Kd[:].rearrange("p h k -> p (h k)"), in_=pbb(2)[:, 0:256]), [PB[2]], [Kd])
            if seg == 1:
                for h in range(4):
                    T(lambda e, h=h: e.matmul(out=pbf(3)[:, h * 128:(h + 1) * 128], lhsT=Kp[:, h, :], rhs=Qp[:, h, :], start=True, stop=True),
                      [Kp, Qp], [PB[3]])
                V(lambda e: e.tensor_tensor(out=att[:], in0=r4(pbf(3)), in1=MK, op=ALU.mult), [PB[3], cst], [att])
                for h in range(4):
                    hs = slice(h * 128, (h + 1) * 128)
                    T(lambda e, h=h, hs=hs: e.matmul(out=pbf(4)[:, hs], lhsT=Qp[:, h, :], rhs=Sbf[:, h, :], start=True, stop=False),
                      [Qp, Sbf], [PB[4]])
                    T(lambda e, h=h, hs=hs: e.matmul(out=pbf(4)[:, hs], lhsT=att[:, h, :], rhs=gv[:, gi, hs], start=False, stop=True),
                      [att, gv], [PB[4]])
                if d == 0:
                    A(lambda e: e.copy(out=oacc[:, i], in_=r4(pbf(4))), [PB[4]], [oacc])
                else:
                    V(lambda e: e.tensor_tensor(out=oacc[:, i], in0=r4(pbf(4)), in1=oacc[:, i], op=ALU.add), [PB[4], oacc], [oacc])
            for h in range(4):
                hs = slice(h * 128, (h + 1) * 128)
                T(lambda e, h=h, hs=hs: e.matmul(out=pbf(5)[0:64, hs], lhsT=Kd[:, h, :], rhs=gv[:, gi, hs], start=True, stop=True),
                  [Kd, gv], [PB[5]])
            V(lambda e: e.tensor_tensor(out=S[:], in0=S[:], in1=eb[:, :, tl:tl + 1].to_broadcast([64, 4, 128]), op=ALU.mult), [S, eb], [S])
            V(lambda e: e.tensor_tensor(out=S[:], in0=r4(pbf(5)[0:64, :]), in1=S[:], op=ALU.add), [PB[5], S], [S])
            A(lambda e: e.copy(out=Sbf[:], in_=S[:]), [S], [Sbf])


def head_norm_gate(fw, PB, pbb, cstb, oacc, gate, gnorm, mixT, chunk0, permute):
    V = lambda fn, r, w: fw.op("dve", fn, r, w)
    A = lambda fn, r, w: fw.op("act", fn, r, w)
    T = lambda fn, r, w: fw.op("pe", fn, r, w)
    ident_b = cstb[:, 0, :]
    sq = fw.sb("hsq", [128, 4, 128])
    ss = fw.sb("hss", [128, 4])
    mix = fw.sb("hmix", [128, 4, 128], BF16)
    for i in range(NT_L):
        o = oacc[:, i]
        V(lambda e: e.tensor_tensor(out=sq[:], in0=o, in1=o, op=ALU.mult), [oacc], [sq])
        V(lambda e: e.tensor_reduce(out=ss[:], in_=sq[:], axis=AX.X, op=ALU.add), [sq], [ss])
        A(lambda e: e.activation(out=ss[:], in_=ss[:], func=AF.Sqrt, scale=1.0 / 128, bias=EPS), [ss], [ss])
        V(lambda e: e.reciprocal(out=ss[:], in_=ss[:]), [ss], [ss])
        V(lambda e: e.tensor_tensor(out=sq[:], in0=o, in1=ss[:].unsqueeze(2).to_broadcast([128, 4, 128]), op=ALU.mult), [oacc, ss], [sq])
        V(lambda e: e.tensor_tensor(out=sq[:], in0=sq[:], in1=gnorm[:].unsqueeze(1).to_broadcast([128, 4, 128]), op=ALU.mult), [sq, gnorm], [sq])
        V(lambda e: e.tensor_tensor(out=mix[:], in0=sq[:], in1=gate[:, i, :].rearrange("p (h t) -> p h t", t=128), op=ALU.mult),
          [sq, gate], [mix])
        bk = i % 2
        for h in range(4):
            T(lambda e, h=h: e.transpose(out=pbb(bk)[:, h * 128:(h + 1) * 128], in_=mix[:, h, :], identity=ident_b), [mix, cstb], [PB[bk]])
        if not permute:
            A(lambda e: e.copy(out=mixT[:, chunk0:chunk0 + 4, i * 128:(i + 1) * 128],
                               in_=pbb(bk)[:, 0:512].rearrange("p (h t) -> p h t", t=128)), [PB[bk]], [mixT])
        else:
            for h in range(4):
                dst = mixT[:, chunk0 + h, :].rearrange("p (r c) -> p c r", c=64)[:, 4 * i:4 * i + 4, :]
                src = pbb(bk)[:, h * 128:(h + 1) * 128].rearrange("p (c r) -> p c r", r=32)
                if h % 2:
                    A(lambda e, dst=dst, src=src: e.copy(out=dst, in_=src), [PB[bk]], [mixT])
                else:
                    V(lambda e, dst=dst, src=src: e.tensor_copy(out=dst, in_=src), [PB[bk]], [mixT])


class StopBuild(Exception):
    pass


NSEQ = 2
TL = 2048
TC = 256
NT_L = 16
NT_C = 2
WCOLS = 3632


def build_program(stage=9):
    nc = bass.Bass("TRN2", target_bir_lowering=False)
    din = lambda n, s: nc.dram_tensor(n, list(s), F32, kind="ExternalInput").ap()
    x_d = din("x", [NSEQ, TL, 1024])
    ctx_d = din("ctx", [NSEQ, TC, 1024])
    cT_d = din("cT", [128, 8, 3])
    wada_d = din("w_ada", [1024, 6144])
    badaT_d = din("b_adaT", [128, 48])
    n1g_d = din("n1gT", [128, 8])
    n2g_d = din("n2gT", [128, 8])
    fing_d = din("final_g", [1, 1024])
    win_d = din("w_in", [1024, WCOLS])
    convT_d = din("convT", [128, 12, 5])
    alog_d = din("a_log", [1, 8])
    dtb_d = din("dt_bias", [1, 8])
    dng_d = din("dn_g", [1, 128])
    glg_d = din("gla_g", [1, 128])
    wa2_d = din("wa2", [2, 16, 256])
    ba_d = din("ba", [2, 1, 256])
    wout_d = din("w_out", [1024, 1024])
    wq_d = din("wq", [1024, 2048])
    keysT_d = din("keysT", [128, 16, 128])
    u_d = din("peer_u", [16384, 1024])
    v_d = din("peer_v", [16384, 1024])
    consts_d = din("consts", [128, 13, 128])
    out_d = nc.dram_tensor("out", [NSEQ, TL, 1024], F32, kind="ExternalOutput").ap()
    dbg_d = nc.dram_tensor("dbg", [128, 8, TL], F32, kind="ExternalOutput").ap() if stage < 2 else None
    PDBG = (stage == 3.5)
    X1OUT = (stage == 8)
    if stage == 8:
        stage = 9
    if PDBG:
        dps_d = nc.dram_tensor("dps", [128, 2048], F32, kind="ExternalOutput").ap()
        dptop_d = nc.dram_tensor("dptop", [128, 256], F32, kind="ExternalOutput").ap()
        dpcv_d = nc.dram_tensor("dpcv", [128, 128], F32, kind="ExternalOutput").ap()
        dpt4_d = nc.dram_tensor("dpt4", [128, 512], F32, kind="ExternalOutput").ap()
        dpt4t_d = nc.dram_tensor("dpt4t", [128, 512], F32, kind="ExternalOutput").ap()
        dpw_d = nc.dram_tensor("dpw", [128, 128, 8], F32, kind="ExternalOutput").ap()
        dppo_d = nc.dram_tensor("dppo", [128, 1024], F32, kind="ExternalOutput").ap()
        dpsr_d = nc.dram_tensor("dpsr", [128, 2, 2048], F32, kind="ExternalOutput").ap()
        dpab_d = nc.dram_tensor("dpab", [128, 2, 512], F32, kind="ExternalOutput").ap()
    modrow_d = nc.dram_tensor("modrow", [3, 6144], F32, kind="Internal").ap()
    x1_d = nc.dram_tensor("x1s", [NSEQ * TL, 1024], F32, kind=("ExternalOutput" if X1OUT else "Internal")).ap()
    ss_d = nc.dram_tensor("sscr", [2, 8, NSEQ * TL, 128], F32, kind="Internal").ap()
    ut_d = nc.dram_tensor("utscr", [128, 128, 8, 128], BF16, kind="Internal").ap()
    vb_d = nc.dram_tensor("vbscr", [128, 128, 1024], BF16, kind="Internal").ap()

    with contextlib.ExitStack() as top:
        fw = FW(nc, top)
        V = lambda fn, r, w: fw.op("dve", fn, r, w)
        A = lambda fn, r, w: fw.op("act", fn, r, w)
        G = lambda fn, r, w: fw.op("pool", fn, r, w)
        T = lambda fn, r, w: fw.op("pe", fn, r, w)

        PS = top.enter_context(nc.psum_tensor("ps", [128, 4096], F32))
        PB = [Buf("pb%d" % i, PS[:, i * 512:(i + 1) * 512]) for i in range(8)]

        def pbf(i, n=1):
            return PS[:, i * 512:(i + n) * 512]

        def pbb(i, n=1):
            return PS[:, i * 512:(i + n) * 512].bitcast(BF16)

        outb = fw.view("outb")
        cst = fw.sb("cst", [128, 13, 128])
        fw.dma("sp", cst[:], consts_d, cst, None)
        cstb = fw.sb("cstb", [128, 13, 128], BF16)
        V(lambda e: e.tensor_copy(out=cstb[:], in_=cst[:]), [cst], [cstb])
        ident_f = cst[:, 0, :]
        ident_b = cstb[:, 0, :]
        ones_b = cstb[:, 9, :]
        ones_f = cst[:, 9, :]

        modT = fw.sb("modT", [128, 48, 3])
        G1 = fw.sb("G1", [128, 8, 3])
        G2 = fw.sb("G2", [128, 8, 3])
        modrow = fw.view("modrow")
        x1buf = fw.view("x1buf")

        with fw.scope():
            cT = fw.sb("cT", [128, 8, 3])
            fw.dma("sp", cT[:], cT_d, cT, None)
            siluT = fw.sb("siluT", [128, 8, 3])
            A(lambda e: e.activation(out=siluT[:], in_=cT[:], func=AF.Silu), [cT], [siluT])
            badaT = fw.sb("badaT", [128, 48])
            fw.dma("sp", badaT[:], badaT_d, badaT, None)
            n1g = fw.sb("n1g", [128, 8])
            n2g = fw.sb("n2g", [128, 8])
            fw.dma("sp", n1g[:], n1g_d, n1g, None)
            fw.dma("sp", n2g[:], n2g_d, n2g, None)
            slabs = [fw.sb("wada%d" % i, [128, 8, 1024]) for i in range(2)]
            wv = wada_d.rearrange("(k p) c -> p k c", p=128)
            pm = pbf(0)[:, 0:144].rearrange("p (j b) -> p j b", b=3)
            for v in range(6):
                sl = slabs[v % 2]
                fw.dma("sp" if v % 2 == 0 else "act", sl[:], wv[:, :, v * 1024:(v + 1) * 1024], sl, None)
                for ch in range(8):
                    j = v * 8 + ch
                    for k in range(8):
                        T(lambda e, sl=sl, ch=ch, k=k, j=j: e.matmul(
                            out=pm[:, j, :], lhsT=sl[:, k, ch * 128:(ch + 1) * 128], rhs=siluT[:, k, :],
                            start=(k == 0), stop=(k == 7)), [sl, siluT], [PB[0]])
            V(lambda e: e.tensor_tensor(out=modT[:], in0=pm[:, 0:48, :],
                                        in1=badaT[:].unsqueeze(2).to_broadcast([128, 48, 3]), op=ALU.add),
              [PB[0], badaT], [modT])
            for (Gt, ng, o) in ((G1, n1g, 8), (G2, n2g, 32)):
                V(lambda e, Gt=Gt, o=o: e.tensor_scalar(out=Gt[:], in0=modT[:, o:o + 8, :], scalar1=1.0, scalar2=None,
                                                        op0=ALU.add), [modT], [Gt])
                V(lambda e, Gt=Gt, ng=ng: e.tensor_tensor(out=Gt[:], in0=Gt[:],
                                                          in1=ng[:].unsqueeze(2).to_broadcast([128, 8, 3]), op=ALU.mult),
                  [Gt, ng], [Gt])
            for b in range(3):
                fw.dma("pool", modrow_d[b].rearrange("(j p) -> p j", p=128), modT[:, :, b], modrow, modT,
                       allow_slow_non_contiguous=True)

        def make_hT(hT, col0, src_ap, ntiles, Gt, SHo, mcol, xt_bufs, wk, perm=False, srcbuf=None):
            for i in range(ntiles):
                xt = xt_bufs[i % 2]
                fw.dma("sp", xt[:], src_ap[i * 128:(i + 1) * 128, :], xt, srcbuf)
                junk, ssq, xn = wk
                A(lambda e, xt=xt: e.activation(out=junk[:], in_=xt[:], func=AF.Square, accum_out=ssq[:]),
                  [xt], [junk, ssq])
                A(lambda e: e.activation(out=ssq[:], in_=ssq[:], func=AF.Sqrt, scale=1.0 / 1024, bias=EPS), [ssq], [ssq])
                V(lambda e: e.reciprocal(out=ssq[:], in_=ssq[:]), [ssq], [ssq])
                V(lambda e, xt=xt: e.tensor_scalar(out=xn[:], in0=xt[:], scalar1=ssq[:, 0:1], scalar2=None, op0=ALU.mult),
                  [xt, ssq], [xn])
                pt = pbb(7).rearrange("p (k t) -> p k t", t=128)
                for k in range(8):
                    T(lambda e, k=k: e.transpose(out=pt[:, k, :], in_=xn[:, k * 128:(k + 1) * 128], identity=ident_b),
                      [xn, cstb], [PB[7]])
                if not perm:
                    dst = hT[:, :, col0 + i * 128: col0 + (i + 1) * 128]
                    src = pt[:, 0:8, :]
                    g_b = Gt[:, :, mcol:mcol + 1].to_broadcast([128, 8, 128])
                    s_b = modT[:, SHo:SHo + 8, mcol:mcol + 1].to_broadcast([128, 8, 128])
                else:
                    dst = hT[:, :, col0:col0 + TL].rearrange("p k (c r) -> p k r c", r=32)[:, :, 2 * i:2 * i + 2, :]
                    src = pt[:, 0:8, :].rearrange("p k (r c) -> p k r c", c=64)
                    g_b = Gt[:, :, mcol:mcol + 1].unsqueeze(3).to_broadcast([128, 8, 2, 64])
                    s_b = modT[:, SHo:SHo + 8, mcol:mcol + 1].unsqueeze(3).to_broadcast([128, 8, 2, 64])
                V(lambda e, dst=dst, src=src, g_b=g_b: e.tensor_tensor(out=dst, in0=src, in1=g_b, op=ALU.mult),
                  [PB[7], Gt], [hT])
                V(lambda e, dst=dst, s_b=s_b: e.tensor_tensor(out=dst, in0=dst, in1=s_b, op=ALU.add), [hT, modT], [hT])

        def load_w_bf16(dst, src_view, ncols, stage_bufs, step=512, d0=0):
            n = 0
            for c0 in range(0, ncols, step):
                c1 = min(ncols, c0 + step)
                sg = stage_bufs[n % 2]
                fw.dma("act" if n % 2 else "sp", sg[:, :, 0:c1 - c0], src_view[:, :, c0:c1], sg, None)
                if n % 2:
                    A(lambda e, sg=sg, c0=c0, c1=c1: e.copy(out=dst[:, :, d0 + c0:d0 + c1], in_=sg[:, :, 0:c1 - c0]), [sg], [dst])
                else:
                    V(lambda e, sg=sg, c0=c0, c1=c1: e.tensor_copy(out=dst[:, :, d0 + c0:d0 + c1], in_=sg[:, :, 0:c1 - c0]), [sg], [dst])
                n += 1

        win_v = win_d.rearrange("(k p) c -> p k c", p=128)

        if 0.6 < stage < 0.7:
            fw.dn_limit = int(round((stage - 0.6) * 1000))

        def ck(x):
            if stage <= x:
                fw.dead = True

        for s in range(NSEQ if stage >= 0.1 else 0):
          try:
            with fw.scope():
                mixT = fw.sb("mixT", [128, 8, TL], BF16)
                with fw.scope():
                    Pq = fw.sb("Pq", [128, 12, TC + TL + 8], BF16)
                    CO = (2, 262)
                    Zg = fw.sb("Zg", [128, NT_L, 512], BF16)
                    SM = fw.sb("SM", [128, 18, 16])
                    with fw.scope():
                        hT = fw.sb("hT", [128, 8, TC + TL], BF16)
                        xts = [fw.sb("xt%d" % i, [128, 1024]) for i in range(2)]
                        wk = (fw.sb("junk", [128, 1024], BF16), fw.sb("ssq", [128, 1]), fw.sb("xn", [128, 1024], BF16))
                        make_hT(hT, 0, ctx_d[s], NT_C, G1, 0, 2, xts, wk)
                        make_hT(hT, TC, x_d[s], NT_L, G1, 0, s, xts, wk)
                        ck(0.2)
                        wdn = fw.sb("wdn", [128, 8, 2064], BF16)
                        stg = [fw.sb("stg%d" % i, [128, 8, 128]) for i in range(2)]
                        load_w_bf16(wdn, win_v[:, :, 0:2048], 2048, stg, 128)
                        load_w_bf16(wdn, win_v[:, :, 3584:3600], 16, stg, 128, 2048)
                        V(lambda e: e.memset(Pq[:], 0.0), [], [Pq])
                        ck(0.3)
                        nb = 0
                        for (t0, tn, seg) in [(0, 256, 0)] + [(TC + b * 512, 512, 1) for b in range(4)]:
                            for c in range(12):
                                bk = nb % 2
                                nb += 1
                                for k in range(8):
                                    T(lambda e, bk=bk, c=c, k=k, t0=t0, tn=tn: e.matmul(
                                        out=pbf(bk)[:, 0:tn], lhsT=wdn[:, k, c * 128:(c + 1) * 128], rhs=hT[:, k, t0:t0 + tn],
                                        start=(k == 0), stop=(k == 7)), [wdn, hT], [PB[bk]])
                                d0 = CO[seg] + (t0 - (TC if seg else 0))
                                if bk:
                                    A(lambda e, bk=bk, c=c, d0=d0, tn=tn: e.copy(out=Pq[:, c, d0:d0 + tn], in_=pbf(bk)[:, 0:tn]),
                                      [PB[bk]], [Pq])
                                else:
                                    V(lambda e, bk=bk, c=c, d0=d0, tn=tn: e.tensor_copy(out=Pq[:, c, d0:d0 + tn], in_=pbf(bk)[:, 0:tn]),
                                      [PB[bk]], [Pq])
                        for i in range(18):
                            t0 = i * 128
                            if i >= 2:
                                for k in range(8):
                                    T(lambda e, k=k, t0=t0: e.matmul(out=pbf(2), lhsT=hT[:, k, t0:t0 + 128], rhs=wdn[:, k, 1536:2048],
                                                                     start=(k == 0), stop=(k == 7)), [wdn, hT], [PB[2]])
                                A(lambda e, i=i: e.activation(out=Zg[:, i - 2, :], in_=pbf(2), func=AF.Silu), [PB[2]], [Zg])
                            for k in range(8):
                                T(lambda e, k=k, t0=t0: e.matmul(out=pbf(3)[:, 0:16], lhsT=hT[:, k, t0:t0 + 128], rhs=wdn[:, k, 2048:2064],
                                                                 start=(k == 0), stop=(k == 7)), [wdn, hT], [PB[3]])
                            V(lambda e, i=i: e.tensor_copy(out=SM[:, i, :], in_=pbf(3)[:, 0:16]), [PB[3]], [SM])
                    ck(0.4)
                    BET = fw.sb("BET", [128, 18, 8])
                    NBET = fw.sb("NBET", [128, 18, 8])
                    GG = fw.sb("GG", [128, 18, 8])
                    alog = fw.sb("alog", [128, 8])
                    dtb = fw.sb("dtb", [128, 8])
                    fw.dma("sp", alog[:], alog_d.partition_broadcast(128), alog, None)
                    fw.dma("sp", dtb[:], dtb_d.partition_broadcast(128), dtb, None)
                    A(lambda e: e.activation(out=BET[:], in_=SM[:, :, 0:8], func=AF.Sigmoid), [SM], [BET])
                    V(lambda e: e.tensor_scalar(out=NBET[:], in0=BET[:], scalar1=-1.0, scalar2=None, op0=ALU.mult), [BET], [NBET])
                    V(lambda e: e.tensor_tensor(out=GG[:], in0=SM[:, :, 8:16], in1=dtb[:].unsqueeze(1).to_broadcast([128, 18, 8]),
                                                op=ALU.add), [SM, dtb], [GG])
                    A(lambda e: e.activation(out=GG[:], in_=GG[:], func=AF.Exp), [GG], [GG])
                    A(lambda e: e.activation(out=GG[:], in_=GG[:], func=AF.Ln, bias=1.0), [GG], [GG])
                    A(lambda e: e.activation(out=alog[:], in_=alog[:], func=AF.Exp), [alog], [alog])
                    V(lambda e: e.tensor_scalar(out=alog[:], in0=alog[:], scalar1=-1.0, scalar2=None, op0=ALU.mult), [alog], [alog])
                    V(lambda e: e.tensor_tensor(out=GG[:], in0=GG[:], in1=alog[:].unsqueeze(1).to_broadcast([128, 18, 8]),
                                                op=ALU.mult), [GG, alog], [GG])
                    ck(0.5)
                    cw = fw.sb("cw", [128, 12, 5])
                    fw.dma("sp", cw[:], convT_d, cw, None)
                    with fw.scope():
                        acc = fw.sb("acc", [128, TL])
                        sq = fw.sb("sq", [128, 512], BF16)
                        rin = fw.sb("rin", [128, 512])
                        for c in range(12):
                            for seg, n in ((0, TC), (1, TL)):
                                o = CO[seg]
                                V(lambda e, c=c, o=o, n=n: e.tensor_scalar(out=acc[:, 0:n], in0=Pq[:, c, o - 2:o - 2 + n],
                                                                           scalar1=cw[:, c, 0:1], scalar2=None, op0=ALU.mult),
                                  [Pq, cw], [acc])
                                for j in range(1, 5):
                                    V(lambda e, c=c, o=o, n=n, j=j: e.scalar_tensor_tensor(
                                        out=acc[:, 0:n], in0=Pq[:, c, o - 2 + j:o - 2 + j + n], scalar=cw[:, c, j:j + 1],
                                        in1=acc[:, 0:n], op0=ALU.mult, op1=ALU.add), [Pq, cw, acc], [acc])
                                if c >= 8:
                                    A(lambda e, c=c, o=o, n=n: e.activation(out=Pq[:, c, o:o + n], in_=acc[:, 0:n], func=AF.Silu),
                                      [acc], [Pq])
                                    continue
                                A(lambda e, n=n: e.activation(out=acc[:, 0:n], in_=acc[:, 0:n], func=AF.Silu), [acc], [acc])
                                for b0 in range(0, n, 512):
                                    bn = min(512, n - b0)
                                    V(lambda e, b0=b0, bn=bn: e.tensor_tensor(out=sq[:, 0:bn], in0=acc[:, b0:b0 + bn],
                                                                              in1=acc[:, b0:b0 + bn], op=ALU.mult), [acc], [sq])
                                    T(lambda e, bn=bn: e.matmul(out=pbf(0)[:, 0:bn], lhsT=ones_b, rhs=sq[:, 0:bn], start=True, stop=True),
                                      [sq, cstb], [PB[0]])
                                    A(lambda e, bn=bn: e.activation(out=rin[:, 0:bn], in_=pbf(0)[:, 0:bn], func=AF.Sqrt, bias=EPS),
                                      [PB[0]], [rin])
                                    V(lambda e, bn=bn: e.reciprocal(out=rin[:, 0:bn], in_=rin[:, 0:bn]), [rin], [rin])
                                    sc = (128.0 ** -0.5) if c < 4 else 1.0
                                    V(lambda e, c=c, o=o, b0=b0, bn=bn, sc=sc: e.scalar_tensor_tensor(
                                        out=Pq[:, c, o + b0:o + b0 + bn], in0=acc[:, b0:b0 + bn], scalar=sc, in1=rin[:, 0:bn],
                                        op0=ALU.mult, op1=ALU.mult), [acc, rin], [Pq])
                    ck(0.6)
                    oacc = fw.sb("oacc", [128, NT_L, 4, 128])
                    with fw.scope():
                        dn_scan(fw, nc, PS, PB, pbf, pbb, cst, cstb, Pq, CO, BET, NBET, GG, oacc)
                    ck(0.7)
                    dng = fw.sb("dng", [128, 128])
                    fw.dma("sp", dng[:], dng_d.partition_broadcast(128), dng, None)
                    with fw.scope():
                        head_norm_gate(fw, PB, pbb, cstb, oacc, Zg, dng, mixT, 0, False)

                with fw.scope():
                    gqk = fw.sb("gqk", [64, 8, TC + TL], BF16)
                    gv = fw.sb("gv", [128, 18, 512], BF16)
                    Rg = fw.sb("Rg", [128, NT_L, 512], BF16)
                    LRT = fw.sb("LRT", [16, 2, TC + TL], BF16)
                    with fw.scope():
                        hT = fw.sb("hTg", [128, 8, TC + TL], BF16)
                        xts = [fw.sb("xtg%d" % i, [128, 1024]) for i in range(2)]
                        wk = (fw.sb("junkg", [128, 1024], BF16), fw.sb("ssqg", [128, 1]), fw.sb("xng", [128, 1024], BF16))
                        make_hT(hT, 0, ctx_d[s], NT_C, G1, 0, 2, xts, wk)
                        make_hT(hT, TC, x_d[s], NT_L, G1, 0, s, xts, wk, perm=True)
                        wgl = fw.sb("wgl", [128, 8, 1568], BF16)
                        stg = [fw.sb("stgg%d" % i, [128, 8, 128]) for i in range(2)]
                        load_w_bf16(wgl, win_v[:, :, 2048:3584], 1536, stg, 128)
                        load_w_bf16(wgl, win_v[:, :, 3600:3632], 32, stg, 128, 1536)
                        nb = 0
                        for (t0, tn) in [(0, 256)] + [(TC + b * 512, 512) for b in range(4)]:
                            for g in range(10):
                                bk = nb % 2
                                nb += 1
                                if g < 8:
                                    cs, m = slice(g * 64, (g + 1) * 64), 64
                                else:
                                    cs, m = slice(1536 + (g - 8) * 16, 1536 + (g - 7) * 16), 16
                                for k in range(8):
                                    T(lambda e, bk=bk, k=k, cs=cs, m=m, t0=t0, tn=tn: e.matmul(
                                        out=pbf(bk)[0:m, 0:tn], lhsT=wgl[:, k, cs], rhs=hT[:, k, t0:t0 + tn],
                                        start=(k == 0), stop=(k == 7)), [wgl, hT], [PB[bk]])
                                if g < 4:
                                    A(lambda e, bk=bk, g=g, t0=t0, tn=tn: e.mul(out=gqk[:, g, t0:t0 + tn], in_=pbf(bk)[0:64, 0:tn], mul=0.125),
                                      [PB[bk]], [gqk])
                                elif g < 8:
                                    V(lambda e, bk=bk, g=g, t0=t0, tn=tn: e.tensor_copy(out=gqk[:, g, t0:t0 + tn], in_=pbf(bk)[0:64, 0:tn]),
                                      [PB[bk]], [gqk])
                                else:
                                    V(lambda e, bk=bk, g=g, t0=t0, tn=tn: e.tensor_copy(out=LRT[:, g - 8, t0:t0 + tn], in_=pbf(bk)[0:16, 0:tn]),
                                      [PB[bk]], [LRT])
                        for i in range(18):
                            t0 = i * 128
                            for k in range(8):
                                T(lambda e, k=k, t0=t0: e.matmul(out=pbf(2), lhsT=hT[:, k, t0:t0 + 128], rhs=wgl[:, k, 512:1024],
                                                                 start=(k == 0), stop=(k == 7)), [wgl, hT], [PB[2]])
                            V(lambda e, i=i: e.tensor_copy(out=gv[:, i, :], in_=pbf(2)), [PB[2]], [gv])
                            if i >= 2:
                                for k in range(8):
                                    T(lambda e, k=k, t0=t0: e.matmul(out=pbf(3), lhsT=hT[:, k, t0:t0 + 128], rhs=wgl[:, k, 1024:1536],
                                                                     start=(k == 0), stop=(k == 7)), [wgl, hT], [PB[3]])
                                A(lambda e, i=i: e.activation(out=Rg[:, i - 2, :], in_=pbf(3), func=AF.Silu), [PB[3]], [Rg])
                    ck(1.2)
                    oacc = fw.sb("oaccg", [128, NT_L, 4, 128])
                    with fw.scope():
                        gla_scan(fw, PB, pbf, pbb, cst, cstb, gqk, gv, LRT, wa2_d, ba_d, oacc)
                    ck(1.3)
                    glg = fw.sb("glg", [128, 128])
                    fw.dma("sp", glg[:], glg_d.partition_broadcast(128), glg, None)
                    with fw.scope():
                        head_norm_gate(fw, PB, pbb, cstb, oacc, Rg, glg, mixT, 4, True)
                ck(1.4)
                with fw.scope():
                    wo = fw.sb("wo", [128, 8, 1024], BF16)
                    stg = [fw.sb("stgo%d" % i, [128, 8, 128]) for i in range(2)]
                    load_w_bf16(wo, wout_d.rearrange("(k p) c -> p k c", p=128), 1024, stg, 128)
                    g1B = fw.sb("g1B", [128, 1024])
                    fw.dma("sp", g1B[:], modrow_d[s:s + 1, 2048:3072].partition_broadcast(128), g1B, modrow)
                    xts = [fw.sb("xto%d" % i, [128, 1024]) for i in range(2)]
                    x1ts = [fw.sb("x1t%d" % i, [128, 1024]) for i in range(2)]
                    for i in range(NT_L):
                        xt = xts[i % 2]
                        x1t = x1ts[i % 2]
                        fw.dma("sp", xt[:], x_d[s][i * 128:(i + 1) * 128, :], xt, None)
                        for hf in range(2):
                            bk = 2 * (i % 2) + hf
                            for k in range(8):
                                T(lambda e, k=k, i=i, hf=hf, bk=bk: e.matmul(out=pbf(bk), lhsT=mixT[:, k, i * 128:(i + 1) * 128],
                                                                             rhs=wo[:, k, hf * 512:(hf + 1) * 512],
                                                                             start=(k == 0), stop=(k == 7)), [mixT, wo], [PB[bk]])
                            hs = slice(hf * 512, (hf + 1) * 512)
                            V(lambda e, bk=bk, hs=hs, x1t=x1t: e.tensor_tensor(out=x1t[:, hs], in0=pbf(bk), in1=g1B[:, hs], op=ALU.mult),
                              [PB[bk], g1B], [x1t])
                            V(lambda e, hs=hs, x1t=x1t, xt=xt: e.tensor_tensor(out=x1t[:, hs], in0=x1t[:, hs], in1=xt[:, hs], op=ALU.add),
                              [x1t, xt], [x1t])
                        fw.dma("pool", x1_d[s * TL + i * 128:s * TL + (i + 1) * 128, :], x1t[:], x1buf, x1t)
                ck(1.45)
                if stage < 2:
                    if s == 0:
                        with fw.scope():
                            dts = [fw.sb("dtmp%d" % i, [128, TL]) for i in range(2)]
                            for ch in range(8):
                                dt_ = dts[ch % 2]
                                V(lambda e, ch=ch, dt_=dt_: e.tensor_copy(out=dt_[:], in_=mixT[:, ch, :]), [mixT], [dt_])
                                fw.dma("sp", dbg_d[:, ch, :], dt_[:], outb, dt_)
                        fw.dead = True
                    continue
          except StopBuild:
            break
        fw.dead = False
        if stage >= 3:
            utscr = fw.view("utscr")
            vbscr = fw.view("vbscr")
            with fw.scope():
                uview = u_d.rearrange("(i j) d -> j i d", j=128)
                vview = v_d.rearrange("(i j) d -> j i d", j=128)
                ubs = [fw.sb("ub%d" % i, [128, 1024]) for i in range(2)]
                vbs = [fw.sb("vb%d" % i, [128, 1024]) for i in range(2)]
                ubb = [fw.sb("ubb%d" % i, [128, 1024], BF16) for i in range(2)]
                vbb = [fw.sb("vbb%d" % i, [128, 1024], BF16) for i in range(2)]
                utb = [fw.sb("utb%d" % i, [128, 8, 128], BF16) for i in range(2)]
                for j in range(128):
                    q = j % 2
                    fw.dma("sp", ubs[q][:], uview[j], ubs[q], None)
                    fw.dma("act", vbs[q][:], vview[j], vbs[q], None)
                    A(lambda e, q=q: e.copy(out=ubb[q][:], in_=ubs[q][:]), [ubs[q]], [ubb[q]])
                    V(lambda e, q=q: e.tensor_copy(out=vbb[q][:], in_=vbs[q][:]), [vbs[q]], [vbb[q]])
                    for k in range(8):
                        T(lambda e, q=q, k=k: e.transpose(out=pbb(q)[:, k * 128:(k + 1) * 128], in_=ubb[q][:, k * 128:(k + 1) * 128],
                                                          identity=ident_b), [ubb[q], cstb], [PB[q]])
                    V(lambda e, q=q: e.tensor_copy(out=utb[q][:].rearrange("p k i -> p (k i)"), in_=pbb(q)), [PB[q]], [utb[q]])
                    fw.dma("pool", ut_d[j], utb[q][:], utscr, utb[q])
                    fw.dma("pool", vb_d[j], vbb[q][:], vbscr, vbb[q])
            with fw.scope():
                wqb = fw.sb("wqb", [128, 8, 2048], BF16)
                keysb = fw.sb("keysb", [128, 16, 128], BF16)
                with fw.scope():
                    stg = [fw.sb("stgp%d" % i, [128, 8, 128]) for i in range(2)]
                    load_w_bf16(wqb, wq_d.rearrange("(k p) c -> p k c", p=128), 2048, stg, 128)
                    keysf = fw.sb("keysf", [128, 16, 128])
                    fw.dma("sp", keysf[:], keysT_d, keysf, None)
                    V(lambda e: e.tensor_copy(out=keysb[:], in_=keysf[:]), [keysf], [keysb])
                g2B = fw.sb("g2B", [128, 1024])
                fingB = fw.sb("fingB", [128, 1024])
                fw.dma("sp", fingB[:], fing_d.partition_broadcast(128), fingB, None)
                h2T = fw.sb("h2T", [128, 8, 256], BF16)
                xts = [fw.sb("xtp%d" % i, [128, 1024]) for i in range(2)]
                xnp = fw.sb("xnp", [128, 1024], BF16)
                wk = (xnp, fw.sb("ssqp", [128, 1]), xnp)
                qT = fw.sb("qT", [128, 16, 256], BF16)
                ssb = fw.sb("ssb", [128, 16, 128])
                top = fw.sb("top", [128, 16, 16])
                idxu = fw.sb("idxu", [128, 8, 16], mybir.dt.uint32)
                wkk = fw.sb("wkk", [128, 128])
                cand = fw.sb("cand", [128, 8, 256])
                wk2 = fw.sb("wk2", [128, 256])
                cv = fw.sb("cv", [128, 8, 16])
                T4s = fw.sb("T4s", [128, 3, 128])
                T4 = fw.sb("T4", [128, 2, 3, 128])
                zz = fw.sb("zz", [128, 8, 16])
                rZ = fw.sb("rZ", [128, 8])
                NSB = 8
                Srep = fw.sb("Srep", [128, NSB, 128])
                A0 = fw.sb("A0", [128, NSB, 128], BF16)
                Ab = fw.sb("Ab", [128, NSB, 128], BF16)
                cmpb = fw.sb("cmpb", [128, NSB, 128], BF16)
                Eb = fw.sb("Eb", [128, NSB, 128], BF16)
                Bb = fw.sb("Bb", [128, NSB, 128], BF16)
                Wsb = fw.sb("Wsb", [128, 128, 256], BF16)
                utl = [fw.sb("utl%d" % i, [128, 2, 8, 128], BF16) for i in range(3)]
                vtl = [fw.sb("vtl%d" % i, [128, 2, 1024], BF16) for i in range(3)]
                gef = [fw.sb("gef%d" % i, [128, 256], BF16) for i in range(2)]
                gw = [fw.sb("gw%d" % i, [128, 256], BF16) for i in range(2)]
                x2ap = cand[:].rearrange("p h c -> p (h c)")[:, 0:1024]
                x2 = cand
                ssbuf = [fw.view("ssbuf%d" % i) for i in range(2)]
                tv = top[:].rearrange("p (h q) k -> p h q k", q=2)
                bk3 = lambda ap: ap.unsqueeze(2).to_broadcast([128, NSB, 128])
                iota_b = cstb[:, 12, :].unsqueeze(1).to_broadcast([128, NSB, 128])
                t4 = lambda q: T4s[:, q, :].rearrange("p (h k) -> p h k", k=16)
                b16 = lambda ap: ap.to_broadcast([128, 8, 16])
                for blk in range(NSEQ * NT_L // 2):
                    sq = (blk * 2) // NT_L
                    if blk % (NT_L // 2) == 0:
                        fw.dma("sp", g2B[:], modrow_d[sq:sq + 1, 5120:6144].partition_broadcast(128), g2B, modrow)
                    make_hT(h2T, 0, x1_d[blk * 256:(blk + 1) * 256, :], 2, G2, 24, sq, xts, wk, srcbuf=x1buf)
                    for hp in range(16):
                        bk = hp % 2
                        for k in range(8):
                            T(lambda e, bk=bk, hp=hp, k=k: e.matmul(out=pbf(bk)[:, 0:256], lhsT=wqb[:, k, hp * 128:(hp + 1) * 128],
                                                                    rhs=h2T[:, k, :], start=(k == 0), stop=(k == 7)), [wqb, h2T], [PB[bk]])
                        if bk:
                            A(lambda e, bk=bk, hp=hp: e.copy(out=qT[:, hp, :], in_=pbf(bk)[:, 0:256]), [PB[bk]], [qT])
                        else:
                            V(lambda e, bk=bk, hp=hp: e.tensor_copy(out=qT[:, hp, :], in_=pbf(bk)[:, 0:256]), [PB[bk]], [qT])
                    sc = ssbuf[blk % 2]
                    for tl in range(2):
                        gt = blk * 2 + tl
                        for g in range(4):
                            bk = 2 + g % 2
                            for q in range(4):
                                hp = g * 4 + q
                                T(lambda e, bk=bk, hp=hp, q=q, tl=tl: e.matmul(out=pbf(bk)[:, q * 128:(q + 1) * 128],
                                                                               lhsT=qT[:, hp, tl * 128:(tl + 1) * 128],
                                                                               rhs=keysb[:, hp, :], start=True, stop=True), [qT, keysb], [PB[bk]])
                            A(lambda e, bk=bk, g=g: e.copy(out=ssb[:, g * 4:(g + 1) * 4, :].rearrange("p a i -> p (a i)"), in_=pbf(bk)),
                              [PB[bk]], [ssb])
                        fw.dma("pool", ss_d[1, :, gt * 128:(gt + 1) * 128, :].rearrange("h t i -> t h i"),
                               ssb[:].rearrange("p (h q) i -> p h q i", q=2)[:, :, 1, :], sc, ssb)
                        for hp in range(16):
                            V(lambda e, hp=hp: e.max(out=top[:, hp, 0:8], in_=ssb[:, hp, :]), [ssb], [top])
                            V(lambda e, hp=hp: e.match_replace(out=wkk[:], in_to_replace=top[:, hp, 0:8], in_values=ssb[:, hp, :],
                                                               imm_value=-1e30), [ssb, top], [wkk])
                            V(lambda e, hp=hp: e.max(out=top[:, hp, 8:16], in_=wkk[:]), [wkk], [top])
                            if hp % 2 == 0:
                                for o8 in (0, 8):
                                    V(lambda e, hp=hp, o8=o8: e.max_index(out=idxu[:, hp // 2, o8:o8 + 8], in_max=top[:, hp, o8:o8 + 8],
                                                                          in_values=ssb[:, hp, :]), [ssb, top], [idxu])
                        V(lambda e: e.tensor_tensor(out=cand[:].rearrange("p h (a b) -> p h a b", b=16),
                                                    in0=tv[:, :, 0, :].unsqueeze(3).to_broadcast([128, 8, 16, 16]),
                                                    in1=tv[:, :, 1, :].unsqueeze(2).to_broadcast([128, 8, 16, 16]), op=ALU.add), [top], [cand])
                        for h in range(8):
                            V(lambda e, h=h: e.max(out=cv[:, h, 0:8], in_=cand[:, h, :]), [cand], [cv])
                            V(lambda e, h=h: e.match_replace(out=wk2[:], in_to_replace=cv[:, h, 0:8], in_values=cand[:, h, :],
                                                             imm_value=-1e30), [cand, cv], [wk2])
                            V(lambda e, h=h: e.max(out=cv[:, h, 8:16], in_=wk2[:]), [wk2], [cv])
                        V(lambda e: e.tensor_copy(out=t4(0), in_=idxu[:]), [idxu], [T4s])
                        V(lambda e: e.tensor_scalar(out=t4(1), in0=tv[:, :, 0, :], scalar1=-1.0, scalar2=-1e-5, op0=ALU.mult, op1=ALU.add),
                          [top], [T4s])
                        V(lambda e: e.tensor_tensor(out=t4(1), in0=t4(1), in1=b16(cv[:, :, 15:16]), op=ALU.add), [T4s, cv], [T4s])
                        V(lambda e: e.tensor_tensor(out=zz[:], in0=cv[:], in1=b16(cv[:, :, 0:1]), op=ALU.subtract), [cv], [zz])
                        A(lambda e: e.activation(out=zz[:], in_=zz[:], func=AF.Exp), [zz], [zz])
                        V(lambda e: e.tensor_reduce(out=rZ[:], in_=zz[:], axis=AX.X, op=ALU.add), [zz], [rZ])
                        V(lambda e: e.reciprocal(out=rZ[:], in_=rZ[:]), [rZ], [rZ])
                        V(lambda e: e.tensor_tensor(out=t4(2), in0=tv[:, :, 0, :], in1=b16(cv[:, :, 0:1]), op=ALU.subtract), [top, cv], [T4s])
                        A(lambda e: e.activation(out=t4(2), in_=t4(2), func=AF.Exp), [T4s], [T4s])
                        V(lambda e: e.tensor_tensor(out=t4(2), in0=t4(2), in1=b16(rZ[:].unsqueeze(2)), op=ALU.mult), [T4s, rZ], [T4s])
                        for q in range(3):
                            T(lambda e, q=q: e.transpose(out=pbf(2)[:, q * 128:(q + 1) * 128], in_=T4s[:, q, :], identity=ident_f),
                              [T4s, cst], [PB[2]])
                        A(lambda e, tl=tl: e.copy(out=T4[:, tl].rearrange("p q t -> p (q t)"), in_=pbf(2)[:, 0:384]), [PB[2]], [T4])
                    for sbk in range(256 // NSB):
                        tl = (sbk * NSB) // 128
                        tin = (sbk * NSB) % 128
                        ta = blk * 256 + sbk * NSB
                        tsl = slice(tin, tin + NSB)
                        for h in range(8):
                            fw.dma("sp" if h % 2 else "act", Srep[h * 16:(h + 1) * 16, :, :].rearrange("q t i -> q (t i)"),
                                   ss_d[1, h:h + 1, ta:ta + NSB, :].rearrange("o t i -> o (t i)").partition_broadcast(16),
                                   Srep, sc)
                        V(lambda e, tsl=tsl, tl=tl: e.tensor_tensor(out=A0[:], in0=iota_b, in1=bk3(T4[:, tl, 0, tsl]), op=ALU.is_equal),
                          [cstb, T4], [A0])
                        V(lambda e, tsl=tsl, tl=tl: e.tensor_tensor(out=Ab[:], in0=A0[:], in1=bk3(T4[:, tl, 2, tsl]), op=ALU.mult), [A0, T4], [Ab])
                        V(lambda e, tsl=tsl, tl=tl: e.tensor_tensor(out=cmpb[:], in0=Srep[:], in1=bk3(T4[:, tl, 1, tsl]), op=ALU.is_ge),
                          [Srep, T4], [cmpb])
                        A(lambda e: e.activation(out=Eb[:], in_=Srep[:], func=AF.Exp), [Srep], [Eb])
                        V(lambda e: e.tensor_tensor(out=Bb[:], in0=cmpb[:], in1=Eb[:], op=ALU.mult), [cmpb, Eb], [Bb])
                        for t in range(NSB):
                            bk = 2 + (t // 4) % 2
                            T(lambda e, bk=bk, t=t: e.matmul(out=pbf(bk)[:, (t % 4) * 128:(t % 4 + 1) * 128], lhsT=Ab[:, t, :], rhs=Bb[:, t, :],
                                                             start=True, stop=True), [Ab, Bb], [PB[bk]])
                            if t % 4 == 3:
                                t0 = sbk * NSB + t - 3
                                dst = Wsb[:, :, t0:t0 + 4].rearrange("p j t -> p t j")
                                src = pbf(bk).rearrange("p (t j) -> p t j", j=128)
                                if (t // 4) % 2:
                                    A(lambda e, dst=dst, src=src: e.copy(out=dst, in_=src), [PB[bk]], [Wsb])
                                else:
                                    V(lambda e, dst=dst, src=src: e.tensor_copy(out=dst, in_=src), [PB[bk]], [Wsb])
                    for jj in range(64):
                        ut2, vt2 = utl[jj % 3], vtl[jj % 3]
                        fw.dma("sp", ut2[:], ut_d[2 * jj:2 * jj + 2].rearrange("j p k i -> p j k i"), ut2, utscr)
                        fw.dma("act", vt2[:], vb_d[2 * jj:2 * jj + 2].rearrange("j p d -> p j d"), vt2, vbscr)
                        for jl in range(2):
                            j = 2 * jj + jl
                            bk = j % 2
                            for k in range(8):
                                T(lambda e, bk=bk, k=k, ut2=ut2, jl=jl: e.matmul(out=pbf(bk)[:, 0:256], lhsT=ut2[:, jl, k, :], rhs=h2T[:, k, :],
                                                                                 start=(k == 0), stop=(k == 7)), [ut2, h2T], [PB[bk]])
                            ge_, gw_ = gef[j % 2], gw[j % 2]
                            A(lambda e, bk=bk, ge_=ge_: e.activation(out=ge_[:], in_=pbf(bk)[:, 0:256], func=AF.Gelu), [PB[bk]], [ge_])
                            V(lambda e, ge_=ge_, gw_=gw_, j=j: e.tensor_tensor(out=gw_[:], in0=ge_[:], in1=Wsb[:, j, :], op=ALU.mult),
                              [ge_, Wsb], [gw_])
                            for tl in range(2):
                                for hf in range(2):
                                    ob = 4 + tl * 2 + hf
                                    T(lambda e, hf=hf, tl=tl, ob=ob, gw_=gw_, vt2=vt2, jl=jl, j=j: e.matmul(
                                        out=pbf(ob), lhsT=gw_[:, tl * 128:(tl + 1) * 128], rhs=vt2[:, jl, hf * 512:(hf + 1) * 512],
                                        start=(j == 0), stop=(j == 127)), [gw_, vt2], [PB[ob]])
                    for tl in range(2):
                        xt = xts[tl]
                        for hf in range(2):
                            hs = slice(hf * 512, (hf + 1) * 512)
                            ob = 4 + tl * 2 + hf
                            V(lambda e, ob=ob, hs=hs: e.tensor_tensor(out=x2ap[:, hs], in0=pbf(ob), in1=g2B[:, hs], op=ALU.mult),
                              [PB[ob], g2B], [x2])
                        V(lambda e, xt=xt: e.tensor_tensor(out=x2ap, in0=x2ap, in1=xt[:], op=ALU.add), [x2, xt], [x2])
                        junk, ssq, xn = wk
                        A(lambda e: e.activation(out=junk[:], in_=x2ap, func=AF.Square, accum_out=ssq[:]), [x2], [junk, ssq])
                        A(lambda e: e.activation(out=ssq[:], in_=ssq[:], func=AF.Sqrt, scale=1.0 / 1024, bias=EPS), [ssq], [ssq])
                        V(lambda e: e.reciprocal(out=ssq[:], in_=ssq[:]), [ssq], [ssq])
                        V(lambda e: e.scalar_tensor_tensor(out=x2ap, in0=x2ap, scalar=ssq[:, 0:1], in1=fingB[:], op0=ALU.mult, op1=ALU.mult),
                          [x2, ssq, fingB], [x2])
                        ti = (blk * 2 + tl) % NT_L
                        fw.dma("pool", out_d[sq][ti * 128:(ti + 1) * 128, :], x2ap, outb, x2)
        fw.dead = False
        fw.finish([outb], "sp")
        fw.barrier()
    return nc


def _layout(inp, core):
    b0 = 2 * core
    f = lambda a: np.ascontiguousarray(a, dtype=np.float32)
    csel = np.stack([inp["c"][b0], inp["c"][b0 + 1], inp["c_ctx"]])
    w_in = inp["w_in"][0]
    w_in_r = np.concatenate([w_in[:, 0:2048], w_in[:, 2064:3600], w_in[:, 2048:2064], w_in[:, 3600:3632]], axis=1)
    return {
        "x": f(inp["x"][b0:b0 + 2]), "ctx": f(inp["ctx"][b0:b0 + 2]),
        "cT": f(csel.reshape(3, 8, 128).transpose(2, 1, 0)),
        "w_ada": f(inp["w_ada"][0]), "b_adaT": f(inp["b_ada"][0].reshape(48, 128).T),
        "n1gT": f(inp["norm1_g"][0].reshape(8, 128).T), "n2gT": f(inp["norm2_g"][0].reshape(8, 128).T),
        "final_g": f(inp["final_g"].reshape(1, 1024)), "w_in": f(w_in_r),
        "convT": f(inp["conv_w"][0].T.reshape(12, 128, 5).transpose(1, 0, 2)),
        "a_log": f(inp["dn_a_log"][0].reshape(1, 8)), "dt_bias": f(inp["dn_dt_bias"][0].reshape(1, 8)),
        "dn_g": f(inp["dn_norm_g"][0].reshape(1, 128)), "gla_g": f(inp["gla_norm_g"][0].reshape(1, 128)),
        "wa2": f(inp["gla_wa2"][0]), "ba": f(inp["gla_ba"][0].reshape(2, 1, 256)),
        "w_out": f(inp["w_out"][0]), "wq": f(inp["peer_wq"][0]),
        "keysT": f(inp["peer_keys"][0].reshape(16, 128, 128).transpose(2, 0, 1)),
        "peer_u": f(inp["peer_u"][0]), "peer_v": f(inp["peer_v"][0]),
        "consts": make_consts(),
    }


def kernel(**inputs):
    inp = {k: np.asarray(v) for k, v in inputs.items()}
    nc = build_program(9)
    in_maps = [_layout(inp, c) for c in range(8)]
    res = run_bass_kernel_spmd(nc, in_maps, core_ids=list(range(8)))
    return np.concatenate([r["out"] for r in res.results], axis=0).astype(np.float32)
```

```python
import contextlib
import numpy as np
import concourse.bass as bass
import concourse.mybir as mybir
from concourse.bass_utils import run_bass_kernel_spmd

F32 = mybir.dt.float32
BF16 = mybir.dt.bfloat16
ALU = mybir.AluOpType
AF = mybir.ActivationFunctionType
AX = mybir.AxisListType

NEG = -30000.0
EPS = 1e-6


class Buf:
    __slots__ = ("name", "t", "wev", "revs", "dsem", "dcnt", "pre")

    def __init__(self, name, t=None):
        self.name = name
        self.t = t
        self.wev = []
        self.revs = []
        self.dsem = None
        self.dcnt = 0
        self.pre = []

    def __getitem__(self, k):
        return self.t[k]


class Eng:
    def __init__(self, name, h, sem):
        self.name = name
        self.h = h
        self.sem = sem
        self.cnt = 0
        self.known = {}


class FW:
    def __init__(self, nc, stack):
        self.nc = nc
        self.top = stack
        self.stack = stack
        self.engs = {}
        self.dsems = []
        for name, h in (("pe", nc.tensor), ("act", nc.scalar), ("dve", nc.vector),
                        ("pool", nc.gpsimd), ("sp", nc.sync)):
            sem = stack.enter_context(nc.semaphore("s_" + name))
            self.engs[name] = Eng(name, h, sem)
        self.ninst = 0

    @contextlib.contextmanager
    def scope(self):
        old = self.stack
        with contextlib.ExitStack() as st:
            self.stack = st
            try:
                yield
            finally:
                self.barrier()
                self.stack = old

    def sb(self, name, shape, dt=F32):
        self.nsb = getattr(self, "nsb", 0) + 1
        name = "sb%d_%s" % (self.nsb, name)
        t = self.stack.enter_context(self.nc.sbuf_tensor(name, list(shape), dt))
        return Buf(name, t)

    def view(self, name, t=None):
        return Buf(name, t)

    def _waits(self, e, reads, writes, skip=None):
        need = {}
        for b in reads:
            for (s, v) in b.wev:
                if need.get(s, 0) < v:
                    need[s] = v
        for b in writes:
            for (s, v) in b.wev:
                if need.get(s, 0) < v:
                    need[s] = v
            for (s, v) in b.revs:
                if need.get(s, 0) < v:
                    need[s] = v
        for s, v in need.items():
            if s is skip or (e.name == "pe" and s is e.sem):
                continue
            if e.known.get(s, 0) < v:
                e.h.wait_ge(s, v)
                e.known[s] = v

    def op(self, eng, fn, reads=(), writes=()):
        if getattr(self, "dead", False):
            return None
        e = self.engs[eng]
        self._waits(e, reads, writes)
        ins = fn(e.h)
        e.cnt += 1
        ins.then_inc(e.sem, 1)
        self.ninst += 1
        ev = (e.sem, e.cnt)
        for b in reads:
            if len(b.revs) > 24:
                b.revs = b.revs[-12:] + self._maxev(b.revs[:-12])
            b.revs.append(ev)
        for b in writes:
            b.wev = [ev]
            b.revs = []
        return ins

    @staticmethod
    def _maxev(evs):
        d = {}
        for (s, v) in evs:
            if d.get(s, (None, 0))[1] < v:
                d[s] = (s, v)
        return list(d.values())

    def dma(self, q, out_ap, in_ap, dst, src, **kw):
        if getattr(self, "dead", False):
            return None
        e = self.engs[q]
        reads = [src] if src is not None else []
        if dst.dsem is None:
            dst.dsem = self.top.enter_context(self.nc.semaphore("d%d_%s" % (len(self.dsems), dst.name)))
            self.dsems.append(dst)
        evs = [ev for ev in dst.wev if ev[0] is not dst.dsem] + list(dst.revs)
        if evs:
            dst.pre = evs
        else:
            evs = dst.pre
        for (sm, v) in evs:
            if e.known.get(sm, 0) < v:
                e.h.wait_ge(sm, v)
                e.known[sm] = v
        self._waits(e, reads, [], skip=dst.dsem)
        ins = e.h.dma_start(out=out_ap, in_=in_ap, **kw)
        self.ninst += 1
        dst.dcnt += 16
        ins.then_inc(dst.dsem, 16)
        ev = (dst.dsem, dst.dcnt)
        if src is not None:
            src.revs.append(ev)
        dst.wev = [ev]
        dst.revs = []
        return ins

    def barrier(self):
        for e in self.engs.values():
            for f in self.engs.values():
                if f is e or f.cnt == 0:
                    continue
                if e.known.get(f.sem, 0) < f.cnt:
                    e.h.wait_ge(f.sem, f.cnt)
                    e.known[f.sem] = f.cnt
            for b in self.dsems:
                if b.dcnt and e.known.get(b.dsem, 0) < b.dcnt:
                    e.h.wait_ge(b.dsem, b.dcnt)
                    e.known[b.dsem] = b.dcnt

    def finish(self, bufs, eng="sp"):
        self._waits(self.engs[eng], bufs, [])


def make_consts():
    p = np.arange(128)[:, None]
    f = np.arange(128)[None, :]
    c = np.zeros((128, 15, 128), np.float32)
    c[:, 0] = (p == f)
    c[:, 1] = (p <= f)
    c[:, 2] = (p >= f)
    c[:, 3] = np.where(p > f, 0.0, NEG)
    c[:, 4] = np.where(p < f, 0.0, NEG)
    c[:, 5] = np.where(p <= f, 0.0, NEG)
    c[:, 6] = np.where(p >= f, 0.0, NEG)
    c[:, 7] = (p <= f)
    c[:, 8] = (p >= f)
    c[:, 9] = 1.0
    c[:, 10] = -(p <= f).astype(np.float32) / 16.0
    c[:, 11] = -(p >= f).astype(np.float32) / 16.0
    c[:, 12] = f + 0.0 * p
    c[:, 13] = -(p <= f).astype(np.float32)
    c[:, 14] = -(p >= f).astype(np.float32)
    return c


def dn_scan(fw, nc, PS, PB, pbf, pbb, cst, cstb, Pq, CO, BET, NBET, GG, oacc):
    import itertools
    V = lambda fn, r, w: fw.op("dve", fn, r, w)
    A = lambda fn, r, w: fw.op("act", fn, r, w)
    T = lambda fn, r, w: fw.op("pe", fn, r, w)
    ident_b = cstb[:, 0, :]
    ident_f = cst[:, 0, :]
    ones_f = cst[:, 9, :]
    r4 = lambda ap: ap.rearrange("p (h t) -> p h t", t=128)
    bc = lambda ap: ap.unsqueeze(2).to_broadcast([128, 4, 128])
    V(lambda e: e.memset(oacc[:].rearrange("p a h t -> p (a h t)"), 0.0), [], [oacc])

    def chain(d, b):
        n_ = lambda nm, sh, dt=F32: fw.sb("d%d%s" % (d, nm), sh, dt)
        S = n_("S", [128, 4, 128]); Sbf = n_("Sbf", [128, 4, 128], BF16)
        kv = n_("kv", [128, 8, 128], BF16)
        Gb = n_("Gb", [128, 4, 128])
        sml = n_("sml", [128, 4, 4]); gct = n_("gct", [128, 8])
        E1 = n_("E1", [128, 4, 128]); E2 = E1; E3 = E1
        Y = n_("Y", [128, 4, 128]); Z = n_("Z", [128, 4, 128]); Rf = n_("Rf", [128, 4, 128])
        R = n_("R", [128, 4, 128], BF16); At = n_("At", [128, 4, 128], BF16); qg = n_("qg", [128, 4, 128], BF16)
        kbg = n_("kbg", [128, 4, 128], BF16); kd = n_("kd", [128, 4, 128], BF16); vb = n_("vb", [128, 4, 128], BF16)
        Usb = E1; WT = n_("WT", [128, 4, 128], BF16); vnew = n_("vn", [128, 4, 128], BF16)
        b0, b1, b2, b3 = b
        Cum = cst[:, 1 + d, :]
        nCum = cst[:, 13 + d, :]
        NM1 = cst[:, 3 + d, :].unsqueeze(1).to_broadcast([128, 4, 128])
        NM2 = cst[:, 5 + d, :].unsqueeze(1).to_broadcast([128, 4, 128])
        V(lambda e: e.memset(S[:], 0.0), [], [S])
        V(lambda e: e.memset(Sbf[:], 0.0), [], [Sbf])
        order = [(0, i) for i in ((0, 1) if d == 0 else (1, 0))] + \
                [(1, i) for i in (range(16) if d == 0 else range(15, -1, -1))]
        H = [slice(h * 128, (h + 1) * 128) for h in range(4)]
        for (seg, i) in order:
            gi = i if seg == 0 else 2 + i
            c0 = CO[seg] + i * 128
            g4 = GG[:, gi, d * 4:d * 4 + 4]
            b4 = BET[:, gi, d * 4:d * 4 + 4]
            nb4 = NBET[:, gi, d * 4:d * 4 + 4]
            T(lambda e: e.matmul(out=pbf(b0)[:, 0:4], lhsT=Cum, rhs=g4, start=True, stop=True), [cst, GG], [PB[b0]])
            T(lambda e: e.matmul(out=pbf(b0)[:, 4:8], lhsT=ones_f, rhs=g4, start=True, stop=True), [cst, GG], [PB[b0]])
            V(lambda e: e.tensor_copy(out=Gb[:], in_=bc(g4)), [GG], [Gb])
            A(lambda e: e.copy(out=gct[:], in_=pbf(b0)[:, 0:8]), [PB[b0]], [gct])
            yield
            for h in range(4):
                kT = Pq[:, 4 + h, c0:c0 + 128]
                qT = Pq[:, h, c0:c0 + 128]
                T(lambda e, kT=kT, h=h: e.matmul(out=pbf(b1)[:, H[h]], lhsT=kT, rhs=kT, start=True, stop=True), [Pq], [PB[b1]])
                T(lambda e, kT=kT, qT=qT, h=h: e.matmul(out=pbf(b2)[:, H[h]], lhsT=kT, rhs=qT, start=True, stop=True), [Pq], [PB[b2]])
                T(lambda e, h=h: e.matmul(out=pbf(b3)[:, H[h]], lhsT=Cum, rhs=Gb[:, h, :], start=True, stop=False), [cst, Gb], [PB[b3]])
                T(lambda e, h=h: e.matmul(out=pbf(b3)[:, H[h]], lhsT=Gb[:, h, :], rhs=nCum, start=False, stop=True), [cst, Gb], [PB[b3]])
            A(lambda e: e.activation(out=sml[:, 0, :], in_=gct[:, 0:4], func=AF.Exp), [gct], [sml])
            V(lambda e: e.tensor_tensor(out=sml[:, 1, :], in0=gct[:, 4:8], in1=gct[:, 0:4], op=ALU.subtract), [gct], [sml])
            A(lambda e: e.activation(out=sml[:, 1, :], in_=sml[:, 1, :], func=AF.Exp), [sml], [sml])
            A(lambda e: e.activation(out=sml[:, 2, :], in_=gct[:, 4:8], func=AF.Exp), [gct], [sml])
            V(lambda e: e.tensor_tensor(out=sml[:, 3, :], in0=sml[:, 0, :], in1=b4, op=ALU.mult), [sml, BET], [sml])
            yield
            V(lambda e: e.scalar_tensor_tensor(out=E1[:], in0=r4(pbf(b3)), scalar=0.0, in1=NM1, op0=ALU.min, op1=ALU.add),
              [PB[b3], cst], [E1])
            for h in range(4):
                T(lambda e, h=h: e.matmul(out=pbf(b3)[:, H[h]], lhsT=Gb[:, h, :], rhs=Cum, start=True, stop=False), [cst, Gb], [PB[b3]])
                T(lambda e, h=h: e.matmul(out=pbf(b3)[:, H[h]], lhsT=nCum, rhs=Gb[:, h, :], start=False, stop=True), [cst, Gb], [PB[b3]])
            A(lambda e: e.activation(out=E1[:], in_=E1[:], func=AF.Exp), [E1], [E1])
            V(lambda e: e.tensor_tensor(out=E1[:], in0=r4(pbf(b1)), in1=E1[:], op=ALU.mult), [PB[b1], E1], [E1])
            V(lambda e: e.tensor_tensor(out=Y[:], in0=E1[:], in1=bc(nb4), op=ALU.mult), [E1, NBET], [Y])
            yield
            V(lambda e: e.scalar_tensor_tensor(out=E2[:], in0=r4(pbf(b3)), scalar=0.0, in1=NM2, op0=ALU.min, op1=ALU.add),
              [PB[b3], cst], [E2])
            for h in range(4):
                T(lambda e, h=h: e.matmul(out=pbf(b3)[:, H[h]], lhsT=Gb[:, h, :], rhs=Cum, start=True, stop=True), [cst, Gb], [PB[b3]])
            A(lambda e: e.activation(out=E2[:], in_=E2[:], func=AF.Exp), [E2], [E2])
            V(lambda e: e.tensor_tensor(out=At[:], in0=r4(pbf(b2)), in1=E2[:], op=ALU.mult), [PB[b2], E2], [At])
            for h in range(4):
                T(lambda e, h=h: e.transpose(out=pbb(b0)[:, h * 128:(h + 1) * 128], in_=Pq[:, 4 + h, c0:c0 + 128], identity=ident_b),
                  [Pq, cstb], [PB[b0]])
                T(lambda e, h=h: e.transpose(out=pbb(b0)[:, 512 + h * 128:512 + (h + 1) * 128], in_=Pq[:, 8 + h, c0:c0 + 128],
                                             identity=ident_b), [Pq, cstb], [PB[b0]])
            A(lambda e: e.activation(out=E3[:], in_=r4(pbf(b3)), func=AF.Exp), [PB[b3]], [E3])
            A(lambda e: e.copy(out=kv[:].rearrange("p a t -> p (a t)"), in_=pbb(b0)), [PB[b0]], [kv])
            V(lambda e: e.tensor_tensor(out=qg[:], in0=Pq[:, 0:4, c0:c0 + 128], in1=E3[:], op=ALU.mult), [Pq, E3], [qg])
            yield
            for h in range(4):
                T(lambda e, h=h: e.transpose(out=pbf(b0)[:, H[h]], in_=Y[:, h, :], identity=ident_f), [Y, cst], [PB[b0]])
            A(lambda e: e.copy(out=Z[:], in_=r4(pbf(b0))), [PB[b0]], [Z])
            V(lambda e: e.tensor_tensor(out=Rf[:], in0=Z[:], in1=cst[:, 0, :].unsqueeze(1).to_broadcast([128, 4, 128]), op=ALU.add),
              [Z, cst], [Rf])
            V(lambda e: e.tensor_tensor(out=kbg[:], in0=kv[:, 0:4, :], in1=bc(sml[:, 3, :]), op=ALU.mult), [kv, sml], [kbg])
            V(lambda e: e.tensor_tensor(out=kd[:], in0=kv[:, 0:4, :], in1=bc(sml[:, 1, :]), op=ALU.mult), [kv, sml], [kd])
            V(lambda e: e.tensor_tensor(out=vb[:], in0=kv[:, 4:8, :], in1=bc(b4), op=ALU.mult), [kv, BET], [vb])
            yield
            for lvl in range(1, 7):
                for h in range(4):
                    T(lambda e, h=h: e.matmul(out=pbf(b1)[:, H[h]], lhsT=Z[:, h, :], rhs=Y[:, h, :], start=True, stop=True), [Y, Z], [PB[b1]])
                    if lvl < 6:
                        T(lambda e, h=h: e.matmul(out=pbf(b2)[:, H[h]], lhsT=Y[:, h, :], rhs=Z[:, h, :], start=True, stop=True), [Y, Z], [PB[b2]])
                A(lambda e: e.copy(out=Y[:], in_=r4(pbf(b1))), [PB[b1]], [Y])
                if lvl < 6:
                    V(lambda e: e.tensor_copy(out=Z[:], in_=r4(pbf(b2))), [PB[b2]], [Z])
                for h in range(4):
                    T(lambda e, h=h: e.matmul(out=pbf(b3)[:, H[h]], lhsT=Y[:, h, :], rhs=Rf[:, h, :], start=True, stop=True), [Y, Rf], [PB[b3]])
                V(lambda e: e.tensor_tensor(out=Rf[:], in0=r4(pbf(b3)), in1=Rf[:], op=ALU.add), [PB[b3], Rf], [Rf])
                yield
            A(lambda e: e.copy(out=R[:], in_=Rf[:]), [Rf], [R])
            for h in range(4):
                T(lambda e, h=h: e.matmul(out=pbf(b1)[:, H[h]], lhsT=R[:, h, :], rhs=vb[:, h, :], start=True, stop=True), [R, vb], [PB[b1]])
                T(lambda e, h=h: e.matmul(out=pbf(b2)[:, H[h]], lhsT=kbg[:, h, :], rhs=R[:, h, :], start=True, stop=True), [R, kbg], [PB[b2]])
            A(lambda e: e.copy(out=Usb[:], in_=r4(pbf(b1))), [PB[b1]], [Usb])
            V(lambda e: e.tensor_copy(out=WT[:], in_=r4(pbf(b2))), [PB[b2]], [WT])
            yield
            for h in range(4):
                T(lambda e, h=h: e.matmul(out=pbf(b0)[:, H[h]], lhsT=WT[:, h, :], rhs=Sbf[:, h, :], start=True, stop=True), [WT, Sbf], [PB[b0]])
            V(lambda e: e.tensor_tensor(out=vnew[:], in0=Usb[:], in1=r4(pbf(b0)), op=ALU.subtract), [Usb, PB[b0]], [vnew])
            if seg == 1:
                for h in range(4):
                    T(lambda e, h=h: e.matmul(out=pbf(b3)[:, H[h]], lhsT=qg[:, h, :], rhs=Sbf[:, h, :], start=True, stop=False), [qg, Sbf], [PB[b3]])
                    T(lambda e, h=h: e.matmul(out=pbf(b3)[:, H[h]], lhsT=At[:, h, :], rhs=vnew[:, h, :], start=False, stop=True), [At, vnew], [PB[b3]])
            for h in range(4):
                T(lambda e, h=h: e.matmul(out=pbf(b1)[:, H[h]], lhsT=kd[:, h, :], rhs=vnew[:, h, :], start=True, stop=True), [kd, vnew], [PB[b1]])
            if seg == 1:
                V(lambda e: e.tensor_tensor(out=oacc[:, i], in0=r4(pbf(b3)), in1=oacc[:, i], op=ALU.add), [PB[b3], oacc], [oacc])
            V(lambda e: e.tensor_tensor(out=S[:], in0=S[:], in1=bc(sml[:, 2, :]), op=ALU.mult), [S, sml], [S])
            V(lambda e: e.tensor_tensor(out=S[:], in0=r4(pbf(b1)), in1=S[:], op=ALU.add), [PB[b1], S], [S])
            A(lambda e: e.copy(out=Sbf[:], in_=S[:]), [S], [Sbf])
            yield

    gens = [chain(0, (0, 1, 2, 3)), chain(1, (4, 5, 6, 7))]
    for _ in itertools.zip_longest(*gens):
        pass


def gla_scan(fw, PB, pbf, pbb, cst, cstb, gqk, gv, LRT, wa2_d, ba_d, oacc):
    V = lambda fn, r, w: fw.op("dve", fn, r, w)
    A = lambda fn, r, w: fw.op("act", fn, r, w)
    T = lambda fn, r, w: fw.op("pe", fn, r, w)
    ident_b = cstb[:, 0, :]
    r4 = lambda ap: ap.rearrange("p (h t) -> p h t", t=128)
    wa2f = fw.sb("wa2f", [16, 2, 256])
    baf = fw.sb("baf", [1, 2, 256])
    wa2b = fw.sb("wa2b", [16, 2, 256], BF16)
    bab = fw.sb("bab", [1, 2, 256], BF16)
    fw.dma("sp", wa2f[:], wa2_d.rearrange("d r c -> r d c"), wa2f, None)
    fw.dma("sp", baf[:], ba_d.rearrange("d o c -> o d c"), baf, None)
    V(lambda e: e.tensor_copy(out=wa2b[:], in_=wa2f[:]), [wa2f], [wa2b])
    V(lambda e: e.tensor_copy(out=bab[:], in_=baf[:]), [baf], [bab])
    S = fw.sb("gS", [64, 4, 128])
    Sbf = fw.sb("gSbf", [64, 4, 128], BF16)
    sp = fw.sb("gsp", [128, 256])
    bT = fw.sb("gbT", [64, 4, 128])
    eb = fw.sb("geb", [64, 4, 128])
    enb = fw.sb("genb", [64, 4, 128])
    ekd = fw.sb("gekd", [64, 4, 128])
    Qp = fw.sb("gQp", [64, 4, 128], BF16)
    Kp = fw.sb("gKp", [64, 4, 128], BF16)
    KdT = fw.sb("gKdT", [64, 4, 128], BF16)
    Kd = fw.sb("gKd", [128, 4, 64], BF16)
    att = fw.sb("gatt", [128, 4, 128], BF16)
    for d in (0, 1):
        CumS = cst[:, 10 + d, :]
        MK = cst[:, 7 + d, :].unsqueeze(1).to_broadcast([128, 4, 128])
        tl = 127 if d == 0 else 0
        V(lambda e: e.memset(S[:], 0.0), [], [S])
        V(lambda e: e.memset(Sbf[:], 0.0), [], [Sbf])
        order = [(0, i) for i in ((0, 1) if d == 0 else (1, 0))] + \
                [(1, i) for i in (range(16) if d == 0 else range(15, -1, -1))]
        for (seg, i) in order:
            gi = i if seg == 0 else 2 + i
            t0 = gi * 128
            T(lambda e: e.matmul(out=pbf(0)[:, 0:256], lhsT=LRT[:, d, t0:t0 + 128], rhs=wa2b[:, d, :], start=True, stop=False),
              [LRT, wa2b], [PB[0]])
            T(lambda e: e.matmul(out=pbf(0)[:, 0:256], lhsT=cstb[0:1, 1, :], rhs=bab[0:1, d, :], start=False, stop=True),
              [cstb, bab], [PB[0]])
            A(lambda e: e.activation(out=sp[:], in_=pbf(0)[:, 0:256], func=AF.Exp, scale=-1.0), [PB[0]], [sp])
            A(lambda e: e.activation(out=sp[:], in_=sp[:], func=AF.Ln, bias=1.0), [sp], [sp])
            for h in range(4):
                T(lambda e, h=h: e.matmul(out=pbf(1)[0:64, h * 128:(h + 1) * 128], lhsT=sp[:, h * 64:(h + 1) * 64], rhs=CumS,
                                          start=True, stop=True), [sp, cst], [PB[1]])
            A(lambda e: e.copy(out=bT[:], in_=r4(pbf(1)[0:64, :])), [PB[1]], [bT])
            A(lambda e: e.activation(out=eb[:], in_=bT[:], func=AF.Exp), [bT], [eb])
            A(lambda e: e.activation(out=enb[:], in_=bT[:], func=AF.Exp, scale=-1.0), [bT], [enb])
            V(lambda e: e.tensor_tensor(out=ekd[:], in0=bT[:], in1=bT[:, :, tl:tl + 1].to_broadcast([64, 4, 128]), op=ALU.subtract),
              [bT], [ekd])
            A(lambda e: e.activation(out=ekd[:], in_=ekd[:], func=AF.Exp, scale=-1.0), [ekd], [ekd])
            V(lambda e: e.tensor_tensor(out=Qp[:], in0=gqk[:, 0:4, t0:t0 + 128], in1=eb[:], op=ALU.mult), [gqk, eb], [Qp])
            V(lambda e: e.tensor_tensor(out=Kp[:], in0=gqk[:, 4:8, t0:t0 + 128], in1=enb[:], op=ALU.mult), [gqk, enb], [Kp])
            V(lambda e: e.tensor_tensor(out=KdT[:], in0=gqk[:, 4:8, t0:t0 + 128], in1=ekd[:], op=ALU.mult), [gqk, ekd], [KdT])
            for h in range(4):
                T(lambda e, h=h: e.transpose(out=pbb(2)[:, h * 64:(h + 1) * 64], in_=KdT[:, h, :], identity=cstb[0:64, 0, 0:64]),
                  [KdT, cstb], [PB[2]])
            A(lambda e: e.copy(out=Kd[:].rearrange("p h k -> p (h k)"), in_=pbb(2)[:, 0:256]), [PB[2]], [Kd])
            if seg == 1:
                for h in range(4):
                    T(lambda e, h=h: e.matmul(out=pbf(3)[:, h * 128:(h + 1) * 128], lhsT=Kp[:, h, :], rhs=Qp[:, h, :], start=True, stop=True),
                      [Kp, Qp], [PB[3]])
                V(lambda e: e.tensor_tensor(out=att[:], in0=r4(pbf(3)), in1=MK, op=ALU.mult), [PB[3], cst], [att])
                for h in range(4):
                    hs = slice(h * 128, (h + 1) * 128)
                    T(lambda e, h=h, hs=hs: e.matmul(out=pbf(4)[:, hs], lhsT=Qp[:, h, :], rhs=Sbf[:, h, :], start=True, stop=False),
                      [Qp, Sbf], [PB[4]])
                    T(lambda e, h=h, hs=hs: e.matmul(out=pbf(4)[:, hs], lhsT=att[:, h, :], rhs=gv[:, gi, hs], start=False, stop=True),
                      [att, gv], [PB[4]])
                if d == 0:
                    A(lambda e: e.copy(out=oacc[:, i], in_=r4(pbf(4))), [PB[4]], [oacc])
                else:
                    V(lambda e: e.tensor_tensor(out=oacc[:, i], in0=r4(pbf(4)), in1=oacc[:, i], op=ALU.add), [PB[4], oacc], [oacc])
            for h in range(4):
                hs = slice(h * 128, (h + 1) * 128)
                T(lambda e, h=h, hs=hs: e.matmul(out=pbf(5)[0:64, hs], lhsT=Kd[:, h, :], rhs=gv[:, gi, hs], start=True, stop=True),
                  [Kd, gv], [PB[5]])
            V(lambda e: e.tensor_tensor(out=S[:], in0=S[:], in1=eb[:, :, tl:tl + 1].to_broadcast([64, 4, 128]), op=ALU.mult), [S, eb], [S])
            V(lambda e: e.tensor_tensor(out=S[:], in0=r4(pbf(5)[0:64, :]), in1=S[:], op=ALU.add), [PB[5], S], [S])
            A(lambda e: e.copy(out=Sbf[:], in_=S[:]), [S], [Sbf])


def head_norm_gate(fw, PB, pbb, cstb, oacc, gate, gnorm, mixT, chunk0, permute):
    V = lambda fn, r, w: fw.op("dve", fn, r, w)
    A = lambda fn, r, w: fw.op("act", fn, r, w)
    T = lambda fn, r, w: fw.op("pe", fn, r, w)
    ident_b = cstb[:, 0, :]
    sq = fw.sb("hsq", [128, 4, 128])
    ss = fw.sb("hss", [128, 4])
    mix = fw.sb("hmix", [128, 4, 128], BF16)
    for i in range(NT_L):
        o = oacc[:, i]
        V(lambda e: e.tensor_tensor(out=sq[:], in0=o, in1=o, op=ALU.mult), [oacc], [sq])
        V(lambda e: e.tensor_reduce(out=ss[:], in_=sq[:], axis=AX.X, op=ALU.add), [sq], [ss])
        A(lambda e: e.activation(out=ss[:], in_=ss[:], func=AF.Sqrt, scale=1.0 / 128, bias=EPS), [ss], [ss])
        V(lambda e: e.reciprocal(out=ss[:], in_=ss[:]), [ss], [ss])
        V(lambda e: e.tensor_tensor(out=sq[:], in0=o, in1=ss[:].unsqueeze(2).to_broadcast([128, 4, 128]), op=ALU.mult), [oacc, ss], [sq])
        V(lambda e: e.tensor_tensor(out=sq[:], in0=sq[:], in1=gnorm[:].unsqueeze(1).to_broadcast([128, 4, 128]), op=ALU.mult), [sq, gnorm], [sq])
        V(lambda e: e.tensor_tensor(out=mix[:], in0=sq[:], in1=gate[:, i, :].rearrange("p (h t) -> p h t", t=128), op=ALU.mult),
          [sq, gate], [mix])
        bk = i % 2
        for h in range(4):
            T(lambda e, h=h: e.transpose(out=pbb(bk)[:, h * 128:(h + 1) * 128], in_=mix[:, h, :], identity=ident_b), [mix, cstb], [PB[bk]])
        if not permute:
            A(lambda e: e.copy(out=mixT[:, chunk0:chunk0 + 4, i * 128:(i + 1) * 128],
                               in_=pbb(bk)[:, 0:512].rearrange("p (h t) -> p h t", t=128)), [PB[bk]], [mixT])
        else:
            for h in range(4):
                dst = mixT[:, chunk0 + h, :].rearrange("p (r c) -> p c r", c=64)[:, 4 * i:4 * i + 4, :]
                src = pbb(bk)[:, h * 128:(h + 1) * 128].rearrange("p (c r) -> p c r", r=32)
                if h % 2:
                    A(lambda e, dst=dst, src=src: e.copy(out=dst, in_=src), [PB[bk]], [mixT])
                else:
                    V(lambda e, dst=dst, src=src: e.tensor_copy(out=dst, in_=src), [PB[bk]], [mixT])


class StopBuild(Exception):
    pass


NSEQ = 2
TL = 2048
TC = 256
NT_L = 16
NT_C = 2
WCOLS = 3632


def build_program(stage=9):
    nc = bass.Bass("TRN2", target_bir_lowering=False)
    din = lambda n, s: nc.dram_tensor(n, list(s), F32, kind="ExternalInput").ap()
    x_d = din("x", [NSEQ, TL, 1024])
    ctx_d = din("ctx", [NSEQ, TC, 1024])
    cT_d = din("cT", [128, 8, 3])
    wada_d = din("w_ada", [1024, 6144])
    badaT_d = din("b_adaT", [128, 48])
    n1g_d = din("n1gT", [128, 8])
    n2g_d = din("n2gT", [128, 8])
    fing_d = din("final_g", [1, 1024])
    win_d = din("w_in", [1024, WCOLS])
    convT_d = din("convT", [128, 12, 5])
    alog_d = din("a_log", [1, 8])
    dtb_d = din("dt_bias", [1, 8])
    dng_d = din("dn_g", [1, 128])
    glg_d = din("gla_g", [1, 128])
    wa2_d = din("wa2", [2, 16, 256])
    ba_d = din("ba", [2, 1, 256])
    wout_d = din("w_out", [1024, 1024])
    wq_d = din("wq", [1024, 2048])
    keysT_d = din("keysT", [128, 16, 128])
    u_d = din("peer_u", [16384, 1024])
    v_d = din("peer_v", [16384, 1024])
    consts_d = din("consts", [128, 15, 128])
    out_d = nc.dram_tensor("out", [NSEQ, TL, 1024], F32, kind="ExternalOutput").ap()
    dbg_d = nc.dram_tensor("dbg", [128, 8, TL], F32, kind="ExternalOutput").ap() if stage < 2 else None
    PDBG = (stage == 3.5)
    X1OUT = (stage == 8)
    if stage == 8:
        stage = 9
    if PDBG:
        dps_d = nc.dram_tensor("dps", [128, 2048], F32, kind="ExternalOutput").ap()
        dptop_d = nc.dram_tensor("dptop", [128, 256], F32, kind="ExternalOutput").ap()
        dpcv_d = nc.dram_tensor("dpcv", [128, 128], F32, kind="ExternalOutput").ap()
        dpt4_d = nc.dram_tensor("dpt4", [128, 512], F32, kind="ExternalOutput").ap()
        dpt4t_d = nc.dram_tensor("dpt4t", [128, 512], F32, kind="ExternalOutput").ap()
        dpw_d = nc.dram_tensor("dpw", [128, 128, 8], F32, kind="ExternalOutput").ap()
        dppo_d = nc.dram_tensor("dppo", [128, 1024], F32, kind="ExternalOutput").ap()
        dpsr_d = nc.dram_tensor("dpsr", [128, 2, 2048], F32, kind="ExternalOutput").ap()
        dpab_d = nc.dram_tensor("dpab", [128, 2, 512], F32, kind="ExternalOutput").ap()
    modrow_d = nc.dram_tensor("modrow", [3, 6144], F32, kind="Internal").ap()
    x1_d = nc.dram_tensor("x1s", [NSEQ * TL, 1024], F32, kind=("ExternalOutput" if X1OUT else "Internal")).ap()
    ss_d = nc.dram_tensor("sscr", [2, 8, NSEQ * TL, 128], F32, kind="Internal").ap()
    ut_d = nc.dram_tensor("utscr", [128, 128, 8, 128], BF16, kind="Internal").ap()
    vb_d = nc.dram_tensor("vbscr", [128, 128, 1024], BF16, kind="Internal").ap()

    with contextlib.ExitStack() as top:
        fw = FW(nc, top)
        V = lambda fn, r, w: fw.op("dve", fn, r, w)
        A = lambda fn, r, w: fw.op("act", fn, r, w)
        G = lambda fn, r, w: fw.op("pool", fn, r, w)
        T = lambda fn, r, w: fw.op("pe", fn, r, w)

        PS = top.enter_context(nc.psum_tensor("ps", [128, 4096], F32))
        PB = [Buf("pb%d" % i, PS[:, i * 512:(i + 1) * 512]) for i in range(8)]

        def pbf(i, n=1):
            return PS[:, i * 512:(i + n) * 512]

        def pbb(i, n=1):
            return PS[:, i * 512:(i + n) * 512].bitcast(BF16)

        outb = fw.view("outb")
        cst = fw.sb("cst", [128, 15, 128])
        fw.dma("sp", cst[:], consts_d, cst, None)
        cstb = fw.sb("cstb", [128, 3, 128], BF16)
        for a_, b_ in ((0, 0), (1, 9), (2, 12)):
            V(lambda e, a_=a_, b_=b_: e.tensor_copy(out=cstb[:, a_, :], in_=cst[:, b_, :]), [cst], [cstb])
        ident_f = cst[:, 0, :]
        ident_b = cstb[:, 0, :]
        ones_b = cstb[:, 1, :]
        ones_f = cst[:, 9, :]

        modT = fw.sb("modT", [128, 48, 3])
        G1 = fw.sb("G1", [128, 8, 3])
        G2 = fw.sb("G2", [128, 8, 3])
        modrow = fw.view("modrow")
        x1buf = fw.view("x1buf")

        with fw.scope():
            cT = fw.sb("cT", [128, 8, 3])
            fw.dma("sp", cT[:], cT_d, cT, None)
            siluT = fw.sb("siluT", [128, 8, 3])
            A(lambda e: e.activation(out=siluT[:], in_=cT[:], func=AF.Silu), [cT], [siluT])
            badaT = fw.sb("badaT", [128, 48])
            fw.dma("sp", badaT[:], badaT_d, badaT, None)
            n1g = fw.sb("n1g", [128, 8])
            n2g = fw.sb("n2g", [128, 8])
            fw.dma("sp", n1g[:], n1g_d, n1g, None)
            fw.dma("sp", n2g[:], n2g_d, n2g, None)
            slabs = [fw.sb("wada%d" % i, [128, 8, 1024]) for i in range(2)]
            wv = wada_d.rearrange("(k p) c -> p k c", p=128)
            pm = pbf(0)[:, 0:144].rearrange("p (j b) -> p j b", b=3)
            for v in range(6):
                sl = slabs[v % 2]
                fw.dma("sp" if v % 2 == 0 else "act", sl[:], wv[:, :, v * 1024:(v + 1) * 1024], sl, None)
                for ch in range(8):
                    j = v * 8 + ch
                    for k in range(8):
                        T(lambda e, sl=sl, ch=ch, k=k, j=j: e.matmul(
                            out=pm[:, j, :], lhsT=sl[:, k, ch * 128:(ch + 1) * 128], rhs=siluT[:, k, :],
                            start=(k == 0), stop=(k == 7)), [sl, siluT], [PB[0]])
            V(lambda e: e.tensor_tensor(out=modT[:], in0=pm[:, 0:48, :],
                                        in1=badaT[:].unsqueeze(2).to_broadcast([128, 48, 3]), op=ALU.add),
              [PB[0], badaT], [modT])
            for (Gt, ng, o) in ((G1, n1g, 8), (G2, n2g, 32)):
                V(lambda e, Gt=Gt, o=o: e.tensor_scalar(out=Gt[:], in0=modT[:, o:o + 8, :], scalar1=1.0, scalar2=None,
                                                        op0=ALU.add), [modT], [Gt])
                V(lambda e, Gt=Gt, ng=ng: e.tensor_tensor(out=Gt[:], in0=Gt[:],
                                                          in1=ng[:].unsqueeze(2).to_broadcast([128, 8, 3]), op=ALU.mult),
                  [Gt, ng], [Gt])
            for b in range(3):
                fw.dma("pool", modrow_d[b].rearrange("(j p) -> p j", p=128), modT[:, :, b], modrow, modT,
                       allow_slow_non_contiguous=True)

        def make_hT(hT, col0, src_ap, ntiles, Gt, SHo, mcol, xt_bufs, wk, perm=False, srcbuf=None):
            for i in range(ntiles):
                xt = xt_bufs[i % 2]
                fw.dma("sp", xt[:], src_ap[i * 128:(i + 1) * 128, :], xt, srcbuf)
                junk, ssq, xn = wk
                A(lambda e, xt=xt: e.activation(out=junk[:], in_=xt[:], func=AF.Square, accum_out=ssq[:]),
                  [xt], [junk, ssq])
                A(lambda e: e.activation(out=ssq[:], in_=ssq[:], func=AF.Sqrt, scale=1.0 / 1024, bias=EPS), [ssq], [ssq])
                V(lambda e: e.reciprocal(out=ssq[:], in_=ssq[:]), [ssq], [ssq])
                V(lambda e, xt=xt: e.tensor_scalar(out=xn[:], in0=xt[:], scalar1=ssq[:, 0:1], scalar2=None, op0=ALU.mult),
                  [xt, ssq], [xn])
                pt = pbb(7).rearrange("p (k t) -> p k t", t=128)
                for k in range(8):
                    T(lambda e, k=k: e.transpose(out=pt[:, k, :], in_=xn[:, k * 128:(k + 1) * 128], identity=ident_b),
                      [xn, cstb], [PB[7]])
                if not perm:
                    dst = hT[:, :, col0 + i * 128: col0 + (i + 1) * 128]
                    src = pt[:, 0:8, :]
                    g_b = Gt[:, :, mcol:mcol + 1].to_broadcast([128, 8, 128])
                    s_b = modT[:, SHo:SHo + 8, mcol:mcol + 1].to_broadcast([128, 8, 128])
                else:
                    dst = hT[:, :, col0:col0 + TL].rearrange("p k (c r) -> p k r c", r=32)[:, :, 2 * i:2 * i + 2, :]
                    src = pt[:, 0:8, :].rearrange("p k (r c) -> p k r c", c=64)
                    g_b = Gt[:, :, mcol:mcol + 1].unsqueeze(3).to_broadcast([128, 8, 2, 64])
                    s_b = modT[:, SHo:SHo + 8, mcol:mcol + 1].unsqueeze(3).to_broadcast([128, 8, 2, 64])
                V(lambda e, dst=dst, src=src, g_b=g_b: e.tensor_tensor(out=dst, in0=src, in1=g_b, op=ALU.mult),
                  [PB[7], Gt], [hT])
                V(lambda e, dst=dst, s_b=s_b: e.tensor_tensor(out=dst, in0=dst, in1=s_b, op=ALU.add), [hT, modT], [hT])

        def load_w_bf16(dst, src_view, ncols, stage_bufs, step=512, d0=0):
            n = 0
            for c0 in range(0, ncols, step):
                c1 = min(ncols, c0 + step)
                sg = stage_bufs[n % 2]
                fw.dma("act" if n % 2 else "sp", sg[:, :, 0:c1 - c0], src_view[:, :, c0:c1], sg, None)
                if n % 2:
                    A(lambda e, sg=sg, c0=c0, c1=c1: e.copy(out=dst[:, :, d0 + c0:d0 + c1], in_=sg[:, :, 0:c1 - c0]), [sg], [dst])
                else:
                    V(lambda e, sg=sg, c0=c0, c1=c1: e.tensor_copy(out=dst[:, :, d0 + c0:d0 + c1], in_=sg[:, :, 0:c1 - c0]), [sg], [dst])
                n += 1

        win_v = win_d.rearrange("(k p) c -> p k c", p=128)

        if 0.6 < stage < 0.7:
            fw.dn_limit = int(round((stage - 0.6) * 1000))

        def ck(x):
            if stage <= x:
                fw.dead = True

        for s in range(NSEQ if stage >= 0.1 else 0):
          try:
            with fw.scope():
                mixT = fw.sb("mixT", [128, 8, TL], BF16)
                with fw.scope():
                    Pq = fw.sb("Pq", [128, 12, TC + TL + 8], BF16)
                    CO = (2, 262)
                    Zg = fw.sb("Zg", [128, NT_L, 512], BF16)
                    SM = fw.sb("SM", [128, 18, 16])
                    with fw.scope():
                        hT = fw.sb("hT", [128, 8, TC + TL], BF16)
                        xts = [fw.sb("xt%d" % i, [128, 1024]) for i in range(2)]
                        wk = (fw.sb("junk", [128, 1024], BF16), fw.sb("ssq", [128, 1]), fw.sb("xn", [128, 1024], BF16))
                        make_hT(hT, 0, ctx_d[s], NT_C, G1, 0, 2, xts, wk)
                        make_hT(hT, TC, x_d[s], NT_L, G1, 0, s, xts, wk)
                        ck(0.2)
                        wdn = fw.sb("wdn", [128, 8, 2064], BF16)
                        stg = [fw.sb("stg%d" % i, [128, 8, 128]) for i in range(2)]
                        load_w_bf16(wdn, win_v[:, :, 0:2048], 2048, stg, 128)
                        load_w_bf16(wdn, win_v[:, :, 3584:3600], 16, stg, 128, 2048)
                        V(lambda e: e.memset(Pq[:], 0.0), [], [Pq])
                        ck(0.3)
                        nb = 0
                        for (t0, tn, seg) in [(0, 256, 0)] + [(TC + b * 512, 512, 1) for b in range(4)]:
                            for c in range(12):
                                bk = nb % 2
                                nb += 1
                                for k in range(8):
                                    T(lambda e, bk=bk, c=c, k=k, t0=t0, tn=tn: e.matmul(
                                        out=pbf(bk)[:, 0:tn], lhsT=wdn[:, k, c * 128:(c + 1) * 128], rhs=hT[:, k, t0:t0 + tn],
                                        start=(k == 0), stop=(k == 7)), [wdn, hT], [PB[bk]])
                                d0 = CO[seg] + (t0 - (TC if seg else 0))
                                if bk:
                                    A(lambda e, bk=bk, c=c, d0=d0, tn=tn: e.copy(out=Pq[:, c, d0:d0 + tn], in_=pbf(bk)[:, 0:tn]),
                                      [PB[bk]], [Pq])
                                else:
                                    V(lambda e, bk=bk, c=c, d0=d0, tn=tn: e.tensor_copy(out=Pq[:, c, d0:d0 + tn], in_=pbf(bk)[:, 0:tn]),
                                      [PB[bk]], [Pq])
                        for i in range(18):
                            t0 = i * 128
                            if i >= 2:
                                for k in range(8):
                                    T(lambda e, k=k, t0=t0: e.matmul(out=pbf(2), lhsT=hT[:, k, t0:t0 + 128], rhs=wdn[:, k, 1536:2048],
                                                                     start=(k == 0), stop=(k == 7)), [wdn, hT], [PB[2]])
                                A(lambda e, i=i: e.activation(out=Zg[:, i - 2, :], in_=pbf(2), func=AF.Silu), [PB[2]], [Zg])
                            for k in range(8):
                                T(lambda e, k=k, t0=t0: e.matmul(out=pbf(3)[:, 0:16], lhsT=hT[:, k, t0:t0 + 128], rhs=wdn[:, k, 2048:2064],
                                                                 start=(k == 0), stop=(k == 7)), [wdn, hT], [PB[3]])
                            V(lambda e, i=i: e.tensor_copy(out=SM[:, i, :], in_=pbf(3)[:, 0:16]), [PB[3]], [SM])
                    ck(0.4)
                    BET = fw.sb("BET", [128, 18, 8])
                    NBET = fw.sb("NBET", [128, 18, 8])
                    GG = fw.sb("GG", [128, 18, 8])
                    alog = fw.sb("alog", [128, 8])
                    dtb = fw.sb("dtb", [128, 8])
                    fw.dma("sp", alog[:], alog_d.partition_broadcast(128), alog, None)
                    fw.dma("sp", dtb[:], dtb_d.partition_broadcast(128), dtb, None)
                    A(lambda e: e.activation(out=BET[:], in_=SM[:, :, 0:8], func=AF.Sigmoid), [SM], [BET])
                    V(lambda e: e.tensor_scalar(out=NBET[:], in0=BET[:], scalar1=-1.0, scalar2=None, op0=ALU.mult), [BET], [NBET])
                    V(lambda e: e.tensor_tensor(out=GG[:], in0=SM[:, :, 8:16], in1=dtb[:].unsqueeze(1).to_broadcast([128, 18, 8]),
                                                op=ALU.add), [SM, dtb], [GG])
                    A(lambda e: e.activation(out=GG[:], in_=GG[:], func=AF.Exp), [GG], [GG])
                    A(lambda e: e.activation(out=GG[:], in_=GG[:], func=AF.Ln, bias=1.0), [GG], [GG])
                    A(lambda e: e.activation(out=alog[:], in_=alog[:], func=AF.Exp), [alog], [alog])
                    V(lambda e: e.tensor_scalar(out=alog[:], in0=alog[:], scalar1=-1.0, scalar2=None, op0=ALU.mult), [alog], [alog])
                    V(lambda e: e.tensor_tensor(out=GG[:], in0=GG[:], in1=alog[:].unsqueeze(1).to_broadcast([128, 18, 8]),
                                                op=ALU.mult), [GG, alog], [GG])
                    ck(0.5)
                    cw = fw.sb("cw", [128, 12, 5])
                    fw.dma("sp", cw[:], convT_d, cw, None)
                    with fw.scope():
                        acc = fw.sb("acc", [128, TL])
                        sq = fw.sb("sq", [128, 512], BF16)
                        rin = fw.sb("rin", [128, 512])
                        for c in range(12):
                            for seg, n in ((0, TC), (1, TL)):
                                o = CO[seg]
                                V(lambda e, c=c, o=o, n=n: e.tensor_scalar(out=acc[:, 0:n], in0=Pq[:, c, o - 2:o - 2 + n],
                                                                           scalar1=cw[:, c, 0:1], scalar2=None, op0=ALU.mult),
                                  [Pq, cw], [acc])
                                for j in range(1, 5):
                                    V(lambda e, c=c, o=o, n=n, j=j: e.scalar_tensor_tensor(
                                        out=acc[:, 0:n], in0=Pq[:, c, o - 2 + j:o - 2 + j + n], scalar=cw[:, c, j:j + 1],
                                        in1=acc[:, 0:n], op0=ALU.mult, op1=ALU.add), [Pq, cw, acc], [acc])
                                if c >= 8:
                                    A(lambda e, c=c, o=o, n=n: e.activation(out=Pq[:, c, o:o + n], in_=acc[:, 0:n], func=AF.Silu),
                                      [acc], [Pq])
                                    continue
                                A(lambda e, n=n: e.activation(out=acc[:, 0:n], in_=acc[:, 0:n], func=AF.Silu), [acc], [acc])
                                for b0 in range(0, n, 512):
                                    bn = min(512, n - b0)
                                    V(lambda e, b0=b0, bn=bn: e.tensor_tensor(out=sq[:, 0:bn], in0=acc[:, b0:b0 + bn],
                                                                              in1=acc[:, b0:b0 + bn], op=ALU.mult), [acc], [sq])
                                    T(lambda e, bn=bn: e.matmul(out=pbf(0)[:, 0:bn], lhsT=ones_b, rhs=sq[:, 0:bn], start=True, stop=True),
                                      [sq, cstb], [PB[0]])
                                    A(lambda e, bn=bn: e.activation(out=rin[:, 0:bn], in_=pbf(0)[:, 0:bn], func=AF.Sqrt, bias=EPS),
                                      [PB[0]], [rin])
                                    V(lambda e, bn=bn: e.reciprocal(out=rin[:, 0:bn], in_=rin[:, 0:bn]), [rin], [rin])
                                    sc = (128.0 ** -0.5) if c < 4 else 1.0
                                    V(lambda e, c=c, o=o, b0=b0, bn=bn, sc=sc: e.scalar_tensor_tensor(
                                        out=Pq[:, c, o + b0:o + b0 + bn], in0=acc[:, b0:b0 + bn], scalar=sc, in1=rin[:, 0:bn],
                                        op0=ALU.mult, op1=ALU.mult), [acc, rin], [Pq])
                    ck(0.6)
                    oacc = fw.sb("oacc", [128, NT_L, 4, 128])
                    with fw.scope():
                        dn_scan(fw, nc, PS, PB, pbf, pbb, cst, cstb, Pq, CO, BET, NBET, GG, oacc)
                    ck(0.7)
                    dng = fw.sb("dng", [128, 128])
                    fw.dma("sp", dng[:], dng_d.partition_broadcast(128), dng, None)
                    with fw.scope():
                        head_norm_gate(fw, PB, pbb, cstb, oacc, Zg, dng, mixT, 0, False)

                with fw.scope():
                    gqk = fw.sb("gqk", [64, 8, TC + TL], BF16)
                    gv = fw.sb("gv", [128, 18, 512], BF16)
                    Rg = fw.sb("Rg", [128, NT_L, 512], BF16)
                    LRT = fw.sb("LRT", [16, 2, TC + TL], BF16)
                    with fw.scope():
                        hT = fw.sb("hTg", [128, 8, TC + TL], BF16)
                        xts = [fw.sb("xtg%d" % i, [128, 1024]) for i in range(2)]
                        wk = (fw.sb("junkg", [128, 1024], BF16), fw.sb("ssqg", [128, 1]), fw.sb("xng", [128, 1024], BF16))
                        make_hT(hT, 0, ctx_d[s], NT_C, G1, 0, 2, xts, wk)
                        make_hT(hT, TC, x_d[s], NT_L, G1, 0, s, xts, wk, perm=True)
                        wgl = fw.sb("wgl", [128, 8, 1568], BF16)
                        stg = [fw.sb("stgg%d" % i, [128, 8, 128]) for i in range(2)]
                        load_w_bf16(wgl, win_v[:, :, 2048:3584], 1536, stg, 128)
                        load_w_bf16(wgl, win_v[:, :, 3600:3632], 32, stg, 128, 1536)
                        nb = 0
                        for (t0, tn) in [(0, 256)] + [(TC + b * 512, 512) for b in range(4)]:
                            for g in range(10):
                                bk = nb % 2
                                nb += 1
                                if g < 8:
                                    cs, m = slice(g * 64, (g + 1) * 64), 64
                                else:
                                    cs, m = slice(1536 + (g - 8) * 16, 1536 + (g - 7) * 16), 16
                                for k in range(8):
                                    T(lambda e, bk=bk, k=k, cs=cs, m=m, t0=t0, tn=tn: e.matmul(
                                        out=pbf(bk)[0:m, 0:tn], lhsT=wgl[:, k, cs], rhs=hT[:, k, t0:t0 + tn],
                                        start=(k == 0), stop=(k == 7)), [wgl, hT], [PB[bk]])
                                if g < 4:
                                    A(lambda e, bk=bk, g=g, t0=t0, tn=tn: e.mul(out=gqk[:, g, t0:t0 + tn], in_=pbf(bk)[0:64, 0:tn], mul=0.125),
                                      [PB[bk]], [gqk])
                                elif g < 8:
                                    V(lambda e, bk=bk, g=g, t0=t0, tn=tn: e.tensor_copy(out=gqk[:, g, t0:t0 + tn], in_=pbf(bk)[0:64, 0:tn]),
                                      [PB[bk]], [gqk])
                                else:
                                    V(lambda e, bk=bk, g=g, t0=t0, tn=tn: e.tensor_copy(out=LRT[:, g - 8, t0:t0 + tn], in_=pbf(bk)[0:16, 0:tn]),
                                      [PB[bk]], [LRT])
                        for i in range(18):
                            t0 = i * 128
                            for k in range(8):
                                T(lambda e, k=k, t0=t0: e.matmul(out=pbf(2), lhsT=hT[:, k, t0:t0 + 128], rhs=wgl[:, k, 512:1024],
                                                                 start=(k == 0), stop=(k == 7)), [wgl, hT], [PB[2]])
                            V(lambda e, i=i: e.tensor_copy(out=gv[:, i, :], in_=pbf(2)), [PB[2]], [gv])
                            if i >= 2:
                                for k in range(8):
                                    T(lambda e, k=k, t0=t0: e.matmul(out=pbf(3), lhsT=hT[:, k, t0:t0 + 128], rhs=wgl[:, k, 1024:1536],
                                                                     start=(k == 0), stop=(k == 7)), [wgl, hT], [PB[3]])
                                A(lambda e, i=i: e.activation(out=Rg[:, i - 2, :], in_=pbf(3), func=AF.Silu), [PB[3]], [Rg])
                    ck(1.2)
                    oacc = fw.sb("oaccg", [128, NT_L, 4, 128])
                    with fw.scope():
                        gla_scan(fw, PB, pbf, pbb, cst, cstb, gqk, gv, LRT, wa2_d, ba_d, oacc)
                    ck(1.3)
                    glg = fw.sb("glg", [128, 128])
                    fw.dma("sp", glg[:], glg_d.partition_broadcast(128), glg, None)
                    with fw.scope():
                        head_norm_gate(fw, PB, pbb, cstb, oacc, Rg, glg, mixT, 4, True)
                ck(1.4)
                with fw.scope():
                    wo = fw.sb("wo", [128, 8, 1024], BF16)
                    stg = [fw.sb("stgo%d" % i, [128, 8, 128]) for i in range(2)]
                    load_w_bf16(wo, wout_d.rearrange("(k p) c -> p k c", p=128), 1024, stg, 128)
                    g1B = fw.sb("g1B", [128, 1024])
                    fw.dma("sp", g1B[:], modrow_d[s:s + 1, 2048:3072].partition_broadcast(128), g1B, modrow)
                    xts = [fw.sb("xto%d" % i, [128, 1024]) for i in range(2)]
                    x1ts = [fw.sb("x1t%d" % i, [128, 1024]) for i in range(2)]
                    for i in range(NT_L):
                        xt = xts[i % 2]
                        x1t = x1ts[i % 2]
                        fw.dma("sp", xt[:], x_d[s][i * 128:(i + 1) * 128, :], xt, None)
                        for hf in range(2):
                            bk = 2 * (i % 2) + hf
                            for k in range(8):
                                T(lambda e, k=k, i=i, hf=hf, bk=bk: e.matmul(out=pbf(bk), lhsT=mixT[:, k, i * 128:(i + 1) * 128],
                                                                             rhs=wo[:, k, hf * 512:(hf + 1) * 512],
                                                                             start=(k == 0), stop=(k == 7)), [mixT, wo], [PB[bk]])
                            hs = slice(hf * 512, (hf + 1) * 512)
                            V(lambda e, bk=bk, hs=hs, x1t=x1t: e.tensor_tensor(out=x1t[:, hs], in0=pbf(bk), in1=g1B[:, hs], op=ALU.mult),
                              [PB[bk], g1B], [x1t])
                            V(lambda e, hs=hs, x1t=x1t, xt=xt: e.tensor_tensor(out=x1t[:, hs], in0=x1t[:, hs], in1=xt[:, hs], op=ALU.add),
                              [x1t, xt], [x1t])
                        fw.dma("pool", x1_d[s * TL + i * 128:s * TL + (i + 1) * 128, :], x1t[:], x1buf, x1t)
                ck(1.45)
                if stage < 2:
                    if s == 0:
                        with fw.scope():
                            dts = [fw.sb("dtmp%d" % i, [128, TL]) for i in range(2)]
                            for ch in range(8):
                                dt_ = dts[ch % 2]
                                V(lambda e, ch=ch, dt_=dt_: e.tensor_copy(out=dt_[:], in_=mixT[:, ch, :]), [mixT], [dt_])
                                fw.dma("sp", dbg_d[:, ch, :], dt_[:], outb, dt_)
                        fw.dead = True
                    continue
          except StopBuild:
            break
        fw.dead = False
        if stage >= 3:
            utscr = fw.view("utscr")
            vbscr = fw.view("vbscr")
            with fw.scope():
                uview = u_d.rearrange("(i j) d -> j i d", j=128)
                vview = v_d.rearrange("(i j) d -> j i d", j=128)
                ubs = [fw.sb("ub%d" % i, [128, 1024]) for i in range(2)]
                vbs = [fw.sb("vb%d" % i, [128, 1024]) for i in range(2)]
                ubb = [fw.sb("ubb%d" % i, [128, 1024], BF16) for i in range(2)]
                vbb = [fw.sb("vbb%d" % i, [128, 1024], BF16) for i in range(2)]
                utb = [fw.sb("utb%d" % i, [128, 8, 128], BF16) for i in range(2)]
                for j in range(128):
                    q = j % 2
                    fw.dma("sp", ubs[q][:], uview[j], ubs[q], None)
                    fw.dma("act", vbs[q][:], vview[j], vbs[q], None)
                    A(lambda e, q=q: e.copy(out=ubb[q][:], in_=ubs[q][:]), [ubs[q]], [ubb[q]])
                    V(lambda e, q=q: e.tensor_copy(out=vbb[q][:], in_=vbs[q][:]), [vbs[q]], [vbb[q]])
                    for k in range(8):
                        T(lambda e, q=q, k=k: e.transpose(out=pbb(q)[:, k * 128:(k + 1) * 128], in_=ubb[q][:, k * 128:(k + 1) * 128],
                                                          identity=ident_b), [ubb[q], cstb], [PB[q]])
                    V(lambda e, q=q: e.tensor_copy(out=utb[q][:].rearrange("p k i -> p (k i)"), in_=pbb(q)), [PB[q]], [utb[q]])
                    fw.dma("pool", ut_d[j], utb[q][:], utscr, utb[q])
                    fw.dma("pool", vb_d[j], vbb[q][:], vbscr, vbb[q])
            def pk(x):
                if stage == x:
                    fw.dead = True
            pk(3.1)
            with fw.scope():
                wqb = fw.sb("wqb", [128, 8, 2048], BF16)
                keysb = fw.sb("keysb", [128, 16, 128], BF16)
                with fw.scope():
                    stg = [fw.sb("stgp%d" % i, [128, 8, 128]) for i in range(2)]
                    load_w_bf16(wqb, wq_d.rearrange("(k p) c -> p k c", p=128), 2048, stg, 128)
                    keysf = fw.sb("keysf", [128, 16, 128])
                    fw.dma("sp", keysf[:], keysT_d, keysf, None)
                    V(lambda e: e.tensor_copy(out=keysb[:], in_=keysf[:]), [keysf], [keysb])
                g2B = fw.sb("g2B", [128, 1024])
                fingB = fw.sb("fingB", [128, 1024])
                fw.dma("sp", fingB[:], fing_d.partition_broadcast(128), fingB, None)
                h2T = fw.sb("h2T", [128, 8, 256], BF16)
                xts = [fw.sb("xtp%d" % i, [128, 1024]) for i in range(2)]
                xnp = fw.sb("xnp", [128, 1024], BF16)
                wk = (xnp, fw.sb("ssqp", [128, 1]), xnp)
                qT = fw.sb("qT", [128, 16, 256], BF16)
                ssb = fw.sb("ssb", [128, 16, 128])
                top = fw.sb("top", [128, 16, 16])
                idxu = fw.sb("idxu", [128, 8, 16], mybir.dt.uint32)
                wkk = fw.sb("wkk", [128, 128])
                cand = fw.sb("cand", [128, 8, 256])
                wk2 = fw.sb("wk2", [128, 256])
                cv = fw.sb("cv", [128, 8, 16])
                T4s = fw.sb("T4s", [128, 3, 128])
                T4 = fw.sb("T4", [128, 2, 3, 128])
                zz = fw.sb("zz", [128, 8, 16])
                rZ = fw.sb("rZ", [128, 8])
                NSB = 8
                Sreps = [fw.sb("Srep%d" % i, [128, NSB, 128]) for i in range(2)]
                Abs_ = [fw.sb("Ab%d" % i, [128, NSB, 128], BF16) for i in range(2)]
                Bbs_ = [fw.sb("Bb%d" % i, [128, NSB, 128], BF16) for i in range(2)]
                Wsb = fw.sb("Wsb", [128, 128, 256], BF16)
                utl = [fw.sb("utl%d" % i, [128, 2, 8, 128], BF16) for i in range(3)]
                vtl = [fw.sb("vtl%d" % i, [128, 2, 1024], BF16) for i in range(3)]
                gef = [fw.sb("gef%d" % i, [128, 256], BF16) for i in range(2)]
                gw = [fw.sb("gw%d" % i, [128, 256], BF16) for i in range(2)]
                x2ap = cand[:].rearrange("p h c -> p (h c)")[:, 0:1024]
                x2 = cand
                ssbuf = [fw.view("ssbuf%d" % i) for i in range(2)]
                tv = top[:].rearrange("p (h q) k -> p h q k", q=2)
                bk3 = lambda ap: ap.unsqueeze(2).to_broadcast([128, NSB, 128])
                iota_b = cstb[:, 2, :].unsqueeze(1).to_broadcast([128, NSB, 128])
                t4 = lambda q: T4s[:, q, :].rearrange("p (h k) -> p h k", k=16)
                b16 = lambda ap: ap.to_broadcast([128, 8, 16])
                for blk in range(NSEQ * NT_L // 2):
                    sq = (blk * 2) // NT_L
                    if blk % (NT_L // 2) == 0:
                        fw.dma("sp", g2B[:], modrow_d[sq:sq + 1, 5120:6144].partition_broadcast(128), g2B, modrow)
                    make_hT(h2T, 0, x1_d[blk * 256:(blk + 1) * 256, :], 2, G2, 24, sq, xts, wk, srcbuf=x1buf)
                    for hp in range(16):
                        bk = hp % 2
                        for k in range(8):
                            T(lambda e, bk=bk, hp=hp, k=k: e.matmul(out=pbf(bk)[:, 0:256], lhsT=wqb[:, k, hp * 128:(hp + 1) * 128],
                                                                    rhs=h2T[:, k, :], start=(k == 0), stop=(k == 7)), [wqb, h2T], [PB[bk]])
                        if bk:
                            A(lambda e, bk=bk, hp=hp: e.copy(out=qT[:, hp, :], in_=pbf(bk)[:, 0:256]), [PB[bk]], [qT])
                        else:
                            V(lambda e, bk=bk, hp=hp: e.tensor_copy(out=qT[:, hp, :], in_=pbf(bk)[:, 0:256]), [PB[bk]], [qT])
                    sc = ssbuf[blk % 2]
                    for tl in range(2):
                        gt = blk * 2 + tl
                        for g in range(4):
                            bk = 2 + g % 2
                            for q in range(4):
                                hp = g * 4 + q
                                T(lambda e, bk=bk, hp=hp, q=q, tl=tl: e.matmul(out=pbf(bk)[:, q * 128:(q + 1) * 128],
                                                                               lhsT=qT[:, hp, tl * 128:(tl + 1) * 128],
                                                                               rhs=keysb[:, hp, :], start=True, stop=True), [qT, keysb], [PB[bk]])
                            A(lambda e, bk=bk, g=g: e.copy(out=ssb[:, g * 4:(g + 1) * 4, :].rearrange("p a i -> p (a i)"), in_=pbf(bk)),
                              [PB[bk]], [ssb])
                        fw.dma("pool", ss_d[1, :, gt * 128:(gt + 1) * 128, :].rearrange("h t i -> t h i"),
                               ssb[:].rearrange("p (h q) i -> p h q i", q=2)[:, :, 1, :], sc, ssb)
                        for hp in range(16):
                            V(lambda e, hp=hp: e.max(out=top[:, hp, 0:8], in_=ssb[:, hp, :]), [ssb], [top])
                            V(lambda e, hp=hp: e.match_replace(out=wkk[:], in_to_replace=top[:, hp, 0:8], in_values=ssb[:, hp, :],
                                                               imm_value=-1e30), [ssb, top], [wkk])
                            V(lambda e, hp=hp: e.max(out=top[:, hp, 8:16], in_=wkk[:]), [wkk], [top])
                            if hp % 2 == 0:
                                for o8 in (0, 8):
                                    V(lambda e, hp=hp, o8=o8: e.max_index(out=idxu[:, hp // 2, o8:o8 + 8], in_max=top[:, hp, o8:o8 + 8],
                                                                          in_values=ssb[:, hp, :]), [ssb, top], [idxu])
                        V(lambda e: e.tensor_tensor(out=cand[:].rearrange("p h (a b) -> p h a b", b=16),
                                                    in0=tv[:, :, 0, :].unsqueeze(3).to_broadcast([128, 8, 16, 16]),
                                                    in1=tv[:, :, 1, :].unsqueeze(2).to_broadcast([128, 8, 16, 16]), op=ALU.add), [top], [cand])
                        for h in range(8):
                            V(lambda e, h=h: e.max(out=cv[:, h, 0:8], in_=cand[:, h, :]), [cand], [cv])
                            V(lambda e, h=h: e.match_replace(out=wk2[:], in_to_replace=cv[:, h, 0:8], in_values=cand[:, h, :],
                                                             imm_value=-1e30), [cand, cv], [wk2])
                            V(lambda e, h=h: e.max(out=cv[:, h, 8:16], in_=wk2[:]), [wk2], [cv])
                        V(lambda e: e.tensor_copy(out=t4(0), in_=idxu[:]), [idxu], [T4s])
                        V(lambda e: e.tensor_scalar(out=t4(1), in0=tv[:, :, 0, :], scalar1=-1.0, scalar2=-1e-5, op0=ALU.mult, op1=ALU.add),
                          [top], [T4s])
                        V(lambda e: e.tensor_tensor(out=t4(1), in0=t4(1), in1=b16(cv[:, :, 15:16]), op=ALU.add), [T4s, cv], [T4s])
                        V(lambda e: e.tensor_tensor(out=zz[:], in0=cv[:], in1=b16(cv[:, :, 0:1]), op=ALU.subtract), [cv], [zz])
                        A(lambda e: e.activation(out=zz[:], in_=zz[:], func=AF.Exp), [zz], [zz])
                        V(lambda e: e.tensor_reduce(out=rZ[:], in_=zz[:], axis=AX.X, op=ALU.add), [zz], [rZ])
                        V(lambda e: e.reciprocal(out=rZ[:], in_=rZ[:]), [rZ], [rZ])
                        V(lambda e: e.tensor_tensor(out=t4(2), in0=tv[:, :, 0, :], in1=b16(cv[:, :, 0:1]), op=ALU.subtract), [top, cv], [T4s])
                        A(lambda e: e.activation(out=t4(2), in_=t4(2), func=AF.Exp), [T4s], [T4s])
                        V(lambda e: e.tensor_tensor(out=t4(2), in0=t4(2), in1=b16(rZ[:].unsqueeze(2)), op=ALU.mult), [T4s, rZ], [T4s])
                        for q in range(3):
                            T(lambda e, q=q: e.transpose(out=pbf(2)[:, q * 128:(q + 1) * 128], in_=T4s[:, q, :], identity=ident_f),
                              [T4s, cst], [PB[2]])
                        A(lambda e, tl=tl: e.copy(out=T4[:, tl].rearrange("p q t -> p (q t)"), in_=pbf(2)[:, 0:384]), [PB[2]], [T4])
                    pk(3.2)
                    pending = []
                    for sbk in range(256 // NSB):
                        tl = (sbk * NSB) // 128
                        tin = (sbk * NSB) % 128
                        ta = blk * 256 + sbk * NSB
                        tsl = slice(tin, tin + NSB)
                        Srep, Ab, Bb = Sreps[sbk % 2], Abs_[sbk % 2], Bbs_[sbk % 2]
                        fw.dma("sp", Srep[:], ss_d[1, :, ta:ta + NSB, :].unsqueeze(1).to_broadcast([8, 16, NSB, 128]), Srep, sc)
                        V(lambda e, tsl=tsl, tl=tl, Ab=Ab: e.tensor_tensor(out=Ab[:], in0=iota_b, in1=bk3(T4[:, tl, 0, tsl]), op=ALU.is_equal),
                          [cstb, T4], [Ab])
                        G(lambda e, tsl=tsl, tl=tl, Ab=Ab: e.tensor_tensor(out=Ab[:], in0=Ab[:], in1=bk3(T4[:, tl, 2, tsl]), op=ALU.mult),
                          [Ab, T4], [Ab])
                        V(lambda e, tsl=tsl, tl=tl, Srep=Srep, Bb=Bb: e.tensor_tensor(out=Bb[:], in0=Srep[:], in1=bk3(T4[:, tl, 1, tsl]), op=ALU.is_ge),
                          [Srep, T4], [Bb])
                        A(lambda e, Srep=Srep: e.activation(out=Srep[:], in_=Srep[:], func=AF.Exp), [Srep], [Srep])
                        G(lambda e, Srep=Srep, Bb=Bb: e.tensor_tensor(out=Bb[:], in0=Bb[:], in1=Srep[:], op=ALU.mult), [Bb, Srep], [Bb])
                        for fn_ in pending:
                            fn_()
                        pending = []
                        for t in range(NSB):
                            bk = 2 + 2 * (sbk % 2) + (t // 4) % 2
                            T(lambda e, bk=bk, t=t, Ab=Ab, Bb=Bb: e.matmul(out=pbf(bk)[:, (t % 4) * 128:(t % 4 + 1) * 128], lhsT=Ab[:, t, :],
                                                                           rhs=Bb[:, t, :], start=True, stop=True), [Ab, Bb], [PB[bk]])
                            if t % 4 == 3:
                                t0 = sbk * NSB + t - 3
                                dst = Wsb[:, :, t0:t0 + 4].rearrange("p j t -> p t j")
                                src = pbf(bk).rearrange("p (t j) -> p t j", j=128)
                                if (t // 4) % 2:
                                    pending.append(lambda dst=dst, src=src, bk=bk: A(lambda e: e.copy(out=dst, in_=src), [PB[bk]], [Wsb]))
                                else:
                                    pending.append(lambda dst=dst, src=src, bk=bk: V(lambda e: e.tensor_copy(out=dst, in_=src), [PB[bk]], [Wsb]))
                    for fn_ in pending:
                        fn_()
                    pending = []
                    pk(3.3)
                    for jj in range(64):
                        ut2, vt2 = utl[jj % 3], vtl[jj % 3]
                        fw.dma("sp", ut2[:], ut_d[2 * jj:2 * jj + 2].rearrange("j p k i -> p j k i"), ut2, utscr)
                        fw.dma("sp", vt2[:], vb_d[2 * jj:2 * jj + 2].rearrange("j p d -> p j d"), vt2, vbscr)
                        for jl in range(2):
                            j = 2 * jj + jl
                            bk = j % 2
                            for k in range(8):
                                T(lambda e, bk=bk, k=k, ut2=ut2, jl=jl: e.matmul(out=pbf(bk)[:, 0:256], lhsT=ut2[:, jl, k, :], rhs=h2T[:, k, :],
                                                                                 start=(k == 0), stop=(k == 7)), [ut2, h2T], [PB[bk]])
                            ge_, gw_ = gef[j % 2], gw[j % 2]
                            A(lambda e, bk=bk, ge_=ge_: e.activation(out=ge_[:], in_=pbf(bk)[:, 0:256], func=AF.Gelu), [PB[bk]], [ge_])
                            V(lambda e, ge_=ge_, gw_=gw_, j=j: e.tensor_tensor(out=gw_[:], in0=ge_[:], in1=Wsb[:, j, :], op=ALU.mult),
                              [ge_, Wsb], [gw_])
                            for tl in range(2):
                                for hf in range(2):
                                    ob = 4 + tl * 2 + hf
                                    T(lambda e, hf=hf, tl=tl, ob=ob, gw_=gw_, vt2=vt2, jl=jl, j=j: e.matmul(
                                        out=pbf(ob), lhsT=gw_[:, tl * 128:(tl + 1) * 128], rhs=vt2[:, jl, hf * 512:(hf + 1) * 512],
                                        start=(j == 0), stop=(j == 127)), [gw_, vt2], [PB[ob]])
                    pk(3.4)
                    for tl in range(2):
                        xt = xts[tl]
                        for hf in range(2):
                            hs = slice(hf * 512, (hf + 1) * 512)
                            ob = 4 + tl * 2 + hf
                            V(lambda e, ob=ob, hs=hs: e.tensor_tensor(out=x2ap[:, hs], in0=pbf(ob), in1=g2B[:, hs], op=ALU.mult),
                              [PB[ob], g2B], [x2])
                        V(lambda e, xt=xt: e.tensor_tensor(out=x2ap, in0=x2ap, in1=xt[:], op=ALU.add), [x2, xt], [x2])
                        junk, ssq, xn = wk
                        A(lambda e: e.activation(out=junk[:], in_=x2ap, func=AF.Square, accum_out=ssq[:]), [x2], [junk, ssq])
                        A(lambda e: e.activation(out=ssq[:], in_=ssq[:], func=AF.Sqrt, scale=1.0 / 1024, bias=EPS), [ssq], [ssq])
                        V(lambda e: e.reciprocal(out=ssq[:], in_=ssq[:]), [ssq], [ssq])
                        V(lambda e: e.scalar_tensor_tensor(out=x2ap, in0=x2ap, scalar=ssq[:, 0:1], in1=fingB[:], op0=ALU.mult, op1=ALU.mult),
                          [x2, ssq, fingB], [x2])
                        ti = (blk * 2 + tl) % NT_L
                        fw.dma("pool", out_d[sq][ti * 128:(ti + 1) * 128, :], x2ap, outb, x2)
                    pk(3.6)
        fw.dead = False
        fw.finish([outb], "sp")
        fw.barrier()
    return nc


def _layout(inp, core):
    b0 = 2 * core
    f = lambda a: np.ascontiguousarray(a, dtype=np.float32)
    csel = np.stack([inp["c"][b0], inp["c"][b0 + 1], inp["c_ctx"]])
    w_in = inp["w_in"][0]
    w_in_r = np.concatenate([w_in[:, 0:2048], w_in[:, 2064:3600], w_in[:, 2048:2064], w_in[:, 3600:3632]], axis=1)
    return {
        "x": f(inp["x"][b0:b0 + 2]), "ctx": f(inp["ctx"][b0:b0 + 2]),
        "cT": f(csel.reshape(3, 8, 128).transpose(2, 1, 0)),
        "w_ada": f(inp["w_ada"][0]), "b_adaT": f(inp["b_ada"][0].reshape(48, 128).T),
        "n1gT": f(inp["norm1_g"][0].reshape(8, 128).T), "n2gT": f(inp["norm2_g"][0].reshape(8, 128).T),
        "final_g": f(inp["final_g"].reshape(1, 1024)), "w_in": f(w_in_r),
        "convT": f(inp["conv_w"][0].T.reshape(12, 128, 5).transpose(1, 0, 2)),
        "a_log": f(inp["dn_a_log"][0].reshape(1, 8)), "dt_bias": f(inp["dn_dt_bias"][0].reshape(1, 8)),
        "dn_g": f(inp["dn_norm_g"][0].reshape(1, 128)), "gla_g": f(inp["gla_norm_g"][0].reshape(1, 128)),
        "wa2": f(inp["gla_wa2"][0]), "ba": f(inp["gla_ba"][0].reshape(2, 1, 256)),
        "w_out": f(inp["w_out"][0]), "wq": f(inp["peer_wq"][0]),
        "keysT": f(inp["peer_keys"][0].reshape(16, 128, 128).transpose(2, 0, 1)),
        "peer_u": f(inp["peer_u"][0]), "peer_v": f(inp["peer_v"][0]),
        "consts": make_consts(),
    }


def kernel(**inputs):
    inp = {k: np.asarray(v) for k, v in inputs.items()}
    nc = build_program(9)
    in_maps = [_layout(inp, c) for c in range(8)]
    res = run_bass_kernel_spmd(nc, in_maps, core_ids=list(range(8)))
    return np.concatenate([r["out"] for r in res.results], axis=0).astype(np.float32)
```

```python
import contextlib
import numpy as np
import concourse.bass as bass
import concourse.mybir as mybir
from concourse.bass_utils import run_bass_kernel_spmd

F32 = mybir.dt.float32
BF16 = mybir.dt.bfloat16
ALU = mybir.AluOpType
AF = mybir.ActivationFunctionType
AX = mybir.AxisListType

NEG = -30000.0
EPS = 1e-6


class Buf:
    __slots__ = ("name", "t", "wev", "revs", "dsem", "dcnt", "pre")

    def __init__(self, name, t=None):
        self.name = name
        self.t = t
        self.wev = []
        self.revs = []
        self.dsem = None
        self.dcnt = 0
        self.pre = []

    def __getitem__(self, k):
        return self.t[k]


class Eng:
    def __init__(self, name, h, sem):
        self.name = name
        self.h = h
        self.sem = sem
        self.cnt = 0
        self.known = {}


class FW:
    def __init__(self, nc, stack):
        self.nc = nc
        self.top = stack
        self.stack = stack
        self.engs = {}
        self.dsems = []
        for name, h in (("pe", nc.tensor), ("act", nc.scalar), ("dve", nc.vector),
                        ("pool", nc.gpsimd), ("sp", nc.sync)):
            sem = stack.enter_context(nc.semaphore("s_" + name))
            self.engs[name] = Eng(name, h, sem)
        self.ninst = 0

    @contextlib.contextmanager
    def scope(self):
        old = self.stack
        with contextlib.ExitStack() as st:
            self.stack = st
            try:
                yield
            finally:
                self.barrier()
                self.stack = old

    def sb(self, name, shape, dt=F32):
        self.nsb = getattr(self, "nsb", 0) + 1
        name = "sb%d_%s" % (self.nsb, name)
        t = self.stack.enter_context(self.nc.sbuf_tensor(name, list(shape), dt))
        return Buf(name, t)

    def view(self, name, t=None):
        return Buf(name, t)

    def _waits(self, e, reads, writes, skip=None):
        need = {}
        for b in reads:
            for (s, v) in b.wev:
                if need.get(s, 0) < v:
                    need[s] = v
        for b in writes:
            for (s, v) in b.wev:
                if need.get(s, 0) < v:
                    need[s] = v
            for (s, v) in b.revs:
                if need.get(s, 0) < v:
                    need[s] = v
        for s, v in need.items():
            if s is skip or (e.name == "pe" and s is e.sem):
                continue
            if e.known.get(s, 0) < v:
                e.h.wait_ge(s, v)
                e.known[s] = v

    def op(self, eng, fn, reads=(), writes=()):
        if getattr(self, "dead", False):
            return None
        e = self.engs[eng]
        self._waits(e, reads, writes)
        ins = fn(e.h)
        e.cnt += 1
        ins.then_inc(e.sem, 1)
        self.ninst += 1
        ev = (e.sem, e.cnt)
        for b in reads:
            if len(b.revs) > 24:
                b.revs = b.revs[-12:] + self._maxev(b.revs[:-12])
            b.revs.append(ev)
        for b in writes:
            b.wev = [ev]
            b.revs = []
        return ins

    @staticmethod
    def _maxev(evs):
        d = {}
        for (s, v) in evs:
            if d.get(s, (None, 0))[1] < v:
                d[s] = (s, v)
        return list(d.values())

    def dma(self, q, out_ap, in_ap, dst, src, **kw):
        if getattr(self, "dead", False):
            return None
        e = self.engs[q]
        reads = [src] if src is not None else []
        if dst.dsem is None:
            dst.dsem = self.top.enter_context(self.nc.semaphore("d%d_%s" % (len(self.dsems), dst.name)))
            self.dsems.append(dst)
        evs = [ev for ev in dst.wev if ev[0] is not dst.dsem] + list(dst.revs)
        if evs:
            dst.pre = evs
        else:
            evs = dst.pre
        for (sm, v) in evs:
            if e.known.get(sm, 0) < v:
                e.h.wait_ge(sm, v)
                e.known[sm] = v
        self._waits(e, reads, [], skip=dst.dsem)
        ins = e.h.dma_start(out=out_ap, in_=in_ap, **kw)
        self.ninst += 1
        dst.dcnt += 16
        ins.then_inc(dst.dsem, 16)
        ev = (dst.dsem, dst.dcnt)
        if src is not None:
            src.revs.append(ev)
        dst.wev = [ev]
        dst.revs = []
        return ins

    def barrier(self):
        for e in self.engs.values():
            for f in self.engs.values():
                if f is e or f.cnt == 0:
                    continue
                if e.known.get(f.sem, 0) < f.cnt:
                    e.h.wait_ge(f.sem, f.cnt)
                    e.known[f.sem] = f.cnt
            for b in self.dsems:
                if b.dcnt and e.known.get(b.dsem, 0) < b.dcnt:
                    e.h.wait_ge(b.dsem, b.dcnt)
                    e.known[b.dsem] = b.dcnt

    def finish(self, bufs, eng="sp"):
        self._waits(self.engs[eng], bufs, [])


def make_consts():
    p = np.arange(128)[:, None]
    f = np.arange(128)[None, :]
    c = np.zeros((128, 15, 128), np.float32)
    c[:, 0] = (p == f)
    c[:, 1] = (p <= f)
    c[:, 2] = (p >= f)
    c[:, 3] = np.where(p > f, 0.0, NEG)
    c[:, 4] = np.where(p < f, 0.0, NEG)
    c[:, 5] = np.where(p <= f, 0.0, NEG)
    c[:, 6] = np.where(p >= f, 0.0, NEG)
    c[:, 7] = (p <= f)
    c[:, 8] = (p >= f)
    c[:, 9] = 1.0
    c[:, 10] = -(p <= f).astype(np.float32) / 16.0
    c[:, 11] = -(p >= f).astype(np.float32) / 16.0
    c[:, 12] = f + 0.0 * p
    c[:, 13] = -(p <= f).astype(np.float32)
    c[:, 14] = -(p >= f).astype(np.float32)
    return c


def dn_scan(fw, nc, PS, PB, pbf, pbb, cst, cstb, Pq, CO, BET, NBET, GG, oacc):
    import itertools
    V = lambda fn, r, w: fw.op("dve", fn, r, w)
    A = lambda fn, r, w: fw.op("act", fn, r, w)
    T = lambda fn, r, w: fw.op("pe", fn, r, w)
    ident_b = cstb[:, 0, :]
    ident_f = cst[:, 0, :]
    ones_f = cst[:, 9, :]
    r4 = lambda ap: ap.rearrange("p (h t) -> p h t", t=128)
    bc = lambda ap: ap.unsqueeze(2).to_broadcast([128, 4, 128])
    V(lambda e: e.memset(oacc[:].rearrange("p a h t -> p (a h t)"), 0.0), [], [oacc])

    def chain(d, b):
        n_ = lambda nm, sh, dt=F32: fw.sb("d%d%s" % (d, nm), sh, dt)
        S = n_("S", [128, 4, 128]); Sbf = n_("Sbf", [128, 4, 128], BF16)
        kv = n_("kv", [128, 8, 128], BF16)
        Gb = n_("Gb", [128, 4, 128])
        sml = n_("sml", [128, 4, 4]); gct = n_("gct", [128, 8])
        E1 = n_("E1", [128, 4, 128]); E2 = E1; E3 = E1
        Y = n_("Y", [128, 4, 128]); Z = n_("Z", [128, 4, 128]); Rf = n_("Rf", [128, 4, 128])
        R = n_("R", [128, 4, 128], BF16); At = n_("At", [128, 4, 128], BF16); qg = n_("qg", [128, 4, 128], BF16)
        kbg = n_("kbg", [128, 4, 128], BF16); kd = n_("kd", [128, 4, 128], BF16); vb = n_("vb", [128, 4, 128], BF16)
        Usb = E1; WT = n_("WT", [128, 4, 128], BF16); vnew = n_("vn", [128, 4, 128], BF16)
        b0, b1, b2, b3 = b
        Cum = cst[:, 1 + d, :]
        nCum = cst[:, 13 + d, :]
        NM1 = cst[:, 3 + d, :].unsqueeze(1).to_broadcast([128, 4, 128])
        NM2 = cst[:, 5 + d, :].unsqueeze(1).to_broadcast([128, 4, 128])
        V(lambda e: e.memset(S[:], 0.0), [], [S])
        V(lambda e: e.memset(Sbf[:], 0.0), [], [Sbf])
        order = [(0, i) for i in ((0, 1) if d == 0 else (1, 0))] + \
                [(1, i) for i in (range(16) if d == 0 else range(15, -1, -1))]
        H = [slice(h * 128, (h + 1) * 128) for h in range(4)]
        for (seg, i) in order:
            gi = i if seg == 0 else 2 + i
            c0 = CO[seg] + i * 128
            g4 = GG[:, gi, d * 4:d * 4 + 4]
            b4 = BET[:, gi, d * 4:d * 4 + 4]
            nb4 = NBET[:, gi, d * 4:d * 4 + 4]
            T(lambda e: e.matmul(out=pbf(b0)[:, 0:4], lhsT=Cum, rhs=g4, start=True, stop=True), [cst, GG], [PB[b0]])
            T(lambda e: e.matmul(out=pbf(b0)[:, 4:8], lhsT=ones_f, rhs=g4, start=True, stop=True), [cst, GG], [PB[b0]])
            V(lambda e: e.tensor_copy(out=Gb[:], in_=bc(g4)), [GG], [Gb])
            A(lambda e: e.copy(out=gct[:], in_=pbf(b0)[:, 0:8]), [PB[b0]], [gct])
            yield
            for h in range(4):
                kT = Pq[:, 4 + h, c0:c0 + 128]
                qT = Pq[:, h, c0:c0 + 128]
                T(lambda e, kT=kT, h=h: e.matmul(out=pbf(b1)[:, H[h]], lhsT=kT, rhs=kT, start=True, stop=True), [Pq], [PB[b1]])
                T(lambda e, kT=kT, qT=qT, h=h: e.matmul(out=pbf(b2)[:, H[h]], lhsT=kT, rhs=qT, start=True, stop=True), [Pq], [PB[b2]])
                T(lambda e, h=h: e.matmul(out=pbf(b3)[:, H[h]], lhsT=Cum, rhs=Gb[:, h, :], start=True, stop=False), [cst, Gb], [PB[b3]])
                T(lambda e, h=h: e.matmul(out=pbf(b3)[:, H[h]], lhsT=Gb[:, h, :], rhs=nCum, start=False, stop=True), [cst, Gb], [PB[b3]])
            A(lambda e: e.activation(out=sml[:, 0, :], in_=gct[:, 0:4], func=AF.Exp), [gct], [sml])
            V(lambda e: e.tensor_tensor(out=sml[:, 1, :], in0=gct[:, 4:8], in1=gct[:, 0:4], op=ALU.subtract), [gct], [sml])
            A(lambda e: e.activation(out=sml[:, 1, :], in_=sml[:, 1, :], func=AF.Exp), [sml], [sml])
            A(lambda e: e.activation(out=sml[:, 2, :], in_=gct[:, 4:8], func=AF.Exp), [gct], [sml])
            V(lambda e: e.tensor_tensor(out=sml[:, 3, :], in0=sml[:, 0, :], in1=b4, op=ALU.mult), [sml, BET], [sml])
            yield
            V(lambda e: e.scalar_tensor_tensor(out=E1[:], in0=r4(pbf(b3)), scalar=0.0, in1=NM1, op0=ALU.min, op1=ALU.add),
              [PB[b3], cst], [E1])
            for h in range(4):
                T(lambda e, h=h: e.matmul(out=pbf(b3)[:, H[h]], lhsT=Gb[:, h, :], rhs=Cum, start=True, stop=False), [cst, Gb], [PB[b3]])
                T(lambda e, h=h: e.matmul(out=pbf(b3)[:, H[h]], lhsT=nCum, rhs=Gb[:, h, :], start=False, stop=True), [cst, Gb], [PB[b3]])
            A(lambda e: e.activation(out=E1[:], in_=E1[:], func=AF.Exp), [E1], [E1])
            V(lambda e: e.tensor_tensor(out=E1[:], in0=r4(pbf(b1)), in1=E1[:], op=ALU.mult), [PB[b1], E1], [E1])
            V(lambda e: e.tensor_tensor(out=Y[:], in0=E1[:], in1=bc(nb4), op=ALU.mult), [E1, NBET], [Y])
            yield
            V(lambda e: e.scalar_tensor_tensor(out=E2[:], in0=r4(pbf(b3)), scalar=0.0, in1=NM2, op0=ALU.min, op1=ALU.add),
              [PB[b3], cst], [E2])
            for h in range(4):
                T(lambda e, h=h: e.matmul(out=pbf(b3)[:, H[h]], lhsT=Gb[:, h, :], rhs=Cum, start=True, stop=True), [cst, Gb], [PB[b3]])
            A(lambda e: e.activation(out=E2[:], in_=E2[:], func=AF.Exp), [E2], [E2])
            V(lambda e: e.tensor_tensor(out=At[:], in0=r4(pbf(b2)), in1=E2[:], op=ALU.mult), [PB[b2], E2], [At])
            for h in range(4):
                T(lambda e, h=h: e.transpose(out=pbb(b0)[:, h * 128:(h + 1) * 128], in_=Pq[:, 4 + h, c0:c0 + 128], identity=ident_b),
                  [Pq, cstb], [PB[b0]])
                T(lambda e, h=h: e.transpose(out=pbb(b0)[:, 512 + h * 128:512 + (h + 1) * 128], in_=Pq[:, 8 + h, c0:c0 + 128],
                                             identity=ident_b), [Pq, cstb], [PB[b0]])
            A(lambda e: e.activation(out=E3[:], in_=r4(pbf(b3)), func=AF.Exp), [PB[b3]], [E3])
            A(lambda e: e.copy(out=kv[:].rearrange("p a t -> p (a t)"), in_=pbb(b0)), [PB[b0]], [kv])
            V(lambda e: e.tensor_tensor(out=qg[:], in0=Pq[:, 0:4, c0:c0 + 128], in1=E3[:], op=ALU.mult), [Pq, E3], [qg])
            yield
            for h in range(4):
                T(lambda e, h=h: e.transpose(out=pbf(b0)[:, H[h]], in_=Y[:, h, :], identity=ident_f), [Y, cst], [PB[b0]])
            A(lambda e: e.copy(out=Z[:], in_=r4(pbf(b0))), [PB[b0]], [Z])
            V(lambda e: e.tensor_tensor(out=Rf[:], in0=Z[:], in1=cst[:, 0, :].unsqueeze(1).to_broadcast([128, 4, 128]), op=ALU.add),
              [Z, cst], [Rf])
            V(lambda e: e.tensor_tensor(out=kbg[:], in0=kv[:, 0:4, :], in1=bc(sml[:, 3, :]), op=ALU.mult), [kv, sml], [kbg])
            V(lambda e: e.tensor_tensor(out=kd[:], in0=kv[:, 0:4, :], in1=bc(sml[:, 1, :]), op=ALU.mult), [kv, sml], [kd])
            V(lambda e: e.tensor_tensor(out=vb[:], in0=kv[:, 4:8, :], in1=bc(b4), op=ALU.mult), [kv, BET], [vb])
            yield
            for lvl in range(1, 7):
                for h in range(4):
                    T(lambda e, h=h: e.matmul(out=pbf(b1)[:, H[h]], lhsT=Z[:, h, :], rhs=Y[:, h, :], start=True, stop=True), [Y, Z], [PB[b1]])
                    if lvl < 6:
                        T(lambda e, h=h: e.matmul(out=pbf(b2)[:, H[h]], lhsT=Y[:, h, :], rhs=Z[:, h, :], start=True, stop=True), [Y, Z], [PB[b2]])
                A(lambda e: e.copy(out=Y[:], in_=r4(pbf(b1))), [PB[b1]], [Y])
                if lvl < 6:
                    V(lambda e: e.tensor_copy(out=Z[:], in_=r4(pbf(b2))), [PB[b2]], [Z])
                for h in range(4):
                    T(lambda e, h=h: e.matmul(out=pbf(b3)[:, H[h]], lhsT=Y[:, h, :], rhs=Rf[:, h, :], start=True, stop=True), [Y, Rf], [PB[b3]])
                V(lambda e: e.tensor_tensor(out=Rf[:], in0=r4(pbf(b3)), in1=Rf[:], op=ALU.add), [PB[b3], Rf], [Rf])
                yield
            A(lambda e: e.copy(out=R[:], in_=Rf[:]), [Rf], [R])
            for h in range(4):
                T(lambda e, h=h: e.matmul(out=pbf(b1)[:, H[h]], lhsT=R[:, h, :], rhs=vb[:, h, :], start=True, stop=True), [R, vb], [PB[b1]])
                T(lambda e, h=h: e.matmul(out=pbf(b2)[:, H[h]], lhsT=kbg[:, h, :], rhs=R[:, h, :], start=True, stop=True), [R, kbg], [PB[b2]])
            A(lambda e: e.copy(out=Usb[:], in_=r4(pbf(b1))), [PB[b1]], [Usb])
            V(lambda e: e.tensor_copy(out=WT[:], in_=r4(pbf(b2))), [PB[b2]], [WT])
            yield
            for h in range(4):
                T(lambda e, h=h: e.matmul(out=pbf(b0)[:, H[h]], lhsT=WT[:, h, :], rhs=Sbf[:, h, :], start=True, stop=True), [WT, Sbf], [PB[b0]])
            V(lambda e: e.tensor_tensor(out=vnew[:], in0=Usb[:], in1=r4(pbf(b0)), op=ALU.subtract), [Usb, PB[b0]], [vnew])
            if seg == 1:
                for h in range(4):
                    T(lambda e, h=h: e.matmul(out=pbf(b3)[:, H[h]], lhsT=qg[:, h, :], rhs=Sbf[:, h, :], start=True, stop=False), [qg, Sbf], [PB[b3]])
                    T(lambda e, h=h: e.matmul(out=pbf(b3)[:, H[h]], lhsT=At[:, h, :], rhs=vnew[:, h, :], start=False, stop=True), [At, vnew], [PB[b3]])
            for h in range(4):
                T(lambda e, h=h: e.matmul(out=pbf(b1)[:, H[h]], lhsT=kd[:, h, :], rhs=vnew[:, h, :], start=True, stop=True), [kd, vnew], [PB[b1]])
            if seg == 1:
                V(lambda e: e.tensor_tensor(out=oacc[:, i], in0=r4(pbf(b3)), in1=oacc[:, i], op=ALU.add), [PB[b3], oacc], [oacc])
            V(lambda e: e.tensor_tensor(out=S[:], in0=S[:], in1=bc(sml[:, 2, :]), op=ALU.mult), [S, sml], [S])
            V(lambda e: e.tensor_tensor(out=S[:], in0=r4(pbf(b1)), in1=S[:], op=ALU.add), [PB[b1], S], [S])
            A(lambda e: e.copy(out=Sbf[:], in_=S[:]), [S], [Sbf])
            yield

    gens = [chain(0, (0, 1, 2, 3)), chain(1, (4, 5, 6, 7))]
    for _ in itertools.zip_longest(*gens):
        pass


def gla_scan(fw, PB, pbf, pbb, cst, cstb, gqk, gv, LRT, wa2_d, ba_d, oacc, extra=None):
    V = lambda fn, r, w: fw.op("dve", fn, r, w)
    A = lambda fn, r, w: fw.op("act", fn, r, w)
    T = lambda fn, r, w: fw.op("pe", fn, r, w)
    ident_b = cstb[:, 0, :]
    r4 = lambda ap: ap.rearrange("p (h t) -> p h t", t=128)
    wa2f = fw.sb("wa2f", [16, 2, 256])
    baf = fw.sb("baf", [1, 2, 256])
    wa2b = fw.sb("wa2b", [16, 2, 256], BF16)
    bab = fw.sb("bab", [1, 2, 256], BF16)
    fw.dma("sp", wa2f[:], wa2_d.rearrange("d r c -> r d c"), wa2f, None)
    fw.dma("sp", baf[:], ba_d.rearrange("d o c -> o d c"), baf, None)
    V(lambda e: e.tensor_copy(out=wa2b[:], in_=wa2f[:]), [wa2f], [wa2b])
    V(lambda e: e.tensor_copy(out=bab[:], in_=baf[:]), [baf], [bab])
    S = fw.sb("gS", [64, 4, 128])
    Sbf = fw.sb("gSbf", [64, 4, 128], BF16)
    sp = fw.sb("gsp", [128, 256])
    bT = fw.sb("gbT", [64, 4, 128])
    eb = fw.sb("geb", [64, 4, 128])
    enb = fw.sb("genb", [64, 4, 128])
    ekd = fw.sb("gekd", [64, 4, 128])
    Qp = fw.sb("gQp", [64, 4, 128], BF16)
    Kp = fw.sb("gKp", [64, 4, 128], BF16)
    KdT = fw.sb("gKdT", [64, 4, 128], BF16)
    Kd = fw.sb("gKd", [128, 4, 64], BF16)
    att = fw.sb("gatt", [128, 4, 128], BF16)
    for d in (0, 1):
        CumS = cst[:, 10 + d, :]
        MK = cst[:, 7 + d, :].unsqueeze(1).to_broadcast([128, 4, 128])
        tl = 127 if d == 0 else 0
        V(lambda e: e.memset(S[:], 0.0), [], [S])
        V(lambda e: e.memset(Sbf[:], 0.0), [], [Sbf])
        order = [(0, i) for i in ((0, 1) if d == 0 else (1, 0))] + \
                [(1, i) for i in (range(16) if d == 0 else range(15, -1, -1))]
        for (seg, i) in order:
            if extra is not None:
                extra()
            gi = i if seg == 0 else 2 + i
            t0 = gi * 128
            T(lambda e: e.matmul(out=pbf(0)[:, 0:256], lhsT=LRT[:, d, t0:t0 + 128], rhs=wa2b[:, d, :], start=True, stop=False),
              [LRT, wa2b], [PB[0]])
            T(lambda e: e.matmul(out=pbf(0)[:, 0:256], lhsT=cstb[0:1, 1, :], rhs=bab[0:1, d, :], start=False, stop=True),
              [cstb, bab], [PB[0]])
            A(lambda e: e.activation(out=sp[:], in_=pbf(0)[:, 0:256], func=AF.Exp, scale=-1.0), [PB[0]], [sp])
            A(lambda e: e.activation(out=sp[:], in_=sp[:], func=AF.Ln, bias=1.0), [sp], [sp])
            for h in range(4):
                T(lambda e, h=h: e.matmul(out=pbf(1)[0:64, h * 128:(h + 1) * 128], lhsT=sp[:, h * 64:(h + 1) * 64], rhs=CumS,
                                          start=True, stop=True), [sp, cst], [PB[1]])
            A(lambda e: e.copy(out=bT[:], in_=r4(pbf(1)[0:64, :])), [PB[1]], [bT])
            A(lambda e: e.activation(out=eb[:], in_=bT[:], func=AF.Exp), [bT], [eb])
            A(lambda e: e.activation(out=enb[:], in_=bT[:], func=AF.Exp, scale=-1.0), [bT], [enb])
            V(lambda e: e.tensor_tensor(out=ekd[:], in0=bT[:], in1=bT[:, :, tl:tl + 1].to_broadcast([64, 4, 128]), op=ALU.subtract),
              [bT], [ekd])
            A(lambda e: e.activation(out=ekd[:], in_=ekd[:], func=AF.Exp, scale=-1.0), [ekd], [ekd])
            V(lambda e: e.tensor_tensor(out=Qp[:], in0=gqk[:, 0:4, t0:t0 + 128], in1=eb[:], op=ALU.mult), [gqk, eb], [Qp])
            V(lambda e: e.tensor_tensor(out=Kp[:], in0=gqk[:, 4:8, t0:t0 + 128], in1=enb[:], op=ALU.mult), [gqk, enb], [Kp])
            V(lambda e: e.tensor_tensor(out=KdT[:], in0=gqk[:, 4:8, t0:t0 + 128], in1=ekd[:], op=ALU.mult), [gqk, ekd], [KdT])
            for h in range(4):
                T(lambda e, h=h: e.transpose(out=pbb(2)[:, h * 64:(h + 1) * 64], in_=KdT[:, h, :], identity=cstb[0:64, 0, 0:64]),
                  [KdT, cstb], [PB[2]])
            A(lambda e: e.copy(out=Kd[:].rearrange("p h k -> p (h k)"), in_=pbb(2)[:, 0:256]), [PB[2]], [Kd])
            if seg == 1:
                for h in range(4):
                    T(lambda e, h=h: e.matmul(out=pbf(3)[:, h * 128:(h + 1) * 128], lhsT=Kp[:, h, :], rhs=Qp[:, h, :], start=True, stop=True),
                      [Kp, Qp], [PB[3]])
                V(lambda e: e.tensor_tensor(out=att[:], in0=r4(pbf(3)), in1=MK, op=ALU.mult), [PB[3], cst], [att])
                for h in range(4):
                    hs = slice(h * 128, (h + 1) * 128)
                    T(lambda e, h=h, hs=hs: e.matmul(out=pbf(4)[:, hs], lhsT=Qp[:, h, :], rhs=Sbf[:, h, :], start=True, stop=False),
                      [Qp, Sbf], [PB[4]])
                    T(lambda e, h=h, hs=hs: e.matmul(out=pbf(4)[:, hs], lhsT=att[:, h, :], rhs=gv[:, gi, hs], start=False, stop=True),
                      [att, gv], [PB[4]])
                if d == 0:
                    A(lambda e: e.copy(out=oacc[:, i], in_=r4(pbf(4))), [PB[4]], [oacc])
                else:
                    V(lambda e: e.tensor_tensor(out=oacc[:, i], in0=r4(pbf(4)), in1=oacc[:, i], op=ALU.add), [PB[4], oacc], [oacc])
            for h in range(4):
                hs = slice(h * 128, (h + 1) * 128)
                T(lambda e, h=h, hs=hs: e.matmul(out=pbf(5)[0:64, hs], lhsT=Kd[:, h, :], rhs=gv[:, gi, hs], start=True, stop=True),
                  [Kd, gv], [PB[5]])
            V(lambda e: e.tensor_tensor(out=S[:], in0=S[:], in1=eb[:, :, tl:tl + 1].to_broadcast([64, 4, 128]), op=ALU.mult), [S, eb], [S])
            V(lambda e: e.tensor_tensor(out=S[:], in0=r4(pbf(5)[0:64, :]), in1=S[:], op=ALU.add), [PB[5], S], [S])
            A(lambda e: e.copy(out=Sbf[:], in_=S[:]), [S], [Sbf])


def head_norm_gate(fw, PB, pbb, cstb, oacc, gate, gnorm, mixT, chunk0, permute):
    V = lambda fn, r, w: fw.op("dve", fn, r, w)
    A = lambda fn, r, w: fw.op("act", fn, r, w)
    T = lambda fn, r, w: fw.op("pe", fn, r, w)
    ident_b = cstb[:, 0, :]
    sq = fw.sb("hsq", [128, 4, 128])
    ss = fw.sb("hss", [128, 4])
    mix = fw.sb("hmix", [128, 4, 128], BF16)
    for i in range(NT_L):
        o = oacc[:, i]
        V(lambda e: e.tensor_tensor(out=sq[:], in0=o, in1=o, op=ALU.mult), [oacc], [sq])
        V(lambda e: e.tensor_reduce(out=ss[:], in_=sq[:], axis=AX.X, op=ALU.add), [sq], [ss])
        A(lambda e: e.activation(out=ss[:], in_=ss[:], func=AF.Sqrt, scale=1.0 / 128, bias=EPS), [ss], [ss])
        V(lambda e: e.reciprocal(out=ss[:], in_=ss[:]), [ss], [ss])
        V(lambda e: e.tensor_tensor(out=sq[:], in0=o, in1=ss[:].unsqueeze(2).to_broadcast([128, 4, 128]), op=ALU.mult), [oacc, ss], [sq])
        V(lambda e: e.tensor_tensor(out=sq[:], in0=sq[:], in1=gnorm[:].unsqueeze(1).to_broadcast([128, 4, 128]), op=ALU.mult), [sq, gnorm], [sq])
        V(lambda e: e.tensor_tensor(out=mix[:], in0=sq[:], in1=gate[:, i, :].rearrange("p (h t) -> p h t", t=128), op=ALU.mult),
          [sq, gate], [mix])
        bk = i % 2
        for h in range(4):
            T(lambda e, h=h: e.transpose(out=pbb(bk)[:, h * 128:(h + 1) * 128], in_=mix[:, h, :], identity=ident_b), [mix, cstb], [PB[bk]])
        if not permute:
            A(lambda e: e.copy(out=mixT[:, chunk0:chunk0 + 4, i * 128:(i + 1) * 128],
                               in_=pbb(bk)[:, 0:512].rearrange("p (h t) -> p h t", t=128)), [PB[bk]], [mixT])
        else:
            for h in range(4):
                dst = mixT[:, chunk0 + h, :].rearrange("p (r c) -> p c r", c=64)[:, 4 * i:4 * i + 4, :]
                src = pbb(bk)[:, h * 128:(h + 1) * 128].rearrange("p (c r) -> p c r", r=32)
                if h % 2:
                    A(lambda e, dst=dst, src=src: e.copy(out=dst, in_=src), [PB[bk]], [mixT])
                else:
                    V(lambda e, dst=dst, src=src: e.tensor_copy(out=dst, in_=src), [PB[bk]], [mixT])


class StopBuild(Exception):
    pass


NSEQ = 2
TL = 2048
TC = 256
NT_L = 16
NT_C = 2
WCOLS = 3632


def build_program(stage=9):
    nc = bass.Bass("TRN2", target_bir_lowering=False)
    din = lambda n, s: nc.dram_tensor(n, list(s), F32, kind="ExternalInput").ap()
    x_d = din("x", [NSEQ, TL, 1024])
    ctx_d = din("ctx", [NSEQ, TC, 1024])
    cT_d = din("cT", [128, 8, 3])
    wada_d = din("w_ada", [1024, 6144])
    badaT_d = din("b_adaT", [128, 48])
    n1g_d = din("n1gT", [128, 8])
    n2g_d = din("n2gT", [128, 8])
    fing_d = din("final_g", [1, 1024])
    win_d = din("w_in", [1024, WCOLS])
    convT_d = din("convT", [128, 12, 5])
    alog_d = din("a_log", [1, 8])
    dtb_d = din("dt_bias", [1, 8])
    dng_d = din("dn_g", [1, 128])
    glg_d = din("gla_g", [1, 128])
    wa2_d = din("wa2", [2, 16, 256])
    ba_d = din("ba", [2, 1, 256])
    wout_d = din("w_out", [1024, 1024])
    wq_d = din("wq", [1024, 2048])
    keysT_d = din("keysT", [128, 16, 128])
    u_d = din("peer_u", [16384, 1024])
    v_d = din("peer_v", [16384, 1024])
    consts_d = din("consts", [128, 15, 128])
    out_d = nc.dram_tensor("out", [NSEQ, TL, 1024], F32, kind="ExternalOutput").ap()
    dbg_d = nc.dram_tensor("dbg", [128, 8, TL], F32, kind="ExternalOutput").ap() if stage < 2 else None
    PDBG = (stage == 3.5)
    X1OUT = (stage == 8)
    if stage == 8:
        stage = 9
    if PDBG:
        dps_d = nc.dram_tensor("dps", [128, 2048], F32, kind="ExternalOutput").ap()
        dptop_d = nc.dram_tensor("dptop", [128, 256], F32, kind="ExternalOutput").ap()
        dpcv_d = nc.dram_tensor("dpcv", [128, 128], F32, kind="ExternalOutput").ap()
        dpt4_d = nc.dram_tensor("dpt4", [128, 512], F32, kind="ExternalOutput").ap()
        dpt4t_d = nc.dram_tensor("dpt4t", [128, 512], F32, kind="ExternalOutput").ap()
        dpw_d = nc.dram_tensor("dpw", [128, 128, 8], F32, kind="ExternalOutput").ap()
        dppo_d = nc.dram_tensor("dppo", [128, 1024], F32, kind="ExternalOutput").ap()
        dpsr_d = nc.dram_tensor("dpsr", [128, 2, 2048], F32, kind="ExternalOutput").ap()
        dpab_d = nc.dram_tensor("dpab", [128, 2, 512], F32, kind="ExternalOutput").ap()
    modrow_d = nc.dram_tensor("modrow", [3, 6144], F32, kind="Internal").ap()
    x1_d = nc.dram_tensor("x1s", [NSEQ * TL, 1024], F32, kind=("ExternalOutput" if X1OUT else "Internal")).ap()
    ss_d = nc.dram_tensor("sscr", [2, 8, NSEQ * TL, 128], F32, kind="Internal").ap()
    ut_d = nc.dram_tensor("utscr", [128, 128, 8, 128], BF16, kind="Internal").ap()
    vb_d = nc.dram_tensor("vbscr", [128, 128, 1024], BF16, kind="Internal").ap()

    with contextlib.ExitStack() as top:
        fw = FW(nc, top)
        V = lambda fn, r, w: fw.op("dve", fn, r, w)
        A = lambda fn, r, w: fw.op("act", fn, r, w)
        G = lambda fn, r, w: fw.op("pool", fn, r, w)
        T = lambda fn, r, w: fw.op("pe", fn, r, w)

        PS = top.enter_context(nc.psum_tensor("ps", [128, 4096], F32))
        PB = [Buf("pb%d" % i, PS[:, i * 512:(i + 1) * 512]) for i in range(8)]

        def pbf(i, n=1):
            return PS[:, i * 512:(i + n) * 512]

        def pbb(i, n=1):
            return PS[:, i * 512:(i + n) * 512].bitcast(BF16)

        outb = fw.view("outb")
        cst = fw.sb("cst", [128, 15, 128])
        fw.dma("sp", cst[:], consts_d, cst, None)
        cstb = fw.sb("cstb", [128, 3, 128], BF16)
        for a_, b_ in ((0, 0), (1, 9), (2, 12)):
            V(lambda e, a_=a_, b_=b_: e.tensor_copy(out=cstb[:, a_, :], in_=cst[:, b_, :]), [cst], [cstb])
        ident_f = cst[:, 0, :]
        ident_b = cstb[:, 0, :]
        ones_b = cstb[:, 1, :]
        ones_f = cst[:, 9, :]

        modT = fw.sb("modT", [128, 48, 3])
        G1 = fw.sb("G1", [128, 8, 3])
        G2 = fw.sb("G2", [128, 8, 3])
        modrow = fw.view("modrow")
        x1buf = fw.view("x1buf")

        with fw.scope():
            cT = fw.sb("cT", [128, 8, 3])
            fw.dma("sp", cT[:], cT_d, cT, None)
            siluT = fw.sb("siluT", [128, 8, 3])
            A(lambda e: e.activation(out=siluT[:], in_=cT[:], func=AF.Silu), [cT], [siluT])
            badaT = fw.sb("badaT", [128, 48])
            fw.dma("sp", badaT[:], badaT_d, badaT, None)
            n1g = fw.sb("n1g", [128, 8])
            n2g = fw.sb("n2g", [128, 8])
            fw.dma("sp", n1g[:], n1g_d, n1g, None)
            fw.dma("sp", n2g[:], n2g_d, n2g, None)
            slabs = [fw.sb("wada%d" % i, [128, 8, 1024]) for i in range(2)]
            wv = wada_d.rearrange("(k p) c -> p k c", p=128)
            pm = pbf(0)[:, 0:144].rearrange("p (j b) -> p j b", b=3)
            for v in range(6):
                sl = slabs[v % 2]
                fw.dma("sp" if v % 2 == 0 else "act", sl[:], wv[:, :, v * 1024:(v + 1) * 1024], sl, None)
                for ch in range(8):
                    j = v * 8 + ch
                    for k in range(8):
                        T(lambda e, sl=sl, ch=ch, k=k, j=j: e.matmul(
                            out=pm[:, j, :], lhsT=sl[:, k, ch * 128:(ch + 1) * 128], rhs=siluT[:, k, :],
                            start=(k == 0), stop=(k == 7)), [sl, siluT], [PB[0]])
            V(lambda e: e.tensor_tensor(out=modT[:], in0=pm[:, 0:48, :],
                                        in1=badaT[:].unsqueeze(2).to_broadcast([128, 48, 3]), op=ALU.add),
              [PB[0], badaT], [modT])
            for (Gt, ng, o) in ((G1, n1g, 8), (G2, n2g, 32)):
                V(lambda e, Gt=Gt, o=o: e.tensor_scalar(out=Gt[:], in0=modT[:, o:o + 8, :], scalar1=1.0, scalar2=None,
                                                        op0=ALU.add), [modT], [Gt])
                V(lambda e, Gt=Gt, ng=ng: e.tensor_tensor(out=Gt[:], in0=Gt[:],
                                                          in1=ng[:].unsqueeze(2).to_broadcast([128, 8, 3]), op=ALU.mult),
                  [Gt, ng], [Gt])
            for b in range(3):
                fw.dma("pool", modrow_d[b].rearrange("(j p) -> p j", p=128), modT[:, :, b], modrow, modT,
                       allow_slow_non_contiguous=True)

        def make_hT(hT, col0, src_ap, ntiles, Gt, SHo, mcol, xt_bufs, wk, perm=False, srcbuf=None, bank=7):
            for i in range(ntiles):
                xt = xt_bufs[i % 2]
                fw.dma("sp", xt[:], src_ap[i * 128:(i + 1) * 128, :], xt, srcbuf)
                junk, ssq, xn = wk
                A(lambda e, xt=xt: e.activation(out=junk[:], in_=xt[:], func=AF.Square, accum_out=ssq[:]),
                  [xt], [junk, ssq])
                A(lambda e: e.activation(out=ssq[:], in_=ssq[:], func=AF.Sqrt, scale=1.0 / 1024, bias=EPS), [ssq], [ssq])
                V(lambda e: e.reciprocal(out=ssq[:], in_=ssq[:]), [ssq], [ssq])
                V(lambda e, xt=xt: e.tensor_scalar(out=xn[:], in0=xt[:], scalar1=ssq[:, 0:1], scalar2=None, op0=ALU.mult),
                  [xt, ssq], [xn])
                pt = pbb(bank).rearrange("p (k t) -> p k t", t=128)
                for k in range(8):
                    T(lambda e, k=k: e.transpose(out=pt[:, k, :], in_=xn[:, k * 128:(k + 1) * 128], identity=ident_b),
                      [xn, cstb], [PB[bank]])
                if not perm:
                    dst = hT[:, :, col0 + i * 128: col0 + (i + 1) * 128]
                    src = pt[:, 0:8, :]
                    g_b = Gt[:, :, mcol:mcol + 1].to_broadcast([128, 8, 128])
                    s_b = modT[:, SHo:SHo + 8, mcol:mcol + 1].to_broadcast([128, 8, 128])
                else:
                    dst = hT[:, :, col0:col0 + TL].rearrange("p k (c r) -> p k r c", r=32)[:, :, 2 * i:2 * i + 2, :]
                    src = pt[:, 0:8, :].rearrange("p k (r c) -> p k r c", c=64)
                    g_b = Gt[:, :, mcol:mcol + 1].unsqueeze(3).to_broadcast([128, 8, 2, 64])
                    s_b = modT[:, SHo:SHo + 8, mcol:mcol + 1].unsqueeze(3).to_broadcast([128, 8, 2, 64])
                V(lambda e, dst=dst, src=src, g_b=g_b: e.tensor_tensor(out=dst, in0=src, in1=g_b, op=ALU.mult),
                  [PB[bank], Gt], [hT])
                V(lambda e, dst=dst, s_b=s_b: e.tensor_tensor(out=dst, in0=dst, in1=s_b, op=ALU.add), [hT, modT], [hT])

        def load_w_bf16(dst, src_view, ncols, stage_bufs, step=512, d0=0):
            n = 0
            for c0 in range(0, ncols, step):
                c1 = min(ncols, c0 + step)
                sg = stage_bufs[n % 2]
                fw.dma("act" if n % 2 else "sp", sg[:, :, 0:c1 - c0], src_view[:, :, c0:c1], sg, None)
                if n % 2:
                    A(lambda e, sg=sg, c0=c0, c1=c1: e.copy(out=dst[:, :, d0 + c0:d0 + c1], in_=sg[:, :, 0:c1 - c0]), [sg], [dst])
                else:
                    V(lambda e, sg=sg, c0=c0, c1=c1: e.tensor_copy(out=dst[:, :, d0 + c0:d0 + c1], in_=sg[:, :, 0:c1 - c0]), [sg], [dst])
                n += 1

        win_v = win_d.rearrange("(k p) c -> p k c", p=128)
        utscr = fw.view("utscr")
        vbscr = fw.view("vbscr")
        uview = u_d.rearrange("(i j) d -> j i d", j=128)
        vview = v_d.rearrange("(i j) d -> j i d", j=128)

        def p0_bufs():
            return dict(ubs=[fw.sb("ub%d" % i, [128, 1024]) for i in range(2)],
                        vbs=[fw.sb("vb%d" % i, [128, 1024]) for i in range(2)],
                        ubb=[fw.sb("ubb%d" % i, [128, 1024], BF16) for i in range(2)],
                        vbb=[fw.sb("vbb%d" % i, [128, 1024], BF16) for i in range(2)],
                        utb=[fw.sb("utb%d" % i, [128, 8, 128], BF16) for i in range(2)])

        def p0_iter(j, B):
            q = j % 2
            ubs, vbs, ubb, vbb, utb = B["ubs"], B["vbs"], B["ubb"], B["vbb"], B["utb"]
            fw.dma("sp", ubs[q][:], uview[j], ubs[q], None)
            fw.dma("sp", vbs[q][:], vview[j], vbs[q], None)
            A(lambda e: e.copy(out=ubb[q][:], in_=ubs[q][:]), [ubs[q]], [ubb[q]])
            V(lambda e: e.tensor_copy(out=vbb[q][:], in_=vbs[q][:]), [vbs[q]], [vbb[q]])
            for k in range(8):
                T(lambda e, k=k: e.transpose(out=pbb(6 + q)[:, k * 128:(k + 1) * 128], in_=ubb[q][:, k * 128:(k + 1) * 128],
                                             identity=ident_b), [ubb[q], cstb], [PB[6 + q]])
            V(lambda e: e.tensor_copy(out=utb[q][:].rearrange("p k i -> p (k i)"), in_=pbb(6 + q)), [PB[6 + q]], [utb[q]])
            fw.dma("pool", ut_d[j], utb[q][:], utscr, utb[q])
            fw.dma("pool", vb_d[j], vbb[q][:], vbscr, vbb[q])

        if 0.6 < stage < 0.7:
            fw.dn_limit = int(round((stage - 0.6) * 1000))

        def ck(x):
            if stage <= x:
                fw.dead = True

        for s in range(NSEQ if stage >= 0.1 else 0):
          try:
            with fw.scope():
                mixT = fw.sb("mixT", [128, 8, TL], BF16)
                with fw.scope():
                    Pq = fw.sb("Pq", [128, 12, TC + TL + 8], BF16)
                    CO = (2, 262)
                    Zg = fw.sb("Zg", [128, NT_L, 512], BF16)
                    SM = fw.sb("SM", [128, 18, 16])
                    with fw.scope():
                        hT = fw.sb("hT", [128, 8, TC + TL], BF16)
                        xts = [fw.sb("xt%d" % i, [128, 1024]) for i in range(2)]
                        wk = (fw.sb("junk", [128, 1024], BF16), fw.sb("ssq", [128, 1]), fw.sb("xn", [128, 1024], BF16))
                        make_hT(hT, 0, ctx_d[s], NT_C, G1, 0, 2, xts, wk)
                        make_hT(hT, TC, x_d[s], NT_L, G1, 0, s, xts, wk)
                        ck(0.2)
                        wdn = fw.sb("wdn", [128, 8, 2064], BF16)
                        stg = [fw.sb("stg%d" % i, [128, 8, 128]) for i in range(2)]
                        load_w_bf16(wdn, win_v[:, :, 0:2048], 2048, stg, 128)
                        load_w_bf16(wdn, win_v[:, :, 3584:3600], 16, stg, 128, 2048)
                        V(lambda e: e.memset(Pq[:], 0.0), [], [Pq])
                        ck(0.3)
                        nb = 0
                        for (t0, tn, seg) in [(0, 256, 0)] + [(TC + b * 512, 512, 1) for b in range(4)]:
                            for c in range(12):
                                bk = nb % 2
                                nb += 1
                                for k in range(8):
                                    T(lambda e, bk=bk, c=c, k=k, t0=t0, tn=tn: e.matmul(
                                        out=pbf(bk)[:, 0:tn], lhsT=wdn[:, k, c * 128:(c + 1) * 128], rhs=hT[:, k, t0:t0 + tn],
                                        start=(k == 0), stop=(k == 7)), [wdn, hT], [PB[bk]])
                                d0 = CO[seg] + (t0 - (TC if seg else 0))
                                if bk:
                                    A(lambda e, bk=bk, c=c, d0=d0, tn=tn: e.copy(out=Pq[:, c, d0:d0 + tn], in_=pbf(bk)[:, 0:tn]),
                                      [PB[bk]], [Pq])
                                else:
                                    V(lambda e, bk=bk, c=c, d0=d0, tn=tn: e.tensor_copy(out=Pq[:, c, d0:d0 + tn], in_=pbf(bk)[:, 0:tn]),
                                      [PB[bk]], [Pq])
                        for i in range(18):
                            t0 = i * 128
                            if i >= 2:
                                for k in range(8):
                                    T(lambda e, k=k, t0=t0: e.matmul(out=pbf(2), lhsT=hT[:, k, t0:t0 + 128], rhs=wdn[:, k, 1536:2048],
                                                                     start=(k == 0), stop=(k == 7)), [wdn, hT], [PB[2]])
                                A(lambda e, i=i: e.activation(out=Zg[:, i - 2, :], in_=pbf(2), func=AF.Silu), [PB[2]], [Zg])
                            for k in range(8):
                                T(lambda e, k=k, t0=t0: e.matmul(out=pbf(3)[:, 0:16], lhsT=hT[:, k, t0:t0 + 128], rhs=wdn[:, k, 2048:2064],
                                                                 start=(k == 0), stop=(k == 7)), [wdn, hT], [PB[3]])
                            V(lambda e, i=i: e.tensor_copy(out=SM[:, i, :], in_=pbf(3)[:, 0:16]), [PB[3]], [SM])
                    ck(0.4)
                    BET = fw.sb("BET", [128, 18, 8])
                    NBET = fw.sb("NBET", [128, 18, 8])
                    GG = fw.sb("GG", [128, 18, 8])
                    alog = fw.sb("alog", [128, 8])
                    dtb = fw.sb("dtb", [128, 8])
                    fw.dma("sp", alog[:], alog_d.partition_broadcast(128), alog, None)
                    fw.dma("sp", dtb[:], dtb_d.partition_broadcast(128), dtb, None)
                    A(lambda e: e.activation(out=BET[:], in_=SM[:, :, 0:8], func=AF.Sigmoid), [SM], [BET])
                    V(lambda e: e.tensor_scalar(out=NBET[:], in0=BET[:], scalar1=-1.0, scalar2=None, op0=ALU.mult), [BET], [NBET])
                    V(lambda e: e.tensor_tensor(out=GG[:], in0=SM[:, :, 8:16], in1=dtb[:].unsqueeze(1).to_broadcast([128, 18, 8]),
                                                op=ALU.add), [SM, dtb], [GG])
                    A(lambda e: e.activation(out=GG[:], in_=GG[:], func=AF.Exp), [GG], [GG])
                    A(lambda e: e.activation(out=GG[:], in_=GG[:], func=AF.Ln, bias=1.0), [GG], [GG])
                    A(lambda e: e.activation(out=alog[:], in_=alog[:], func=AF.Exp), [alog], [alog])
                    V(lambda e: e.tensor_scalar(out=alog[:], in0=alog[:], scalar1=-1.0, scalar2=None, op0=ALU.mult), [alog], [alog])
                    V(lambda e: e.tensor_tensor(out=GG[:], in0=GG[:], in1=alog[:].unsqueeze(1).to_broadcast([128, 18, 8]),
                                                op=ALU.mult), [GG, alog], [GG])
                    ck(0.5)
                    cw = fw.sb("cw", [128, 12, 5])
                    fw.dma("sp", cw[:], convT_d, cw, None)
                    with fw.scope():
                        acc = fw.sb("acc", [128, TL])
                        sq = fw.sb("sq", [128, 512], BF16)
                        rin = fw.sb("rin", [128, 512])
                        for c in range(12):
                            for seg, n in ((0, TC), (1, TL)):
                                o = CO[seg]
                                V(lambda e, c=c, o=o, n=n: e.tensor_scalar(out=acc[:, 0:n], in0=Pq[:, c, o - 2:o - 2 + n],
                                                                           scalar1=cw[:, c, 0:1], scalar2=None, op0=ALU.mult),
                                  [Pq, cw], [acc])
                                for j in range(1, 5):
                                    V(lambda e, c=c, o=o, n=n, j=j: e.scalar_tensor_tensor(
                                        out=acc[:, 0:n], in0=Pq[:, c, o - 2 + j:o - 2 + j + n], scalar=cw[:, c, j:j + 1],
                                        in1=acc[:, 0:n], op0=ALU.mult, op1=ALU.add), [Pq, cw, acc], [acc])
                                if c >= 8:
                                    A(lambda e, c=c, o=o, n=n: e.activation(out=Pq[:, c, o:o + n], in_=acc[:, 0:n], func=AF.Silu),
                                      [acc], [Pq])
                                    continue
                                A(lambda e, n=n: e.activation(out=acc[:, 0:n], in_=acc[:, 0:n], func=AF.Silu), [acc], [acc])
                                for b0 in range(0, n, 512):
                                    bn = min(512, n - b0)
                                    V(lambda e, b0=b0, bn=bn: e.tensor_tensor(out=sq[:, 0:bn], in0=acc[:, b0:b0 + bn],
                                                                              in1=acc[:, b0:b0 + bn], op=ALU.mult), [acc], [sq])
                                    T(lambda e, bn=bn: e.matmul(out=pbf(0)[:, 0:bn], lhsT=ones_b, rhs=sq[:, 0:bn], start=True, stop=True),
                                      [sq, cstb], [PB[0]])
                                    A(lambda e, bn=bn: e.activation(out=rin[:, 0:bn], in_=pbf(0)[:, 0:bn], func=AF.Sqrt, bias=EPS),
                                      [PB[0]], [rin])
                                    V(lambda e, bn=bn: e.reciprocal(out=rin[:, 0:bn], in_=rin[:, 0:bn]), [rin], [rin])
                                    sc = (128.0 ** -0.5) if c < 4 else 1.0
                                    V(lambda e, c=c, o=o, b0=b0, bn=bn, sc=sc: e.scalar_tensor_tensor(
                                        out=Pq[:, c, o + b0:o + b0 + bn], in0=acc[:, b0:b0 + bn], scalar=sc, in1=rin[:, 0:bn],
                                        op0=ALU.mult, op1=ALU.mult), [acc, rin], [Pq])
                    ck(0.6)
                    oacc = fw.sb("oacc", [128, NT_L, 4, 128])
                    with fw.scope():
                        dn_scan(fw, nc, PS, PB, pbf, pbb, cst, cstb, Pq, CO, BET, NBET, GG, oacc)
                    ck(0.7)
                    dng = fw.sb("dng", [128, 128])
                    fw.dma("sp", dng[:], dng_d.partition_broadcast(128), dng, None)
                    with fw.scope():
                        head_norm_gate(fw, PB, pbb, cstb, oacc, Zg, dng, mixT, 0, False)

                with fw.scope():
                    gqk = fw.sb("gqk", [64, 8, TC + TL], BF16)
                    gv = fw.sb("gv", [128, 18, 512], BF16)
                    Rg = fw.sb("Rg", [128, NT_L, 512], BF16)
                    LRT = fw.sb("LRT", [16, 2, TC + TL], BF16)
                    with fw.scope():
                        hT = fw.sb("hTg", [128, 8, TC + TL], BF16)
                        xts = [fw.sb("xtg%d" % i, [128, 1024]) for i in range(2)]
                        wk = (fw.sb("junkg", [128, 1024], BF16), fw.sb("ssqg", [128, 1]), fw.sb("xng", [128, 1024], BF16))
                        make_hT(hT, 0, ctx_d[s], NT_C, G1, 0, 2, xts, wk)
                        make_hT(hT, TC, x_d[s], NT_L, G1, 0, s, xts, wk, perm=True)
                        wgl = fw.sb("wgl", [128, 8, 1568], BF16)
                        stg = [fw.sb("stgg%d" % i, [128, 8, 128]) for i in range(2)]
                        load_w_bf16(wgl, win_v[:, :, 2048:3584], 1536, stg, 128)
                        load_w_bf16(wgl, win_v[:, :, 3600:3632], 32, stg, 128, 1536)
                        nb = 0
                        for (t0, tn) in [(0, 256)] + [(TC + b * 512, 512) for b in range(4)]:
                            for g in range(10):
                                bk = nb % 2
                                nb += 1
                                if g < 8:
                                    cs, m = slice(g * 64, (g + 1) * 64), 64
                                else:
                                    cs, m = slice(1536 + (g - 8) * 16, 1536 + (g - 7) * 16), 16
                                for k in range(8):
                                    T(lambda e, bk=bk, k=k, cs=cs, m=m, t0=t0, tn=tn: e.matmul(
                                        out=pbf(bk)[0:m, 0:tn], lhsT=wgl[:, k, cs], rhs=hT[:, k, t0:t0 + tn],
                                        start=(k == 0), stop=(k == 7)), [wgl, hT], [PB[bk]])
                                if g < 4:
                                    A(lambda e, bk=bk, g=g, t0=t0, tn=tn: e.mul(out=gqk[:, g, t0:t0 + tn], in_=pbf(bk)[0:64, 0:tn], mul=0.125),
                                      [PB[bk]], [gqk])
                                elif g < 8:
                                    V(lambda e, bk=bk, g=g, t0=t0, tn=tn: e.tensor_copy(out=gqk[:, g, t0:t0 + tn], in_=pbf(bk)[0:64, 0:tn]),
                                      [PB[bk]], [gqk])
                                else:
                                    V(lambda e, bk=bk, g=g, t0=t0, tn=tn: e.tensor_copy(out=LRT[:, g - 8, t0:t0 + tn], in_=pbf(bk)[0:16, 0:tn]),
                                      [PB[bk]], [LRT])
                        for i in range(18):
                            t0 = i * 128
                            for k in range(8):
                                T(lambda e, k=k, t0=t0: e.matmul(out=pbf(2), lhsT=hT[:, k, t0:t0 + 128], rhs=wgl[:, k, 512:1024],
                                                                 start=(k == 0), stop=(k == 7)), [wgl, hT], [PB[2]])
                            V(lambda e, i=i: e.tensor_copy(out=gv[:, i, :], in_=pbf(2)), [PB[2]], [gv])
                            if i >= 2:
                                for k in range(8):
                                    T(lambda e, k=k, t0=t0: e.matmul(out=pbf(3), lhsT=hT[:, k, t0:t0 + 128], rhs=wgl[:, k, 1024:1536],
                                                                     start=(k == 0), stop=(k == 7)), [wgl, hT], [PB[3]])
                                A(lambda e, i=i: e.activation(out=Rg[:, i - 2, :], in_=pbf(3), func=AF.Silu), [PB[3]], [Rg])
                    ck(1.2)
                    oacc = fw.sb("oaccg", [128, NT_L, 4, 128])
                    with fw.scope():
                        if stage >= 3:
                            p0B = p0_bufs()
                            p0it = iter(range(s * 64, (s + 1) * 64))

                            def extra():
                                for _ in range(2):
                                    j_ = next(p0it, None)
                                    if j_ is not None:
                                        p0_iter(j_, p0B)
                        else:
                            extra = None
                        gla_scan(fw, PB, pbf, pbb, cst, cstb, gqk, gv, LRT, wa2_d, ba_d, oacc, extra)
                    ck(1.3)
                    glg = fw.sb("glg", [128, 128])
                    fw.dma("sp", glg[:], glg_d.partition_broadcast(128), glg, None)
                    with fw.scope():
                        head_norm_gate(fw, PB, pbb, cstb, oacc, Rg, glg, mixT, 4, True)
                ck(1.4)
                with fw.scope():
                    wo = fw.sb("wo", [128, 8, 1024], BF16)
                    stg = [fw.sb("stgo%d" % i, [128, 8, 128]) for i in range(2)]
                    load_w_bf16(wo, wout_d.rearrange("(k p) c -> p k c", p=128), 1024, stg, 128)
                    g1B = fw.sb("g1B", [128, 1024])
                    fw.dma("sp", g1B[:], modrow_d[s:s + 1, 2048:3072].partition_broadcast(128), g1B, modrow)
                    xts = [fw.sb("xto%d" % i, [128, 1024]) for i in range(2)]
                    x1ts = [fw.sb("x1t%d" % i, [128, 1024]) for i in range(2)]
                    for i in range(NT_L):
                        xt = xts[i % 2]
                        x1t = x1ts[i % 2]
                        fw.dma("sp", xt[:], x_d[s][i * 128:(i + 1) * 128, :], xt, None)
                        for hf in range(2):
                            bk = 2 * (i % 2) + hf
                            for k in range(8):
                                T(lambda e, k=k, i=i, hf=hf, bk=bk: e.matmul(out=pbf(bk), lhsT=mixT[:, k, i * 128:(i + 1) * 128],
                                                                             rhs=wo[:, k, hf * 512:(hf + 1) * 512],
                                                                             start=(k == 0), stop=(k == 7)), [mixT, wo], [PB[bk]])
                            hs = slice(hf * 512, (hf + 1) * 512)
                            V(lambda e, bk=bk, hs=hs, x1t=x1t: e.tensor_tensor(out=x1t[:, hs], in0=pbf(bk), in1=g1B[:, hs], op=ALU.mult),
                              [PB[bk], g1B], [x1t])
                            V(lambda e, hs=hs, x1t=x1t, xt=xt: e.tensor_tensor(out=x1t[:, hs], in0=x1t[:, hs], in1=xt[:, hs], op=ALU.add),
                              [x1t, xt], [x1t])
                        fw.dma("pool", x1_d[s * TL + i * 128:s * TL + (i + 1) * 128, :], x1t[:], x1buf, x1t)
                ck(1.45)
                if stage < 2:
                    if s == 0:
                        with fw.scope():
                            dts = [fw.sb("dtmp%d" % i, [128, TL]) for i in range(2)]
                            for ch in range(8):
                                dt_ = dts[ch % 2]
                                V(lambda e, ch=ch, dt_=dt_: e.tensor_copy(out=dt_[:], in_=mixT[:, ch, :]), [mixT], [dt_])
                                fw.dma("sp", dbg_d[:, ch, :], dt_[:], outb, dt_)
                        fw.dead = True
                    continue
          except StopBuild:
            break
        fw.dead = False
        if stage >= 3:
            def pk(x):
                if stage == x:
                    fw.dead = True
            pk(3.1)
            with fw.scope():
                wqb = fw.sb("wqb", [128, 8, 2048], BF16)
                keysb = fw.sb("keysb", [128, 16, 128], BF16)
                with fw.scope():
                    stg = [fw.sb("stgp%d" % i, [128, 8, 128]) for i in range(2)]
                    load_w_bf16(wqb, wq_d.rearrange("(k p) c -> p k c", p=128), 2048, stg, 128)
                    keysf = fw.sb("keysf", [128, 16, 128])
                    fw.dma("sp", keysf[:], keysT_d, keysf, None)
                    V(lambda e: e.tensor_copy(out=keysb[:], in_=keysf[:]), [keysf], [keysb])
                g2B = fw.sb("g2B", [128, 1024])
                fingB = fw.sb("fingB", [128, 1024])
                fw.dma("sp", fingB[:], fing_d.partition_broadcast(128), fingB, None)
                h2T = fw.sb("h2T", [128, 8, 256], BF16)
                xts = [fw.sb("xtp%d" % i, [128, 1024]) for i in range(2)]
                xnp = fw.sb("xnp", [128, 1024], BF16)
                wk = (xnp, fw.sb("ssqp", [128, 1]), xnp)
                qT = fw.sb("qT", [128, 16, 256], BF16)
                ssb = fw.sb("ssb", [128, 16, 128])
                top = fw.sb("top", [128, 16, 16])
                idxu = fw.sb("idxu", [128, 8, 16], mybir.dt.uint32)
                cand = fw.sb("cand", [128, 4, 256])
                wk2 = fw.sb("wk2", [128, 256])
                wkk = wk2
                cv = fw.sb("cv", [128, 8, 16])
                T4s = fw.sb("T4s", [128, 3, 128])
                T4 = fw.sb("T4", [128, 2, 3, 128])
                rZ = fw.sb("rZ", [128, 8])
                NSB = 8
                NWB = 3
                Sreps = [fw.sb("Srep%d" % i, [128, NSB, 128]) for i in range(NWB)]
                Abs_ = [fw.sb("Ab%d" % i, [128, NSB, 128], BF16) for i in range(2)]
                Bbs_ = [fw.sb("Bb%d" % i, [128, NSB, 128], BF16) for i in range(2)]
                Ebs_ = [fw.sb("Eb%d" % i, [128, NSB, 128], BF16) for i in range(2)]
                Wsb = fw.sb("Wsb", [128, 128, 256], BF16)
                utl = [fw.sb("utl%d" % i, [128, 2, 8, 128], BF16) for i in range(2)]
                vtl = [fw.sb("vtl%d" % i, [128, 2, 1024], BF16) for i in range(2)]
                gef = [fw.sb("gef%d" % i, [128, 256], BF16) for i in range(2)]
                gw = [fw.sb("gw%d" % i, [128, 256], BF16) for i in range(2)]
                x2ap = cand[:].rearrange("p h c -> p (h c)")
                x2 = cand
                ssbuf = [fw.view("ssbuf%d" % i) for i in range(2)]
                tv = top[:].rearrange("p (h q) k -> p h q k", q=2)
                zzap = wk2[:, 0:128].rearrange("p (h k) -> p h k", k=16)
                bk3 = lambda ap: ap.unsqueeze(2).to_broadcast([128, NSB, 128])
                iota_b = cstb[:, 2, :].unsqueeze(1).to_broadcast([128, NSB, 128])
                t4 = lambda q: T4s[:, q, :].rearrange("p (h k) -> p h k", k=16)
                b16 = lambda ap: ap.to_broadcast([128, 8, 16])
                def phase1(blk, h2T, T4):
                    sq = (blk * 2) // NT_L
                    sq = (blk * 2) // NT_L
                    make_hT(h2T, 0, x1_d[blk * 256:(blk + 1) * 256, :], 2, G2, 24, sq, xts, wk, srcbuf=x1buf, bank=3)
                    yield
                    for hp in range(16):
                        bk = 2 + hp % 2
                        for k in range(8):
                            T(lambda e, bk=bk, hp=hp, k=k: e.matmul(out=pbf(bk)[:, 0:256], lhsT=wqb[:, k, hp * 128:(hp + 1) * 128],
                                                                    rhs=h2T[:, k, :], start=(k == 0), stop=(k == 7)), [wqb, h2T], [PB[bk]])
                        if bk % 2:
                            A(lambda e, bk=bk, hp=hp: e.copy(out=qT[:, hp, :], in_=pbf(bk)[:, 0:256]), [PB[bk]], [qT])
                        else:
                            V(lambda e, bk=bk, hp=hp: e.tensor_copy(out=qT[:, hp, :], in_=pbf(bk)[:, 0:256]), [PB[bk]], [qT])
                        yield
                    sc = ssbuf[blk % 2]
                    for tl in range(2):
                        gt = blk * 2 + tl
                        for g in range(4):
                            bk = 2 + g % 2
                            for q in range(4):
                                hp = g * 4 + q
                                T(lambda e, bk=bk, hp=hp, q=q, tl=tl: e.matmul(out=pbf(bk)[:, q * 128:(q + 1) * 128],
                                                                               lhsT=qT[:, hp, tl * 128:(tl + 1) * 128],
                                                                               rhs=keysb[:, hp, :], start=True, stop=True), [qT, keysb], [PB[bk]])
                            A(lambda e, bk=bk, g=g: e.copy(out=ssb[:, g * 4:(g + 1) * 4, :].rearrange("p a i -> p (a i)"), in_=pbf(bk)),
                              [PB[bk]], [ssb])
                            yield
                        fw.dma("pool", ss_d[1, :, gt * 128:(gt + 1) * 128, :].rearrange("h t i -> t h i"),
                               ssb[:].rearrange("p (h q) i -> p h q i", q=2)[:, :, 1, :], sc, ssb)
                        for hp in range(16):
                            V(lambda e, hp=hp: e.max(out=top[:, hp, 0:8], in_=ssb[:, hp, :]), [ssb], [top])
                            V(lambda e, hp=hp: e.match_replace(out=wkk[:, 0:128], in_to_replace=top[:, hp, 0:8], in_values=ssb[:, hp, :],
                                                               imm_value=-1e30), [ssb, top], [wkk])
                            V(lambda e, hp=hp: e.max(out=top[:, hp, 8:16], in_=wkk[:, 0:128]), [wkk], [top])
                            yield
                            if hp % 2 == 0:
                                for o8 in (0, 8):
                                    V(lambda e, hp=hp, o8=o8: e.max_index(out=idxu[:, hp // 2, o8:o8 + 8], in_max=top[:, hp, o8:o8 + 8],
                                                                          in_values=ssb[:, hp, :]), [ssb, top], [idxu])
                        for hh in range(2):
                            V(lambda e, hh=hh: e.tensor_tensor(out=cand[:].rearrange("p h (a b) -> p h a b", b=16),
                                                               in0=tv[:, hh * 4:hh * 4 + 4, 0, :].unsqueeze(3).to_broadcast([128, 4, 16, 16]),
                                                               in1=tv[:, hh * 4:hh * 4 + 4, 1, :].unsqueeze(2).to_broadcast([128, 4, 16, 16]),
                                                               op=ALU.add), [top], [cand])
                            for h in range(hh * 4, hh * 4 + 4):
                                V(lambda e, h=h: e.max(out=cv[:, h, 0:8], in_=cand[:, h % 4, :]), [cand], [cv])
                                V(lambda e, h=h: e.match_replace(out=wk2[:], in_to_replace=cv[:, h, 0:8], in_values=cand[:, h % 4, :],
                                                                 imm_value=-1e30), [cand, cv], [wk2])
                                V(lambda e, h=h: e.max(out=cv[:, h, 8:16], in_=wk2[:]), [wk2], [cv])
                                yield
                        V(lambda e: e.tensor_copy(out=t4(0), in_=idxu[:]), [idxu], [T4s])
                        V(lambda e: e.tensor_scalar(out=t4(1), in0=tv[:, :, 0, :], scalar1=-1.0, scalar2=-1e-5, op0=ALU.mult, op1=ALU.add),
                          [top], [T4s])
                        V(lambda e: e.tensor_tensor(out=t4(1), in0=t4(1), in1=b16(cv[:, :, 15:16]), op=ALU.add), [T4s, cv], [T4s])
                        V(lambda e: e.tensor_tensor(out=zzap, in0=cv[:], in1=b16(cv[:, :, 0:1]), op=ALU.subtract), [cv], [wk2])
                        A(lambda e: e.activation(out=zzap, in_=zzap, func=AF.Exp), [wk2], [wk2])
                        V(lambda e: e.tensor_reduce(out=rZ[:], in_=zzap, axis=AX.X, op=ALU.add), [wk2], [rZ])
                        V(lambda e: e.reciprocal(out=rZ[:], in_=rZ[:]), [rZ], [rZ])
                        yield
                        V(lambda e: e.tensor_tensor(out=t4(2), in0=tv[:, :, 0, :], in1=b16(cv[:, :, 0:1]), op=ALU.subtract), [top, cv], [T4s])
                        A(lambda e: e.activation(out=t4(2), in_=t4(2), func=AF.Exp), [T4s], [T4s])
                        V(lambda e: e.tensor_tensor(out=t4(2), in0=t4(2), in1=b16(rZ[:].unsqueeze(2)), op=ALU.mult), [T4s, rZ], [T4s])
                        for q in range(3):
                            T(lambda e, q=q: e.transpose(out=pbf(2)[:, q * 128:(q + 1) * 128], in_=T4s[:, q, :], identity=ident_f),
                              [T4s, cst], [PB[2]])
                        A(lambda e, tl=tl: e.copy(out=T4[:, tl].rearrange("p q t -> p (q t)"), in_=pbf(2)[:, 0:384]), [PB[2]], [T4])
                        yield

                h2Ts = [h2T, fw.sb("h2Tb", [128, 8, 256], BF16)]
                T4x = [T4, fw.sb("T4b", [128, 2, 3, 128])]
                NB = NSEQ * NT_L // 2
                import os as _os
                NXT_PER_JJ = int(_os.environ.get("NXT", "4"))
                for _ in phase1(0, h2Ts[0], T4x[0]):
                    pass
                for blk in range(NB):
                    sq = (blk * 2) // NT_L
                    h2T, T4 = h2Ts[blk % 2], T4x[blk % 2]
                    sc = ssbuf[blk % 2]
                    nxt = phase1(blk + 1, h2Ts[(blk + 1) % 2], T4x[(blk + 1) % 2]) if blk + 1 < NB else iter(())
                    pk(3.2)
                    pending = []
                    for sbk in range(256 // NSB):
                        tl = (sbk * NSB) // 128
                        tin = (sbk * NSB) % 128
                        ta = blk * 256 + sbk * NSB
                        tsl = slice(tin, tin + NSB)
                        Srep, Ab, Bb, Eb = Sreps[sbk % NWB], Abs_[sbk % 2], Bbs_[sbk % 2], Ebs_[sbk % 2]
                        fw.dma("sp", Srep[:], ss_d[1, :, ta:ta + NSB, :].unsqueeze(1).to_broadcast([8, 16, NSB, 128]), Srep, sc)
                        for t in range(NSB):
                            V(lambda e, t=t, tin=tin, tl=tl, Ab=Ab: e.tensor_scalar(
                                out=Ab[:, t, :], in0=cstb[:, 2, :], scalar1=T4[:, tl, 0, tin + t:tin + t + 1],
                                scalar2=T4[:, tl, 2, tin + t:tin + t + 1], op0=ALU.is_equal, op1=ALU.mult), [cstb, T4], [Ab])
                        V(lambda e, tsl=tsl, tl=tl, Srep=Srep, Bb=Bb: e.tensor_tensor(out=Bb[:], in0=Srep[:], in1=bk3(T4[:, tl, 1, tsl]), op=ALU.is_ge),
                          [Srep, T4], [Bb])
                        A(lambda e, Srep=Srep, Eb=Eb: e.activation(out=Eb[:], in_=Srep[:], func=AF.Exp), [Srep], [Eb])
                        G(lambda e, Eb=Eb, Bb=Bb: e.tensor_tensor(out=Bb[:], in0=Bb[:], in1=Eb[:], op=ALU.mult), [Bb, Eb], [Bb])
                        for fn_ in pending:
                            fn_()
                        pending = []
                        for t in range(NSB):
                            bk = 2 + 2 * (sbk % 2) + (t // 4) % 2
                            T(lambda e, bk=bk, t=t, Ab=Ab, Bb=Bb: e.matmul(out=pbf(bk)[:, (t % 4) * 128:(t % 4 + 1) * 128], lhsT=Ab[:, t, :],
                                                                           rhs=Bb[:, t, :], start=True, stop=True), [Ab, Bb], [PB[bk]])
                            if t % 4 == 3:
                                t0 = sbk * NSB + t - 3
                                dst = Wsb[:, :, t0:t0 + 4].rearrange("p j t -> p t j")
                                src = pbf(bk).rearrange("p (t j) -> p t j", j=128)
                                if (t // 4) % 2:
                                    pending.append(lambda dst=dst, src=src, bk=bk: A(lambda e: e.copy(out=dst, in_=src), [PB[bk]], [Wsb]))
                                else:
                                    pending.append(lambda dst=dst, src=src, bk=bk: V(lambda e: e.tensor_copy(out=dst, in_=src), [PB[bk]], [Wsb]))
                    for fn_ in pending:
                        fn_()
                    pending = []
                    pk(3.3)
                    pend2 = []
                    for jj in range(64):
                        for _ in range(NXT_PER_JJ):
                            next(nxt, None)
                        ut2, vt2 = utl[jj % 2], vtl[jj % 2]
                        fw.dma("sp", ut2[:], ut_d[2 * jj:2 * jj + 2].rearrange("j p k i -> p j k i"), ut2, utscr)
                        fw.dma("sp", vt2[:], vb_d[2 * jj:2 * jj + 2].rearrange("j p d -> p j d"), vt2, vbscr)
                        for jl in range(2):
                            j = 2 * jj + jl
                            bk = j % 2
                            for k in range(8):
                                T(lambda e, bk=bk, k=k, ut2=ut2, jl=jl: e.matmul(out=pbf(bk)[:, 0:256], lhsT=ut2[:, jl, k, :], rhs=h2T[:, k, :],
                                                                                 start=(k == 0), stop=(k == 7)), [ut2, h2T], [PB[bk]])
                            ge_, gw_ = gef[j % 2], gw[j % 2]
                            A(lambda e, bk=bk, ge_=ge_: e.activation(out=ge_[:], in_=pbf(bk)[:, 0:256], func=AF.Gelu), [PB[bk]], [ge_])
                            V(lambda e, ge_=ge_, gw_=gw_, j=j: e.tensor_tensor(out=gw_[:], in0=ge_[:], in1=Wsb[:, j, :], op=ALU.mult),
                              [ge_, Wsb], [gw_])
                            for fn_ in pend2:
                                fn_()
                            pend2 = []
                            for tl in range(2):
                                for hf in range(2):
                                    ob = 4 + tl * 2 + hf
                                    pend2.append(lambda hf=hf, tl=tl, ob=ob, gw_=gw_, vt2=vt2, jl=jl, j=j: T(lambda e: e.matmul(
                                        out=pbf(ob), lhsT=gw_[:, tl * 128:(tl + 1) * 128], rhs=vt2[:, jl, hf * 512:(hf + 1) * 512],
                                        start=(j == 0), stop=(j == 127)), [gw_, vt2], [PB[ob]]))
                    for fn_ in pend2:
                        fn_()
                    pend2 = []
                    pk(3.4)
                    for _ in nxt:
                        pass
                    if blk % (NT_L // 2) == 0:
                        fw.dma("sp", g2B[:], modrow_d[sq:sq + 1, 5120:6144].partition_broadcast(128), g2B, modrow)
                    for tl in range(2):
                        xt = xts[tl]
                        fw.dma("sp", xt[:], x1_d[blk * 256 + tl * 128:blk * 256 + (tl + 1) * 128, :], xt, x1buf)
                        for hf in range(2):
                            hs = slice(hf * 512, (hf + 1) * 512)
                            ob = 4 + tl * 2 + hf
                            V(lambda e, ob=ob, hs=hs: e.tensor_tensor(out=x2ap[:, hs], in0=pbf(ob), in1=g2B[:, hs], op=ALU.mult),
                              [PB[ob], g2B], [x2])
                        V(lambda e, xt=xt: e.tensor_tensor(out=x2ap, in0=x2ap, in1=xt[:], op=ALU.add), [x2, xt], [x2])
                        junk, ssq, xn = wk
                        A(lambda e: e.activation(out=junk[:], in_=x2ap, func=AF.Square, accum_out=ssq[:]), [x2], [junk, ssq])
                        A(lambda e: e.activation(out=ssq[:], in_=ssq[:], func=AF.Sqrt, scale=1.0 / 1024, bias=EPS), [ssq], [ssq])
                        V(lambda e: e.reciprocal(out=ssq[:], in_=ssq[:]), [ssq], [ssq])
                        V(lambda e: e.scalar_tensor_tensor(out=x2ap, in0=x2ap, scalar=ssq[:, 0:1], in1=fingB[:], op0=ALU.mult, op1=ALU.mult),
                          [x2, ssq, fingB], [x2])
                        ti = (blk * 2 + tl) % NT_L
                        fw.dma("pool", out_d[sq][ti * 128:(ti + 1) * 128, :], x2ap, outb, x2)
                    pk(3.6)
        fw.dead = False
        fw.finish([outb], "sp")
        fw.barrier()
    return nc


def _layout(inp, core):
    b0 = 2 * core
    f = lambda a: np.ascontiguousarray(a, dtype=np.float32)
    csel = np.stack([inp["c"][b0], inp["c"][b0 + 1], inp["c_ctx"]])
    w_in = inp["w_in"][0]
    w_in_r = np.concatenate([w_in[:, 0:2048], w_in[:, 2064:3600], w_in[:, 2048:2064], w_in[:, 3600:3632]], axis=1)
    return {
        "x": f(inp["x"][b0:b0 + 2]), "ctx": f(inp["ctx"][b0:b0 + 2]),
        "cT": f(csel.reshape(3, 8, 128).transpose(2, 1, 0)),
        "w_ada": f(inp["w_ada"][0]), "b_adaT": f(inp["b_ada"][0].reshape(48, 128).T),
        "n1gT": f(inp["norm1_g"][0].reshape(8, 128).T), "n2gT": f(inp["norm2_g"][0].reshape(8, 128).T),
        "final_g": f(inp["final_g"].reshape(1, 1024)), "w_in": f(w_in_r),
        "convT": f(inp["conv_w"][0].T.reshape(12, 128, 5).transpose(1, 0, 2)),
        "a_log": f(inp["dn_a_log"][0].reshape(1, 8)), "dt_bias": f(inp["dn_dt_bias"][0].reshape(1, 8)),
        "dn_g": f(inp["dn_norm_g"][0].reshape(1, 128)), "gla_g": f(inp["gla_norm_g"][0].reshape(1, 128)),
        "wa2": f(inp["gla_wa2"][0]), "ba": f(inp["gla_ba"][0].reshape(2, 1, 256)),
        "w_out": f(inp["w_out"][0]), "wq": f(inp["peer_wq"][0]),
        "keysT": f(inp["peer_keys"][0].reshape(16, 128, 128).transpose(2, 0, 1)),
        "peer_u": f(inp["peer_u"][0]), "peer_v": f(inp["peer_v"][0]),
        "consts": make_consts(),
    }


def kernel(**inputs):
    inp = {k: np.asarray(v) for k, v in inputs.items()}
    nc = build_program(9)
    in_maps = [_layout(inp, c) for c in range(8)]
    res = run_bass_kernel_spmd(nc, in_maps, core_ids=list(range(8)))
    return np.concatenate([r["out"] for r in res.results], axis=0).astype(np.float32)
```

```python
import contextlib
import numpy as np
import concourse.bass as bass
import concourse.mybir as mybir
from concourse.bass_utils import run_bass_kernel_spmd

F32 = mybir.dt.float32
BF16 = mybir.dt.bfloat16
ALU = mybir.AluOpType
AF = mybir.ActivationFunctionType
AX = mybir.AxisListType

NEG = -30000.0
EPS = 1e-6


class Buf:
    __slots__ = ("name", "t", "wev", "revs", "dsem", "dcnt", "pre")

    def __init__(self, name, t=None):
        self.name = name
        self.t = t
        self.wev = []
        self.revs = []
        self.dsem = None
        self.dcnt = 0
        self.pre = []

    def __getitem__(self, k):
        return self.t[k]


class Eng:
    def __init__(self, name, h, sem):
        self.name = name
        self.h = h
        self.sem = sem
        self.cnt = 0
        self.known = {}


class FW:
    def __init__(self, nc, stack):
        self.nc = nc
        self.top = stack
        self.stack = stack
        self.engs = {}
        self.dsems = []
        for name, h in (("pe", nc.tensor), ("act", nc.scalar), ("dve", nc.vector),
                        ("pool", nc.gpsimd), ("sp", nc.sync)):
            sem = stack.enter_context(nc.semaphore("s_" + name))
            self.engs[name] = Eng(name, h, sem)
        self.ninst = 0

    @contextlib.contextmanager
    def scope(self):
        old = self.stack
        with contextlib.ExitStack() as st:
            self.stack = st
            try:
                yield
            finally:
                self.barrier()
                self.stack = old

    def sb(self, name, shape, dt=F32):
        self.nsb = getattr(self, "nsb", 0) + 1
        name = "sb%d_%s" % (self.nsb, name)
        t = self.stack.enter_context(self.nc.sbuf_tensor(name, list(shape), dt))
        return Buf(name, t)

    def view(self, name, t=None):
        return Buf(name, t)

    def _waits(self, e, reads, writes, skip=None):
        need = {}
        for b in reads:
            for (s, v) in b.wev:
                if need.get(s, 0) < v:
                    need[s] = v
        for b in writes:
            for (s, v) in b.wev:
                if need.get(s, 0) < v:
                    need[s] = v
            for (s, v) in b.revs:
                if need.get(s, 0) < v:
                    need[s] = v
        for s, v in need.items():
            if s is skip or (e.name == "pe" and s is e.sem):
                continue
            if e.known.get(s, 0) < v:
                e.h.wait_ge(s, v)
                e.known[s] = v

    def op(self, eng, fn, reads=(), writes=()):
        if getattr(self, "dead", False):
            return None
        e = self.engs[eng]
        self._waits(e, reads, writes)
        ins = fn(e.h)
        e.cnt += 1
        ins.then_inc(e.sem, 1)
        self.ninst += 1
        ev = (e.sem, e.cnt)
        for b in reads:
            if len(b.revs) > 24:
                b.revs = b.revs[-12:] + self._maxev(b.revs[:-12])
            b.revs.append(ev)
        for b in writes:
            b.wev = [ev]
            b.revs = []
        return ins

    @staticmethod
    def _maxev(evs):
        d = {}
        for (s, v) in evs:
            if d.get(s, (None, 0))[1] < v:
                d[s] = (s, v)
        return list(d.values())

    def dma(self, q, out_ap, in_ap, dst, src, **kw):
        if getattr(self, "dead", False):
            return None
        e = self.engs[q]
        reads = [src] if src is not None else []
        if dst.dsem is None:
            dst.dsem = self.top.enter_context(self.nc.semaphore("d%d_%s" % (len(self.dsems), dst.name)))
            self.dsems.append(dst)
        evs = [ev for ev in dst.wev if ev[0] is not dst.dsem] + list(dst.revs)
        if evs:
            dst.pre = evs
        else:
            evs = dst.pre
        for (sm, v) in evs:
            if e.known.get(sm, 0) < v:
                e.h.wait_ge(sm, v)
                e.known[sm] = v
        self._waits(e, reads, [], skip=dst.dsem)
        ins = e.h.dma_start(out=out_ap, in_=in_ap, **kw)
        self.ninst += 1
        dst.dcnt += 16
        ins.then_inc(dst.dsem, 16)
        ev = (dst.dsem, dst.dcnt)
        if src is not None:
            src.revs.append(ev)
        dst.wev = [ev]
        dst.revs = []
        return ins

    def barrier(self):
        for e in self.engs.values():
            for f in self.engs.values():
                if f is e or f.cnt == 0:
                    continue
                if e.known.get(f.sem, 0) < f.cnt:
                    e.h.wait_ge(f.sem, f.cnt)
                    e.known[f.sem] = f.cnt
            for b in self.dsems:
                if b.dcnt and e.known.get(b.dsem, 0) < b.dcnt:
                    e.h.wait_ge(b.dsem, b.dcnt)
                    e.known[b.dsem] = b.dcnt

    def finish(self, bufs, eng="sp"):
        self._waits(self.engs[eng], bufs, [])


def make_consts():
    p = np.arange(128)[:, None]
    f = np.arange(128)[None, :]
    c = np.zeros((128, 15, 128), np.float32)
    c[:, 0] = (p == f)
    c[:, 1] = (p <= f)
    c[:, 2] = (p >= f)
    c[:, 3] = np.where(p > f, 0.0, NEG)
    c[:, 4] = np.where(p < f, 0.0, NEG)
    c[:, 5] = np.where(p <= f, 0.0, NEG)
    c[:, 6] = np.where(p >= f, 0.0, NEG)
    c[:, 7] = (p <= f)
    c[:, 8] = (p >= f)
    c[:, 9] = 1.0
    c[:, 10] = -(p <= f).astype(np.float32) / 16.0
    c[:, 11] = -(p >= f).astype(np.float32) / 16.0
    c[:, 12] = f + 0.0 * p
    c[:, 13] = -(p <= f).astype(np.float32)
    c[:, 14] = -(p >= f).astype(np.float32)
    return c


def dn_scan(fw, nc, PS, PB, pbf, pbb, cst, cstb, Pq, CO, BET, NBET, GG, oacc):
    import itertools
    V = lambda fn, r, w: fw.op("dve", fn, r, w)
    A = lambda fn, r, w: fw.op("act", fn, r, w)
    T = lambda fn, r, w: fw.op("pe", fn, r, w)
    ident_b = cstb[:, 0, :]
    ident_f = cst[:, 0, :]
    ones_f = cst[:, 9, :]
    r4 = lambda ap: ap.rearrange("p (h t) -> p h t", t=128)
    bc = lambda ap: ap.unsqueeze(2).to_broadcast([128, 4, 128])
    V(lambda e: e.memset(oacc[:].rearrange("p a h t -> p (a h t)"), 0.0), [], [oacc])

    def chain(d, b):
        n_ = lambda nm, sh, dt=F32: fw.sb("d%d%s" % (d, nm), sh, dt)
        S = n_("S", [128, 4, 128]); Sbf = n_("Sbf", [128, 4, 128], BF16)
        kv = n_("kv", [128, 8, 128], BF16)
        Gb = n_("Gb", [128, 4, 128])
        sml = n_("sml", [128, 4, 4]); gct = n_("gct", [128, 8])
        E1 = n_("E1", [128, 4, 128]); E2 = E1; E3 = E1
        Y = n_("Y", [128, 4, 128]); Z = n_("Z", [128, 4, 128]); Rf = n_("Rf", [128, 4, 128])
        R = n_("R", [128, 4, 128], BF16); At = n_("At", [128, 4, 128], BF16); qg = n_("qg", [128, 4, 128], BF16)
        kbg = n_("kbg", [128, 4, 128], BF16); kd = n_("kd", [128, 4, 128], BF16); vb = n_("vb", [128, 4, 128], BF16)
        Usb = E1; WT = n_("WT", [128, 4, 128], BF16); vnew = n_("vn", [128, 4, 128], BF16)
        b0, b1, b2, b3 = b
        Cum = cst[:, 1 + d, :]
        nCum = cst[:, 13 + d, :]
        NM1 = cst[:, 3 + d, :].unsqueeze(1).to_broadcast([128, 4, 128])
        NM2 = cst[:, 5 + d, :].unsqueeze(1).to_broadcast([128, 4, 128])
        V(lambda e: e.memset(S[:], 0.0), [], [S])
        V(lambda e: e.memset(Sbf[:], 0.0), [], [Sbf])
        order = [(0, i) for i in ((0, 1) if d == 0 else (1, 0))] + \
                [(1, i) for i in (range(16) if d == 0 else range(15, -1, -1))]
        H = [slice(h * 128, (h + 1) * 128) for h in range(4)]
        for (seg, i) in order:
            gi = i if seg == 0 else 2 + i
            c0 = CO[seg] + i * 128
            g4 = GG[:, gi, d * 4:d * 4 + 4]
            b4 = BET[:, gi, d * 4:d * 4 + 4]
            nb4 = NBET[:, gi, d * 4:d * 4 + 4]
            T(lambda e: e.matmul(out=pbf(b0)[:, 0:4], lhsT=Cum, rhs=g4, start=True, stop=True), [cst, GG], [PB[b0]])
            T(lambda e: e.matmul(out=pbf(b0)[:, 4:8], lhsT=ones_f, rhs=g4, start=True, stop=True), [cst, GG], [PB[b0]])
            V(lambda e: e.tensor_copy(out=Gb[:], in_=bc(g4)), [GG], [Gb])
            A(lambda e: e.copy(out=gct[:], in_=pbf(b0)[:, 0:8]), [PB[b0]], [gct])
            yield
            for h in range(4):
                kT = Pq[:, 4 + h, c0:c0 + 128]
                qT = Pq[:, h, c0:c0 + 128]
                T(lambda e, kT=kT, h=h: e.matmul(out=pbf(b1)[:, H[h]], lhsT=kT, rhs=kT, start=True, stop=True), [Pq], [PB[b1]])
                T(lambda e, kT=kT, qT=qT, h=h: e.matmul(out=pbf(b2)[:, H[h]], lhsT=kT, rhs=qT, start=True, stop=True), [Pq], [PB[b2]])
                T(lambda e, h=h: e.matmul(out=pbf(b3)[:, H[h]], lhsT=Cum, rhs=Gb[:, h, :], start=True, stop=False), [cst, Gb], [PB[b3]])
                T(lambda e, h=h: e.matmul(out=pbf(b3)[:, H[h]], lhsT=Gb[:, h, :], rhs=nCum, start=False, stop=True), [cst, Gb], [PB[b3]])
            A(lambda e: e.activation(out=sml[:, 0, :], in_=gct[:, 0:4], func=AF.Exp), [gct], [sml])
            V(lambda e: e.tensor_tensor(out=sml[:, 1, :], in0=gct[:, 4:8], in1=gct[:, 0:4], op=ALU.subtract), [gct], [sml])
            A(lambda e: e.activation(out=sml[:, 1, :], in_=sml[:, 1, :], func=AF.Exp), [sml], [sml])
            A(lambda e: e.activation(out=sml[:, 2, :], in_=gct[:, 4:8], func=AF.Exp), [gct], [sml])
            V(lambda e: e.tensor_tensor(out=sml[:, 3, :], in0=sml[:, 0, :], in1=b4, op=ALU.mult), [sml, BET], [sml])
            yield
            V(lambda e: e.scalar_tensor_tensor(out=E1[:], in0=r4(pbf(b3)), scalar=0.0, in1=NM1, op0=ALU.min, op1=ALU.add),
              [PB[b3], cst], [E1])
            for h in range(4):
                T(lambda e, h=h: e.matmul(out=pbf(b3)[:, H[h]], lhsT=Gb[:, h, :], rhs=Cum, start=True, stop=False), [cst, Gb], [PB[b3]])
                T(lambda e, h=h: e.matmul(out=pbf(b3)[:, H[h]], lhsT=nCum, rhs=Gb[:, h, :], start=False, stop=True), [cst, Gb], [PB[b3]])
            A(lambda e: e.activation(out=E1[:], in_=E1[:], func=AF.Exp), [E1], [E1])
            V(lambda e: e.tensor_tensor(out=E1[:], in0=r4(pbf(b1)), in1=E1[:], op=ALU.mult), [PB[b1], E1], [E1])
            V(lambda e: e.tensor_tensor(out=Y[:], in0=E1[:], in1=bc(nb4), op=ALU.mult), [E1, NBET], [Y])
            yield
            V(lambda e: e.scalar_tensor_tensor(out=E2[:], in0=r4(pbf(b3)), scalar=0.0, in1=NM2, op0=ALU.min, op1=ALU.add),
              [PB[b3], cst], [E2])
            for h in range(4):
                T(lambda e, h=h: e.matmul(out=pbf(b3)[:, H[h]], lhsT=Gb[:, h, :], rhs=Cum, start=True, stop=True), [cst, Gb], [PB[b3]])
            A(lambda e: e.activation(out=E2[:], in_=E2[:], func=AF.Exp), [E2], [E2])
            V(lambda e: e.tensor_tensor(out=At[:], in0=r4(pbf(b2)), in1=E2[:], op=ALU.mult), [PB[b2], E2], [At])
            for h in range(4):
                T(lambda e, h=h: e.transpose(out=pbb(b0)[:, h * 128:(h + 1) * 128], in_=Pq[:, 4 + h, c0:c0 + 128], identity=ident_b),
                  [Pq, cstb], [PB[b0]])
                T(lambda e, h=h: e.transpose(out=pbb(b0)[:, 512 + h * 128:512 + (h + 1) * 128], in_=Pq[:, 8 + h, c0:c0 + 128],
                                             identity=ident_b), [Pq, cstb], [PB[b0]])
            A(lambda e: e.activation(out=E3[:], in_=r4(pbf(b3)), func=AF.Exp), [PB[b3]], [E3])
            A(lambda e: e.copy(out=kv[:].rearrange("p a t -> p (a t)"), in_=pbb(b0)), [PB[b0]], [kv])
            V(lambda e: e.tensor_tensor(out=qg[:], in0=Pq[:, 0:4, c0:c0 + 128], in1=E3[:], op=ALU.mult), [Pq, E3], [qg])
            yield
            for h in range(4):
                T(lambda e, h=h: e.transpose(out=pbf(b0)[:, H[h]], in_=Y[:, h, :], identity=ident_f), [Y, cst], [PB[b0]])
            A(lambda e: e.copy(out=Z[:], in_=r4(pbf(b0))), [PB[b0]], [Z])
            V(lambda e: e.tensor_tensor(out=Rf[:], in0=Z[:], in1=cst[:, 0, :].unsqueeze(1).to_broadcast([128, 4, 128]), op=ALU.add),
              [Z, cst], [Rf])
            V(lambda e: e.tensor_tensor(out=kbg[:], in0=kv[:, 0:4, :], in1=bc(sml[:, 3, :]), op=ALU.mult), [kv, sml], [kbg])
            V(lambda e: e.tensor_tensor(out=kd[:], in0=kv[:, 0:4, :], in1=bc(sml[:, 1, :]), op=ALU.mult), [kv, sml], [kd])
            V(lambda e: e.tensor_tensor(out=vb[:], in0=kv[:, 4:8, :], in1=bc(b4), op=ALU.mult), [kv, BET], [vb])
            yield
            for lvl in range(1, 7):
                for h in range(4):
                    T(lambda e, h=h: e.matmul(out=pbf(b1)[:, H[h]], lhsT=Z[:, h, :], rhs=Y[:, h, :], start=True, stop=True), [Y, Z], [PB[b1]])
                    if lvl < 6:
                        T(lambda e, h=h: e.matmul(out=pbf(b2)[:, H[h]], lhsT=Y[:, h, :], rhs=Z[:, h, :], start=True, stop=True), [Y, Z], [PB[b2]])
                A(lambda e: e.copy(out=Y[:], in_=r4(pbf(b1))), [PB[b1]], [Y])
                if lvl < 6:
                    V(lambda e: e.tensor_copy(out=Z[:], in_=r4(pbf(b2))), [PB[b2]], [Z])
                for h in range(4):
                    T(lambda e, h=h: e.matmul(out=pbf(b3)[:, H[h]], lhsT=Y[:, h, :], rhs=Rf[:, h, :], start=True, stop=True), [Y, Rf], [PB[b3]])
                V(lambda e: e.tensor_tensor(out=Rf[:], in0=r4(pbf(b3)), in1=Rf[:], op=ALU.add), [PB[b3], Rf], [Rf])
                yield
            A(lambda e: e.copy(out=R[:], in_=Rf[:]), [Rf], [R])
            for h in range(4):
                T(lambda e, h=h: e.matmul(out=pbf(b1)[:, H[h]], lhsT=R[:, h, :], rhs=vb[:, h, :], start=True, stop=True), [R, vb], [PB[b1]])
                T(lambda e, h=h: e.matmul(out=pbf(b2)[:, H[h]], lhsT=kbg[:, h, :], rhs=R[:, h, :], start=True, stop=True), [R, kbg], [PB[b2]])
            A(lambda e: e.copy(out=Usb[:], in_=r4(pbf(b1))), [PB[b1]], [Usb])
            V(lambda e: e.tensor_copy(out=WT[:], in_=r4(pbf(b2))), [PB[b2]], [WT])
            yield
            for h in range(4):
                T(lambda e, h=h: e.matmul(out=pbf(b0)[:, H[h]], lhsT=WT[:, h, :], rhs=Sbf[:, h, :], start=True, stop=True), [WT, Sbf], [PB[b0]])
            V(lambda e: e.tensor_tensor(out=vnew[:], in0=Usb[:], in1=r4(pbf(b0)), op=ALU.subtract), [Usb, PB[b0]], [vnew])
            if seg == 1:
                for h in range(4):
                    T(lambda e, h=h: e.matmul(out=pbf(b3)[:, H[h]], lhsT=qg[:, h, :], rhs=Sbf[:, h, :], start=True, stop=False), [qg, Sbf], [PB[b3]])
                    T(lambda e, h=h: e.matmul(out=pbf(b3)[:, H[h]], lhsT=At[:, h, :], rhs=vnew[:, h, :], start=False, stop=True), [At, vnew], [PB[b3]])
            for h in range(4):
                T(lambda e, h=h: e.matmul(out=pbf(b1)[:, H[h]], lhsT=kd[:, h, :], rhs=vnew[:, h, :], start=True, stop=True), [kd, vnew], [PB[b1]])
            if seg == 1:
                V(lambda e: e.tensor_tensor(out=oacc[:, i], in0=r4(pbf(b3)), in1=oacc[:, i], op=ALU.add), [PB[b3], oacc], [oacc])
            V(lambda e: e.tensor_tensor(out=S[:], in0=S[:], in1=bc(sml[:, 2, :]), op=ALU.mult), [S, sml], [S])
            V(lambda e: e.tensor_tensor(out=S[:], in0=r4(pbf(b1)), in1=S[:], op=ALU.add), [PB[b1], S], [S])
            A(lambda e: e.copy(out=Sbf[:], in_=S[:]), [S], [Sbf])
            yield

    gens = [chain(0, (0, 1, 2, 3)), chain(1, (4, 5, 6, 7))]
    for _ in itertools.zip_longest(*gens):
        pass


def gla_scan(fw, PB, pbf, pbb, cst, cstb, gqk, gv, LRT, wa2_d, ba_d, oacc, extra=None):
    V = lambda fn, r, w: fw.op("dve", fn, r, w)
    A = lambda fn, r, w: fw.op("act", fn, r, w)
    T = lambda fn, r, w: fw.op("pe", fn, r, w)
    ident_b = cstb[:, 0, :]
    r4 = lambda ap: ap.rearrange("p (h t) -> p h t", t=128)
    wa2f = fw.sb("wa2f", [16, 2, 256])
    baf = fw.sb("baf", [1, 2, 256])
    wa2b = fw.sb("wa2b", [16, 2, 256], BF16)
    bab = fw.sb("bab", [1, 2, 256], BF16)
    fw.dma("sp", wa2f[:], wa2_d.rearrange("d r c -> r d c"), wa2f, None)
    fw.dma("sp", baf[:], ba_d.rearrange("d o c -> o d c"), baf, None)
    V(lambda e: e.tensor_copy(out=wa2b[:], in_=wa2f[:]), [wa2f], [wa2b])
    V(lambda e: e.tensor_copy(out=bab[:], in_=baf[:]), [baf], [bab])
    S = fw.sb("gS", [64, 4, 128])
    Sbf = fw.sb("gSbf", [64, 4, 128], BF16)
    sp = fw.sb("gsp", [128, 256])
    bT = fw.sb("gbT", [64, 4, 128])
    eb = fw.sb("geb", [64, 4, 128])
    enb = fw.sb("genb", [64, 4, 128])
    ekd = fw.sb("gekd", [64, 4, 128])
    Qp = fw.sb("gQp", [64, 4, 128], BF16)
    Kp = fw.sb("gKp", [64, 4, 128], BF16)
    KdT = fw.sb("gKdT", [64, 4, 128], BF16)
    Kd = fw.sb("gKd", [128, 4, 64], BF16)
    att = fw.sb("gatt", [128, 4, 128], BF16)
    for d in (0, 1):
        CumS = cst[:, 10 + d, :]
        MK = cst[:, 7 + d, :].unsqueeze(1).to_broadcast([128, 4, 128])
        tl = 127 if d == 0 else 0
        V(lambda e: e.memset(S[:], 0.0), [], [S])
        V(lambda e: e.memset(Sbf[:], 0.0), [], [Sbf])
        order = [(0, i) for i in ((0, 1) if d == 0 else (1, 0))] + \
                [(1, i) for i in (range(16) if d == 0 else range(15, -1, -1))]
        for (seg, i) in order:
            if extra is not None:
                extra()
            gi = i if seg == 0 else 2 + i
            t0 = gi * 128
            T(lambda e: e.matmul(out=pbf(0)[:, 0:256], lhsT=LRT[:, d, t0:t0 + 128], rhs=wa2b[:, d, :], start=True, stop=False),
              [LRT, wa2b], [PB[0]])
            T(lambda e: e.matmul(out=pbf(0)[:, 0:256], lhsT=cstb[0:1, 1, :], rhs=bab[0:1, d, :], start=False, stop=True),
              [cstb, bab], [PB[0]])
            A(lambda e: e.activation(out=sp[:], in_=pbf(0)[:, 0:256], func=AF.Exp, scale=-1.0), [PB[0]], [sp])
            A(lambda e: e.activation(out=sp[:], in_=sp[:], func=AF.Ln, bias=1.0), [sp], [sp])
            for h in range(4):
                T(lambda e, h=h: e.matmul(out=pbf(1)[0:64, h * 128:(h + 1) * 128], lhsT=sp[:, h * 64:(h + 1) * 64], rhs=CumS,
                                          start=True, stop=True), [sp, cst], [PB[1]])
            A(lambda e: e.copy(out=bT[:], in_=r4(pbf(1)[0:64, :])), [PB[1]], [bT])
            A(lambda e: e.activation(out=eb[:], in_=bT[:], func=AF.Exp), [bT], [eb])
            A(lambda e: e.activation(out=enb[:], in_=bT[:], func=AF.Exp, scale=-1.0), [bT], [enb])
            V(lambda e: e.tensor_tensor(out=ekd[:], in0=bT[:], in1=bT[:, :, tl:tl + 1].to_broadcast([64, 4, 128]), op=ALU.subtract),
              [bT], [ekd])
            A(lambda e: e.activation(out=ekd[:], in_=ekd[:], func=AF.Exp, scale=-1.0), [ekd], [ekd])
            V(lambda e: e.tensor_tensor(out=Qp[:], in0=gqk[:, 0:4, t0:t0 + 128], in1=eb[:], op=ALU.mult), [gqk, eb], [Qp])
            V(lambda e: e.tensor_tensor(out=Kp[:], in0=gqk[:, 4:8, t0:t0 + 128], in1=enb[:], op=ALU.mult), [gqk, enb], [Kp])
            V(lambda e: e.tensor_tensor(out=KdT[:], in0=gqk[:, 4:8, t0:t0 + 128], in1=ekd[:], op=ALU.mult), [gqk, ekd], [KdT])
            for h in range(4):
                T(lambda e, h=h: e.transpose(out=pbb(2)[:, h * 64:(h + 1) * 64], in_=KdT[:, h, :], identity=cstb[0:64, 0, 0:64]),
                  [KdT, cstb], [PB[2]])
            A(lambda e: e.copy(out=Kd[:].rearrange("p h k -> p (h k)"), in_=pbb(2)[:, 0:256]), [PB[2]], [Kd])
            if seg == 1:
                for h in range(4):
                    T(lambda e, h=h: e.matmul(out=pbf(3)[:, h * 128:(h + 1) * 128], lhsT=Kp[:, h, :], rhs=Qp[:, h, :], start=True, stop=True),
                      [Kp, Qp], [PB[3]])
                V(lambda e: e.tensor_tensor(out=att[:], in0=r4(pbf(3)), in1=MK, op=ALU.mult), [PB[3], cst], [att])
                for h in range(4):
                    hs = slice(h * 128, (h + 1) * 128)
                    T(lambda e, h=h, hs=hs: e.matmul(out=pbf(4)[:, hs], lhsT=Qp[:, h, :], rhs=Sbf[:, h, :], start=True, stop=False),
                      [Qp, Sbf], [PB[4]])
                    T(lambda e, h=h, hs=hs: e.matmul(out=pbf(4)[:, hs], lhsT=att[:, h, :], rhs=gv[:, gi, hs], start=False, stop=True),
                      [att, gv], [PB[4]])
                if d == 0:
                    A(lambda e: e.copy(out=oacc[:, i], in_=r4(pbf(4))), [PB[4]], [oacc])
                else:
                    V(lambda e: e.tensor_tensor(out=oacc[:, i], in0=r4(pbf(4)), in1=oacc[:, i], op=ALU.add), [PB[4], oacc], [oacc])
            for h in range(4):
                hs = slice(h * 128, (h + 1) * 128)
                T(lambda e, h=h, hs=hs: e.matmul(out=pbf(5)[0:64, hs], lhsT=Kd[:, h, :], rhs=gv[:, gi, hs], start=True, stop=True),
                  [Kd, gv], [PB[5]])
            V(lambda e: e.tensor_tensor(out=S[:], in0=S[:], in1=eb[:, :, tl:tl + 1].to_broadcast([64, 4, 128]), op=ALU.mult), [S, eb], [S])
            V(lambda e: e.tensor_tensor(out=S[:], in0=r4(pbf(5)[0:64, :]), in1=S[:], op=ALU.add), [PB[5], S], [S])
            A(lambda e: e.copy(out=Sbf[:], in_=S[:]), [S], [Sbf])


def head_norm_gate(fw, PB, pbb, cstb, oacc, gate, gnorm, mixT, chunk0, permute):
    V = lambda fn, r, w: fw.op("dve", fn, r, w)
    A = lambda fn, r, w: fw.op("act", fn, r, w)
    T = lambda fn, r, w: fw.op("pe", fn, r, w)
    ident_b = cstb[:, 0, :]
    sq = fw.sb("hsq", [128, 4, 128])
    ss = fw.sb("hss", [128, 4])
    mix = fw.sb("hmix", [128, 4, 128], BF16)
    for i in range(NT_L):
        o = oacc[:, i]
        V(lambda e: e.tensor_tensor(out=sq[:], in0=o, in1=o, op=ALU.mult), [oacc], [sq])
        V(lambda e: e.tensor_reduce(out=ss[:], in_=sq[:], axis=AX.X, op=ALU.add), [sq], [ss])
        A(lambda e: e.activation(out=ss[:], in_=ss[:], func=AF.Sqrt, scale=1.0 / 128, bias=EPS), [ss], [ss])
        V(lambda e: e.reciprocal(out=ss[:], in_=ss[:]), [ss], [ss])
        V(lambda e: e.tensor_tensor(out=sq[:], in0=o, in1=ss[:].unsqueeze(2).to_broadcast([128, 4, 128]), op=ALU.mult), [oacc, ss], [sq])
        V(lambda e: e.tensor_tensor(out=sq[:], in0=sq[:], in1=gnorm[:].unsqueeze(1).to_broadcast([128, 4, 128]), op=ALU.mult), [sq, gnorm], [sq])
        V(lambda e: e.tensor_tensor(out=mix[:], in0=sq[:], in1=gate[:, i, :].rearrange("p (h t) -> p h t", t=128), op=ALU.mult),
          [sq, gate], [mix])
        bk = i % 2
        for h in range(4):
            T(lambda e, h=h: e.transpose(out=pbb(bk)[:, h * 128:(h + 1) * 128], in_=mix[:, h, :], identity=ident_b), [mix, cstb], [PB[bk]])
        if not permute:
            A(lambda e: e.copy(out=mixT[:, chunk0:chunk0 + 4, i * 128:(i + 1) * 128],
                               in_=pbb(bk)[:, 0:512].rearrange("p (h t) -> p h t", t=128)), [PB[bk]], [mixT])
        else:
            for h in range(4):
                dst = mixT[:, chunk0 + h, :].rearrange("p (r c) -> p c r", c=64)[:, 4 * i:4 * i + 4, :]
                src = pbb(bk)[:, h * 128:(h + 1) * 128].rearrange("p (c r) -> p c r", r=32)
                if h % 2:
                    A(lambda e, dst=dst, src=src: e.copy(out=dst, in_=src), [PB[bk]], [mixT])
                else:
                    V(lambda e, dst=dst, src=src: e.tensor_copy(out=dst, in_=src), [PB[bk]], [mixT])


class StopBuild(Exception):
    pass


NSEQ = 2
TL = 2048
TC = 256
NT_L = 16
NT_C = 2
WCOLS = 3632


def build_program(stage=9):
    nc = bass.Bass("TRN2", target_bir_lowering=False)
    din = lambda n, s: nc.dram_tensor(n, list(s), F32, kind="ExternalInput").ap()
    x_d = din("x", [NSEQ, TL, 1024])
    ctx_d = din("ctx", [NSEQ, TC, 1024])
    cT_d = din("cT", [128, 8, 3])
    wada_d = din("w_ada", [1024, 6144])
    badaT_d = din("b_adaT", [128, 48])
    n1g_d = din("n1gT", [128, 8])
    n2g_d = din("n2gT", [128, 8])
    fing_d = din("final_g", [1, 1024])
    win_d = din("w_in", [1024, WCOLS])
    convT_d = din("convT", [128, 12, 5])
    alog_d = din("a_log", [1, 8])
    dtb_d = din("dt_bias", [1, 8])
    dng_d = din("dn_g", [1, 128])
    glg_d = din("gla_g", [1, 128])
    wa2_d = din("wa2", [2, 16, 256])
    ba_d = din("ba", [2, 1, 256])
    wout_d = din("w_out", [1024, 1024])
    wq_d = din("wq", [1024, 2048])
    keysT_d = din("keysT", [128, 16, 128])
    u_d = din("peer_u", [16384, 1024])
    v_d = din("peer_v", [16384, 1024])
    consts_d = din("consts", [128, 15, 128])
    out_d = nc.dram_tensor("out", [NSEQ, TL, 1024], F32, kind="ExternalOutput").ap()
    dbg_d = nc.dram_tensor("dbg", [128, 8, TL], F32, kind="ExternalOutput").ap() if stage < 2 else None
    PDBG = (stage == 3.5)
    X1OUT = (stage == 8)
    if stage == 8:
        stage = 9
    if PDBG:
        dps_d = nc.dram_tensor("dps", [128, 2048], F32, kind="ExternalOutput").ap()
        dptop_d = nc.dram_tensor("dptop", [128, 256], F32, kind="ExternalOutput").ap()
        dpcv_d = nc.dram_tensor("dpcv", [128, 128], F32, kind="ExternalOutput").ap()
        dpt4_d = nc.dram_tensor("dpt4", [128, 512], F32, kind="ExternalOutput").ap()
        dpt4t_d = nc.dram_tensor("dpt4t", [128, 512], F32, kind="ExternalOutput").ap()
        dpw_d = nc.dram_tensor("dpw", [128, 128, 8], F32, kind="ExternalOutput").ap()
        dppo_d = nc.dram_tensor("dppo", [128, 1024], F32, kind="ExternalOutput").ap()
        dpsr_d = nc.dram_tensor("dpsr", [128, 2, 2048], F32, kind="ExternalOutput").ap()
        dpab_d = nc.dram_tensor("dpab", [128, 2, 512], F32, kind="ExternalOutput").ap()
    modrow_d = nc.dram_tensor("modrow", [3, 6144], F32, kind="Internal").ap()
    x1_d = nc.dram_tensor("x1s", [NSEQ * TL, 1024], F32, kind=("ExternalOutput" if X1OUT else "Internal")).ap()
    ss_d = nc.dram_tensor("sscr", [2, 8, NSEQ * TL, 128], F32, kind="Internal").ap()
    ut_d = nc.dram_tensor("utscr", [128, 128, 8, 128], BF16, kind="Internal").ap()
    vb_d = nc.dram_tensor("vbscr", [128, 128, 1024], BF16, kind="Internal").ap()

    with contextlib.ExitStack() as top:
        fw = FW(nc, top)
        V = lambda fn, r, w: fw.op("dve", fn, r, w)
        A = lambda fn, r, w: fw.op("act", fn, r, w)
        G = lambda fn, r, w: fw.op("pool", fn, r, w)
        T = lambda fn, r, w: fw.op("pe", fn, r, w)

        PS = top.enter_context(nc.psum_tensor("ps", [128, 4096], F32))
        PB = [Buf("pb%d" % i, PS[:, i * 512:(i + 1) * 512]) for i in range(8)]

        def pbf(i, n=1):
            return PS[:, i * 512:(i + n) * 512]

        def pbb(i, n=1):
            return PS[:, i * 512:(i + n) * 512].bitcast(BF16)

        outb = fw.view("outb")
        cst = fw.sb("cst", [128, 15, 128])
        fw.dma("sp", cst[:], consts_d, cst, None)
        cstb = fw.sb("cstb", [128, 3, 128], BF16)
        for a_, b_ in ((0, 0), (1, 9), (2, 12)):
            V(lambda e, a_=a_, b_=b_: e.tensor_copy(out=cstb[:, a_, :], in_=cst[:, b_, :]), [cst], [cstb])
        ident_f = cst[:, 0, :]
        ident_b = cstb[:, 0, :]
        ones_b = cstb[:, 1, :]
        ones_f = cst[:, 9, :]

        modT = fw.sb("modT", [128, 48, 3])
        G1 = fw.sb("G1", [128, 8, 3])
        G2 = fw.sb("G2", [128, 8, 3])
        modrow = fw.view("modrow")
        x1buf = fw.view("x1buf")

        with fw.scope():
            cT = fw.sb("cT", [128, 8, 3])
            fw.dma("sp", cT[:], cT_d, cT, None)
            siluT = fw.sb("siluT", [128, 8, 3])
            A(lambda e: e.activation(out=siluT[:], in_=cT[:], func=AF.Silu), [cT], [siluT])
            badaT = fw.sb("badaT", [128, 48])
            fw.dma("sp", badaT[:], badaT_d, badaT, None)
            n1g = fw.sb("n1g", [128, 8])
            n2g = fw.sb("n2g", [128, 8])
            fw.dma("sp", n1g[:], n1g_d, n1g, None)
            fw.dma("sp", n2g[:], n2g_d, n2g, None)
            slabs = [fw.sb("wada%d" % i, [128, 8, 1024]) for i in range(2)]
            wv = wada_d.rearrange("(k p) c -> p k c", p=128)
            pm = pbf(0)[:, 0:144].rearrange("p (j b) -> p j b", b=3)
            for v in range(6):
                sl = slabs[v % 2]
                fw.dma("sp" if v % 2 == 0 else "act", sl[:], wv[:, :, v * 1024:(v + 1) * 1024], sl, None)
                for ch in range(8):
                    j = v * 8 + ch
                    for k in range(8):
                        T(lambda e, sl=sl, ch=ch, k=k, j=j: e.matmul(
                            out=pm[:, j, :], lhsT=sl[:, k, ch * 128:(ch + 1) * 128], rhs=siluT[:, k, :],
                            start=(k == 0), stop=(k == 7)), [sl, siluT], [PB[0]])
            V(lambda e: e.tensor_tensor(out=modT[:], in0=pm[:, 0:48, :],
                                        in1=badaT[:].unsqueeze(2).to_broadcast([128, 48, 3]), op=ALU.add),
              [PB[0], badaT], [modT])
            for (Gt, ng, o) in ((G1, n1g, 8), (G2, n2g, 32)):
                V(lambda e, Gt=Gt, o=o: e.tensor_scalar(out=Gt[:], in0=modT[:, o:o + 8, :], scalar1=1.0, scalar2=None,
                                                        op0=ALU.add), [modT], [Gt])
                V(lambda e, Gt=Gt, ng=ng: e.tensor_tensor(out=Gt[:], in0=Gt[:],
                                                          in1=ng[:].unsqueeze(2).to_broadcast([128, 8, 3]), op=ALU.mult),
                  [Gt, ng], [Gt])
            for b in range(3):
                fw.dma("pool", modrow_d[b].rearrange("(j p) -> p j", p=128), modT[:, :, b], modrow, modT,
                       allow_slow_non_contiguous=True)

        def make_hT(hT, col0, src_ap, ntiles, Gt, SHo, mcol, xt_bufs, wk, perm=False, srcbuf=None, bank=7):
            for i in range(ntiles):
                xt = xt_bufs[i % 2]
                fw.dma("sp", xt[:], src_ap[i * 128:(i + 1) * 128, :], xt, srcbuf)
                junk, ssq, xn = wk
                A(lambda e, xt=xt: e.activation(out=junk[:], in_=xt[:], func=AF.Square, accum_out=ssq[:]),
                  [xt], [junk, ssq])
                A(lambda e: e.activation(out=ssq[:], in_=ssq[:], func=AF.Sqrt, scale=1.0 / 1024, bias=EPS), [ssq], [ssq])
                V(lambda e: e.reciprocal(out=ssq[:], in_=ssq[:]), [ssq], [ssq])
                V(lambda e, xt=xt: e.tensor_scalar(out=xn[:], in0=xt[:], scalar1=ssq[:, 0:1], scalar2=None, op0=ALU.mult),
                  [xt, ssq], [xn])
                pt = pbb(bank).rearrange("p (k t) -> p k t", t=128)
                for k in range(8):
                    T(lambda e, k=k: e.transpose(out=pt[:, k, :], in_=xn[:, k * 128:(k + 1) * 128], identity=ident_b),
                      [xn, cstb], [PB[bank]])
                if not perm:
                    dst = hT[:, :, col0 + i * 128: col0 + (i + 1) * 128]
                    src = pt[:, 0:8, :]
                    g_b = Gt[:, :, mcol:mcol + 1].to_broadcast([128, 8, 128])
                    s_b = modT[:, SHo:SHo + 8, mcol:mcol + 1].to_broadcast([128, 8, 128])
                else:
                    dst = hT[:, :, col0:col0 + TL].rearrange("p k (c r) -> p k r c", r=32)[:, :, 2 * i:2 * i + 2, :]
                    src = pt[:, 0:8, :].rearrange("p k (r c) -> p k r c", c=64)
                    g_b = Gt[:, :, mcol:mcol + 1].unsqueeze(3).to_broadcast([128, 8, 2, 64])
                    s_b = modT[:, SHo:SHo + 8, mcol:mcol + 1].unsqueeze(3).to_broadcast([128, 8, 2, 64])
                V(lambda e, dst=dst, src=src, g_b=g_b: e.tensor_tensor(out=dst, in0=src, in1=g_b, op=ALU.mult),
                  [PB[bank], Gt], [hT])
                V(lambda e, dst=dst, s_b=s_b: e.tensor_tensor(out=dst, in0=dst, in1=s_b, op=ALU.add), [hT, modT], [hT])

        def load_w_bf16(dst, src_view, ncols, stage_bufs, step=512, d0=0):
            n = 0
            for c0 in range(0, ncols, step):
                c1 = min(ncols, c0 + step)
                sg = stage_bufs[n % 2]
                fw.dma("act" if n % 2 else "sp", sg[:, :, 0:c1 - c0], src_view[:, :, c0:c1], sg, None)
                if n % 2:
                    A(lambda e, sg=sg, c0=c0, c1=c1: e.copy(out=dst[:, :, d0 + c0:d0 + c1], in_=sg[:, :, 0:c1 - c0]), [sg], [dst])
                else:
                    V(lambda e, sg=sg, c0=c0, c1=c1: e.tensor_copy(out=dst[:, :, d0 + c0:d0 + c1], in_=sg[:, :, 0:c1 - c0]), [sg], [dst])
                n += 1

        win_v = win_d.rearrange("(k p) c -> p k c", p=128)
        utscr = fw.view("utscr")
        vbscr = fw.view("vbscr")
        uview = u_d.rearrange("(i j) d -> j i d", j=128)
        vview = v_d.rearrange("(i j) d -> j i d", j=128)

        def p0_bufs():
            return dict(ubs=[fw.sb("ub%d" % i, [128, 1024]) for i in range(2)],
                        vbs=[fw.sb("vb%d" % i, [128, 1024]) for i in range(2)],
                        ubb=[fw.sb("ubb%d" % i, [128, 1024], BF16) for i in range(2)],
                        vbb=[fw.sb("vbb%d" % i, [128, 1024], BF16) for i in range(2)],
                        utb=[fw.sb("utb%d" % i, [128, 8, 128], BF16) for i in range(2)])

        def p0_iter(j, B):
            q = j % 2
            ubs, vbs, ubb, vbb, utb = B["ubs"], B["vbs"], B["ubb"], B["vbb"], B["utb"]
            fw.dma("sp", ubs[q][:], uview[j], ubs[q], None)
            fw.dma("sp", vbs[q][:], vview[j], vbs[q], None)
            A(lambda e: e.copy(out=ubb[q][:], in_=ubs[q][:]), [ubs[q]], [ubb[q]])
            V(lambda e: e.tensor_copy(out=vbb[q][:], in_=vbs[q][:]), [vbs[q]], [vbb[q]])
            for k in range(8):
                T(lambda e, k=k: e.transpose(out=pbb(6 + q)[:, k * 128:(k + 1) * 128], in_=ubb[q][:, k * 128:(k + 1) * 128],
                                             identity=ident_b), [ubb[q], cstb], [PB[6 + q]])
            V(lambda e: e.tensor_copy(out=utb[q][:].rearrange("p k i -> p (k i)"), in_=pbb(6 + q)), [PB[6 + q]], [utb[q]])
            fw.dma("pool", ut_d[j], utb[q][:], utscr, utb[q])
            fw.dma("pool", vb_d[j], vbb[q][:], vbscr, vbb[q])

        if 0.6 < stage < 0.7:
            fw.dn_limit = int(round((stage - 0.6) * 1000))

        def ck(x):
            if stage <= x:
                fw.dead = True

        for s in range(NSEQ if stage >= 0.1 else 0):
          try:
            with fw.scope():
                mixT = fw.sb("mixT", [128, 8, TL], BF16)
                with fw.scope():
                    Pq = fw.sb("Pq", [128, 12, TC + TL + 8], BF16)
                    CO = (2, 262)
                    Zg = fw.sb("Zg", [128, NT_L, 512], BF16)
                    SM = fw.sb("SM", [128, 18, 16])
                    with fw.scope():
                        hT = fw.sb("hT", [128, 8, TC + TL], BF16)
                        xts = [fw.sb("xt%d" % i, [128, 1024]) for i in range(2)]
                        wk = (fw.sb("junk", [128, 1024], BF16), fw.sb("ssq", [128, 1]), fw.sb("xn", [128, 1024], BF16))
                        make_hT(hT, 0, ctx_d[s], NT_C, G1, 0, 2, xts, wk)
                        make_hT(hT, TC, x_d[s], NT_L, G1, 0, s, xts, wk)
                        ck(0.2)
                        wdn = fw.sb("wdn", [128, 8, 2064], BF16)
                        stg = [fw.sb("stg%d" % i, [128, 8, 128]) for i in range(2)]
                        load_w_bf16(wdn, win_v[:, :, 0:2048], 2048, stg, 128)
                        load_w_bf16(wdn, win_v[:, :, 3584:3600], 16, stg, 128, 2048)
                        V(lambda e: e.memset(Pq[:], 0.0), [], [Pq])
                        ck(0.3)
                        nb = 0
                        for (t0, tn, seg) in [(0, 256, 0)] + [(TC + b * 512, 512, 1) for b in range(4)]:
                            for c in range(12):
                                bk = nb % 2
                                nb += 1
                                for k in range(8):
                                    T(lambda e, bk=bk, c=c, k=k, t0=t0, tn=tn: e.matmul(
                                        out=pbf(bk)[:, 0:tn], lhsT=wdn[:, k, c * 128:(c + 1) * 128], rhs=hT[:, k, t0:t0 + tn],
                                        start=(k == 0), stop=(k == 7)), [wdn, hT], [PB[bk]])
                                d0 = CO[seg] + (t0 - (TC if seg else 0))
                                if bk:
                                    A(lambda e, bk=bk, c=c, d0=d0, tn=tn: e.copy(out=Pq[:, c, d0:d0 + tn], in_=pbf(bk)[:, 0:tn]),
                                      [PB[bk]], [Pq])
                                else:
                                    V(lambda e, bk=bk, c=c, d0=d0, tn=tn: e.tensor_copy(out=Pq[:, c, d0:d0 + tn], in_=pbf(bk)[:, 0:tn]),
                                      [PB[bk]], [Pq])
                        for i in range(18):
                            t0 = i * 128
                            if i >= 2:
                                for k in range(8):
                                    T(lambda e, k=k, t0=t0: e.matmul(out=pbf(2), lhsT=hT[:, k, t0:t0 + 128], rhs=wdn[:, k, 1536:2048],
                                                                     start=(k == 0), stop=(k == 7)), [wdn, hT], [PB[2]])
                                A(lambda e, i=i: e.activation(out=Zg[:, i - 2, :], in_=pbf(2), func=AF.Silu), [PB[2]], [Zg])
                            for k in range(8):
                                T(lambda e, k=k, t0=t0: e.matmul(out=pbf(3)[:, 0:16], lhsT=hT[:, k, t0:t0 + 128], rhs=wdn[:, k, 2048:2064],
                                                                 start=(k == 0), stop=(k == 7)), [wdn, hT], [PB[3]])
                            V(lambda e, i=i: e.tensor_copy(out=SM[:, i, :], in_=pbf(3)[:, 0:16]), [PB[3]], [SM])
                    ck(0.4)
                    BET = fw.sb("BET", [128, 18, 8])
                    NBET = fw.sb("NBET", [128, 18, 8])
                    GG = fw.sb("GG", [128, 18, 8])
                    alog = fw.sb("alog", [128, 8])
                    dtb = fw.sb("dtb", [128, 8])
                    fw.dma("sp", alog[:], alog_d.partition_broadcast(128), alog, None)
                    fw.dma("sp", dtb[:], dtb_d.partition_broadcast(128), dtb, None)
                    A(lambda e: e.activation(out=BET[:], in_=SM[:, :, 0:8], func=AF.Sigmoid), [SM], [BET])
                    V(lambda e: e.tensor_scalar(out=NBET[:], in0=BET[:], scalar1=-1.0, scalar2=None, op0=ALU.mult), [BET], [NBET])
                    V(lambda e: e.tensor_tensor(out=GG[:], in0=SM[:, :, 8:16], in1=dtb[:].unsqueeze(1).to_broadcast([128, 18, 8]),
                                                op=ALU.add), [SM, dtb], [GG])
                    A(lambda e: e.activation(out=GG[:], in_=GG[:], func=AF.Exp), [GG], [GG])
                    A(lambda e: e.activation(out=GG[:], in_=GG[:], func=AF.Ln, bias=1.0), [GG], [GG])
                    A(lambda e: e.activation(out=alog[:], in_=alog[:], func=AF.Exp), [alog], [alog])
                    V(lambda e: e.tensor_scalar(out=alog[:], in0=alog[:], scalar1=-1.0, scalar2=None, op0=ALU.mult), [alog], [alog])
                    V(lambda e: e.tensor_tensor(out=GG[:], in0=GG[:], in1=alog[:].unsqueeze(1).to_broadcast([128, 18, 8]),
                                                op=ALU.mult), [GG, alog], [GG])
                    ck(0.5)
                    cw = fw.sb("cw", [128, 12, 5])
                    fw.dma("sp", cw[:], convT_d, cw, None)
                    with fw.scope():
                        acc = fw.sb("acc", [128, TL])
                        sq = fw.sb("sq", [128, 512], BF16)
                        rin = fw.sb("rin", [128, 512])
                        for c in range(12):
                            for seg, n in ((0, TC), (1, TL)):
                                o = CO[seg]
                                V(lambda e, c=c, o=o, n=n: e.tensor_scalar(out=acc[:, 0:n], in0=Pq[:, c, o - 2:o - 2 + n],
                                                                           scalar1=cw[:, c, 0:1], scalar2=None, op0=ALU.mult),
                                  [Pq, cw], [acc])
                                for j in range(1, 5):
                                    V(lambda e, c=c, o=o, n=n, j=j: e.scalar_tensor_tensor(
                                        out=acc[:, 0:n], in0=Pq[:, c, o - 2 + j:o - 2 + j + n], scalar=cw[:, c, j:j + 1],
                                        in1=acc[:, 0:n], op0=ALU.mult, op1=ALU.add), [Pq, cw, acc], [acc])
                                if c >= 8:
                                    A(lambda e, c=c, o=o, n=n: e.activation(out=Pq[:, c, o:o + n], in_=acc[:, 0:n], func=AF.Silu),
                                      [acc], [Pq])
                                    continue
                                A(lambda e, n=n: e.activation(out=acc[:, 0:n], in_=acc[:, 0:n], func=AF.Silu), [acc], [acc])
                                for b0 in range(0, n, 512):
                                    bn = min(512, n - b0)
                                    V(lambda e, b0=b0, bn=bn: e.tensor_tensor(out=sq[:, 0:bn], in0=acc[:, b0:b0 + bn],
                                                                              in1=acc[:, b0:b0 + bn], op=ALU.mult), [acc], [sq])
                                    T(lambda e, bn=bn: e.matmul(out=pbf(0)[:, 0:bn], lhsT=ones_b, rhs=sq[:, 0:bn], start=True, stop=True),
                                      [sq, cstb], [PB[0]])
                                    A(lambda e, bn=bn: e.activation(out=rin[:, 0:bn], in_=pbf(0)[:, 0:bn], func=AF.Sqrt, bias=EPS),
                                      [PB[0]], [rin])
                                    V(lambda e, bn=bn: e.reciprocal(out=rin[:, 0:bn], in_=rin[:, 0:bn]), [rin], [rin])
                                    sc = (128.0 ** -0.5) if c < 4 else 1.0
                                    V(lambda e, c=c, o=o, b0=b0, bn=bn, sc=sc: e.scalar_tensor_tensor(
                                        out=Pq[:, c, o + b0:o + b0 + bn], in0=acc[:, b0:b0 + bn], scalar=sc, in1=rin[:, 0:bn],
                                        op0=ALU.mult, op1=ALU.mult), [acc, rin], [Pq])
                    ck(0.6)
                    oacc = fw.sb("oacc", [128, NT_L, 4, 128])
                    with fw.scope():
                        dn_scan(fw, nc, PS, PB, pbf, pbb, cst, cstb, Pq, CO, BET, NBET, GG, oacc)
                    ck(0.7)
                    dng = fw.sb("dng", [128, 128])
                    fw.dma("sp", dng[:], dng_d.partition_broadcast(128), dng, None)
                    with fw.scope():
                        head_norm_gate(fw, PB, pbb, cstb, oacc, Zg, dng, mixT, 0, False)

                with fw.scope():
                    gqk = fw.sb("gqk", [64, 8, TC + TL], BF16)
                    gv = fw.sb("gv", [128, 18, 512], BF16)
                    Rg = fw.sb("Rg", [128, NT_L, 512], BF16)
                    LRT = fw.sb("LRT", [16, 2, TC + TL], BF16)
                    with fw.scope():
                        hT = fw.sb("hTg", [128, 8, TC + TL], BF16)
                        xts = [fw.sb("xtg%d" % i, [128, 1024]) for i in range(2)]
                        wk = (fw.sb("junkg", [128, 1024], BF16), fw.sb("ssqg", [128, 1]), fw.sb("xng", [128, 1024], BF16))
                        make_hT(hT, 0, ctx_d[s], NT_C, G1, 0, 2, xts, wk)
                        make_hT(hT, TC, x_d[s], NT_L, G1, 0, s, xts, wk, perm=True)
                        wgl = fw.sb("wgl", [128, 8, 1568], BF16)
                        stg = [fw.sb("stgg%d" % i, [128, 8, 128]) for i in range(2)]
                        load_w_bf16(wgl, win_v[:, :, 2048:3584], 1536, stg, 128)
                        load_w_bf16(wgl, win_v[:, :, 3600:3632], 32, stg, 128, 1536)
                        nb = 0
                        for (t0, tn) in [(0, 256)] + [(TC + b * 512, 512) for b in range(4)]:
                            for g in range(10):
                                bk = nb % 2
                                nb += 1
                                if g < 8:
                                    cs, m = slice(g * 64, (g + 1) * 64), 64
                                else:
                                    cs, m = slice(1536 + (g - 8) * 16, 1536 + (g - 7) * 16), 16
                                for k in range(8):
                                    T(lambda e, bk=bk, k=k, cs=cs, m=m, t0=t0, tn=tn: e.matmul(
                                        out=pbf(bk)[0:m, 0:tn], lhsT=wgl[:, k, cs], rhs=hT[:, k, t0:t0 + tn],
                                        start=(k == 0), stop=(k == 7)), [wgl, hT], [PB[bk]])
                                if g < 4:
                                    A(lambda e, bk=bk, g=g, t0=t0, tn=tn: e.mul(out=gqk[:, g, t0:t0 + tn], in_=pbf(bk)[0:64, 0:tn], mul=0.125),
                                      [PB[bk]], [gqk])
                                elif g < 8:
                                    V(lambda e, bk=bk, g=g, t0=t0, tn=tn: e.tensor_copy(out=gqk[:, g, t0:t0 + tn], in_=pbf(bk)[0:64, 0:tn]),
                                      [PB[bk]], [gqk])
                                else:
                                    V(lambda e, bk=bk, g=g, t0=t0, tn=tn: e.tensor_copy(out=LRT[:, g - 8, t0:t0 + tn], in_=pbf(bk)[0:16, 0:tn]),
                                      [PB[bk]], [LRT])
                        for i in range(18):
                            t0 = i * 128
                            for k in range(8):
                                T(lambda e, k=k, t0=t0: e.matmul(out=pbf(2), lhsT=hT[:, k, t0:t0 + 128], rhs=wgl[:, k, 512:1024],
                                                                 start=(k == 0), stop=(k == 7)), [wgl, hT], [PB[2]])
                            V(lambda e, i=i: e.tensor_copy(out=gv[:, i, :], in_=pbf(2)), [PB[2]], [gv])
                            if i >= 2:
                                for k in range(8):
                                    T(lambda e, k=k, t0=t0: e.matmul(out=pbf(3), lhsT=hT[:, k, t0:t0 + 128], rhs=wgl[:, k, 1024:1536],
                                                                     start=(k == 0), stop=(k == 7)), [wgl, hT], [PB[3]])
                                A(lambda e, i=i: e.activation(out=Rg[:, i - 2, :], in_=pbf(3), func=AF.Silu), [PB[3]], [Rg])
                    ck(1.2)
                    oacc = fw.sb("oaccg", [128, NT_L, 4, 128])
                    with fw.scope():
                        if stage >= 3:
                            p0B = p0_bufs()
                            p0it = iter(range(s * 64, (s + 1) * 64))

                            def extra():
                                for _ in range(2):
                                    j_ = next(p0it, None)
                                    if j_ is not None:
                                        p0_iter(j_, p0B)
                        else:
                            extra = None
                        gla_scan(fw, PB, pbf, pbb, cst, cstb, gqk, gv, LRT, wa2_d, ba_d, oacc, extra)
                    ck(1.3)
                    glg = fw.sb("glg", [128, 128])
                    fw.dma("sp", glg[:], glg_d.partition_broadcast(128), glg, None)
                    with fw.scope():
                        head_norm_gate(fw, PB, pbb, cstb, oacc, Rg, glg, mixT, 4, True)
                ck(1.4)
                with fw.scope():
                    wo = fw.sb("wo", [128, 8, 1024], BF16)
                    stg = [fw.sb("stgo%d" % i, [128, 8, 128]) for i in range(2)]
                    load_w_bf16(wo, wout_d.rearrange("(k p) c -> p k c", p=128), 1024, stg, 128)
                    g1B = fw.sb("g1B", [128, 1024])
                    fw.dma("sp", g1B[:], modrow_d[s:s + 1, 2048:3072].partition_broadcast(128), g1B, modrow)
                    xts = [fw.sb("xto%d" % i, [128, 1024]) for i in range(2)]
                    x1ts = [fw.sb("x1t%d" % i, [128, 1024]) for i in range(2)]
                    for i in range(NT_L):
                        xt = xts[i % 2]
                        x1t = x1ts[i % 2]
                        fw.dma("sp", xt[:], x_d[s][i * 128:(i + 1) * 128, :], xt, None)
                        for hf in range(2):
                            bk = 2 * (i % 2) + hf
                            for k in range(8):
                                T(lambda e, k=k, i=i, hf=hf, bk=bk: e.matmul(out=pbf(bk), lhsT=mixT[:, k, i * 128:(i + 1) * 128],
                                                                             rhs=wo[:, k, hf * 512:(hf + 1) * 512],
                                                                             start=(k == 0), stop=(k == 7)), [mixT, wo], [PB[bk]])
                            hs = slice(hf * 512, (hf + 1) * 512)
                            V(lambda e, bk=bk, hs=hs, x1t=x1t: e.tensor_tensor(out=x1t[:, hs], in0=pbf(bk), in1=g1B[:, hs], op=ALU.mult),
                              [PB[bk], g1B], [x1t])
                            V(lambda e, hs=hs, x1t=x1t, xt=xt: e.tensor_tensor(out=x1t[:, hs], in0=x1t[:, hs], in1=xt[:, hs], op=ALU.add),
                              [x1t, xt], [x1t])
                        fw.dma("pool", x1_d[s * TL + i * 128:s * TL + (i + 1) * 128, :], x1t[:], x1buf, x1t)
                ck(1.45)
                if stage < 2:
                    if s == 0:
                        with fw.scope():
                            dts = [fw.sb("dtmp%d" % i, [128, TL]) for i in range(2)]
                            for ch in range(8):
                                dt_ = dts[ch % 2]
                                V(lambda e, ch=ch, dt_=dt_: e.tensor_copy(out=dt_[:], in_=mixT[:, ch, :]), [mixT], [dt_])
                                fw.dma("sp", dbg_d[:, ch, :], dt_[:], outb, dt_)
                        fw.dead = True
                    continue
          except StopBuild:
            break
        fw.dead = False
        if stage >= 3:
            def pk(x):
                if stage == x:
                    fw.dead = True
            pk(3.1)
            with fw.scope():
                wqb = fw.sb("wqb", [128, 8, 2048], BF16)
                keysb = fw.sb("keysb", [128, 16, 128], BF16)
                with fw.scope():
                    stg = [fw.sb("stgp%d" % i, [128, 8, 128]) for i in range(2)]
                    load_w_bf16(wqb, wq_d.rearrange("(k p) c -> p k c", p=128), 2048, stg, 128)
                    keysf = fw.sb("keysf", [128, 16, 128])
                    fw.dma("sp", keysf[:], keysT_d, keysf, None)
                    V(lambda e: e.tensor_copy(out=keysb[:], in_=keysf[:]), [keysf], [keysb])
                g2B = fw.sb("g2B", [128, 1024])
                fingB = fw.sb("fingB", [128, 1024])
                fw.dma("sp", fingB[:], fing_d.partition_broadcast(128), fingB, None)
                h2T = fw.sb("h2T", [128, 8, 256], BF16)
                xts = [fw.sb("xtp%d" % i, [128, 1024]) for i in range(2)]
                xnp = fw.sb("xnp", [128, 1024], BF16)
                wk = (xnp, fw.sb("ssqp", [128, 1]), xnp)
                qT = fw.sb("qT", [128, 16, 256], BF16)
                ssb = fw.sb("ssb", [128, 16, 128])
                top = fw.sb("top", [128, 16, 16])
                idxu = fw.sb("idxu", [128, 8, 16], mybir.dt.uint32)
                cand = fw.sb("cand", [128, 4, 256])
                wk2 = fw.sb("wk2", [128, 256])
                wkk = wk2
                cv = fw.sb("cv", [128, 8, 16])
                T4s = fw.sb("T4s", [128, 3, 128])
                T4 = fw.sb("T4", [128, 2, 3, 128])
                rZ = fw.sb("rZ", [128, 8])
                NSB = 8
                NWB = 3
                Sreps = [fw.sb("Srep%d" % i, [128, NSB, 128]) for i in range(NWB)]
                Abs_ = [fw.sb("Ab%d" % i, [128, NSB, 128], BF16) for i in range(2)]
                Bbs_ = [fw.sb("Bb%d" % i, [128, NSB, 128], BF16) for i in range(2)]
                Ebs_ = [fw.sb("Eb%d" % i, [128, NSB, 128], BF16) for i in range(2)]
                Wsb = fw.sb("Wsb", [128, 128, 256], BF16)
                utl = [fw.sb("utl%d" % i, [128, 2, 8, 128], BF16) for i in range(2)]
                vtl = [fw.sb("vtl%d" % i, [128, 2, 1024], BF16) for i in range(2)]
                gef = [fw.sb("gef%d" % i, [128, 256], BF16) for i in range(2)]
                gw = [fw.sb("gw%d" % i, [128, 256], BF16) for i in range(2)]
                x2ap = cand[:].rearrange("p h c -> p (h c)")
                x2 = cand
                ssbuf = [fw.view("ssbuf%d" % i) for i in range(2)]
                tv = top[:].rearrange("p (h q) k -> p h q k", q=2)
                zzap = wk2[:, 0:128].rearrange("p (h k) -> p h k", k=16)
                bk3 = lambda ap: ap.unsqueeze(2).to_broadcast([128, NSB, 128])
                iota_b = cstb[:, 2, :].unsqueeze(1).to_broadcast([128, NSB, 128])
                t4 = lambda q: T4s[:, q, :].rearrange("p (h k) -> p h k", k=16)
                b16 = lambda ap: ap.to_broadcast([128, 8, 16])
                def phase1(blk, h2T, T4):
                    sq = (blk * 2) // NT_L
                    sq = (blk * 2) // NT_L
                    make_hT(h2T, 0, x1_d[blk * 256:(blk + 1) * 256, :], 2, G2, 24, sq, xts, wk, srcbuf=x1buf, bank=3)
                    yield
                    for hp in range(16):
                        bk = 2 + hp % 2
                        for k in range(8):
                            T(lambda e, bk=bk, hp=hp, k=k: e.matmul(out=pbf(bk)[:, 0:256], lhsT=wqb[:, k, hp * 128:(hp + 1) * 128],
                                                                    rhs=h2T[:, k, :], start=(k == 0), stop=(k == 7)), [wqb, h2T], [PB[bk]])
                        if bk % 2:
                            A(lambda e, bk=bk, hp=hp: e.copy(out=qT[:, hp, :], in_=pbf(bk)[:, 0:256]), [PB[bk]], [qT])
                        else:
                            V(lambda e, bk=bk, hp=hp: e.tensor_copy(out=qT[:, hp, :], in_=pbf(bk)[:, 0:256]), [PB[bk]], [qT])
                        yield
                    sc = ssbuf[blk % 2]
                    for tl in range(2):
                        gt = blk * 2 + tl
                        for g in range(4):
                            bk = 2 + g % 2
                            for q in range(4):
                                hp = g * 4 + q
                                T(lambda e, bk=bk, hp=hp, q=q, tl=tl: e.matmul(out=pbf(bk)[:, q * 128:(q + 1) * 128],
                                                                               lhsT=qT[:, hp, tl * 128:(tl + 1) * 128],
                                                                               rhs=keysb[:, hp, :], start=True, stop=True), [qT, keysb], [PB[bk]])
                            A(lambda e, bk=bk, g=g: e.copy(out=ssb[:, g * 4:(g + 1) * 4, :].rearrange("p a i -> p (a i)"), in_=pbf(bk)),
                              [PB[bk]], [ssb])
                            yield
                        fw.dma("pool", ss_d[1, :, gt * 128:(gt + 1) * 128, :].rearrange("h t i -> t h i"),
                               ssb[:].rearrange("p (h q) i -> p h q i", q=2)[:, :, 1, :], sc, ssb)
                        for hp in range(16):
                            V(lambda e, hp=hp: e.max(out=top[:, hp, 0:8], in_=ssb[:, hp, :]), [ssb], [top])
                            V(lambda e, hp=hp: e.match_replace(out=wkk[:, 0:128], in_to_replace=top[:, hp, 0:8], in_values=ssb[:, hp, :],
                                                               imm_value=-1e30), [ssb, top], [wkk])
                            V(lambda e, hp=hp: e.max(out=top[:, hp, 8:16], in_=wkk[:, 0:128]), [wkk], [top])
                            yield
                            if hp % 2 == 0:
                                for o8 in (0, 8):
                                    V(lambda e, hp=hp, o8=o8: e.max_index(out=idxu[:, hp // 2, o8:o8 + 8], in_max=top[:, hp, o8:o8 + 8],
                                                                          in_values=ssb[:, hp, :]), [ssb, top], [idxu])
                        for hh in range(2):
                            V(lambda e, hh=hh: e.tensor_tensor(out=cand[:].rearrange("p h (a b) -> p h a b", b=16),
                                                               in0=tv[:, hh * 4:hh * 4 + 4, 0, :].unsqueeze(3).to_broadcast([128, 4, 16, 16]),
                                                               in1=tv[:, hh * 4:hh * 4 + 4, 1, :].unsqueeze(2).to_broadcast([128, 4, 16, 16]),
                                                               op=ALU.add), [top], [cand])
                            for h in range(hh * 4, hh * 4 + 4):
                                V(lambda e, h=h: e.max(out=cv[:, h, 0:8], in_=cand[:, h % 4, :]), [cand], [cv])
                                V(lambda e, h=h: e.match_replace(out=wk2[:], in_to_replace=cv[:, h, 0:8], in_values=cand[:, h % 4, :],
                                                                 imm_value=-1e30), [cand, cv], [wk2])
                                V(lambda e, h=h: e.max(out=cv[:, h, 8:16], in_=wk2[:]), [wk2], [cv])
                                yield
                        V(lambda e: e.tensor_copy(out=t4(0), in_=idxu[:]), [idxu], [T4s])
                        V(lambda e: e.tensor_scalar(out=t4(1), in0=tv[:, :, 0, :], scalar1=-1.0, scalar2=-1e-5, op0=ALU.mult, op1=ALU.add),
                          [top], [T4s])
                        V(lambda e: e.tensor_tensor(out=t4(1), in0=t4(1), in1=b16(cv[:, :, 15:16]), op=ALU.add), [T4s, cv], [T4s])
                        V(lambda e: e.tensor_tensor(out=zzap, in0=cv[:], in1=b16(cv[:, :, 0:1]), op=ALU.subtract), [cv], [wk2])
                        A(lambda e: e.activation(out=zzap, in_=zzap, func=AF.Exp), [wk2], [wk2])
                        V(lambda e: e.tensor_reduce(out=rZ[:], in_=zzap, axis=AX.X, op=ALU.add), [wk2], [rZ])
                        V(lambda e: e.reciprocal(out=rZ[:], in_=rZ[:]), [rZ], [rZ])
                        yield
                        V(lambda e: e.tensor_tensor(out=t4(2), in0=tv[:, :, 0, :], in1=b16(cv[:, :, 0:1]), op=ALU.subtract), [top, cv], [T4s])
                        A(lambda e: e.activation(out=t4(2), in_=t4(2), func=AF.Exp), [T4s], [T4s])
                        V(lambda e: e.tensor_tensor(out=t4(2), in0=t4(2), in1=b16(rZ[:].unsqueeze(2)), op=ALU.mult), [T4s, rZ], [T4s])
                        for q in range(3):
                            T(lambda e, q=q: e.transpose(out=pbf(2)[:, q * 128:(q + 1) * 128], in_=T4s[:, q, :], identity=ident_f),
                              [T4s, cst], [PB[2]])
                        A(lambda e, tl=tl: e.copy(out=T4[:, tl].rearrange("p q t -> p (q t)"), in_=pbf(2)[:, 0:384]), [PB[2]], [T4])
                        yield

                h2Ts = [h2T, fw.sb("h2Tb", [128, 8, 256], BF16)]
                T4x = [T4, fw.sb("T4b", [128, 2, 3, 128])]
                NB = NSEQ * NT_L // 2
                NXT_PER_JJ = 1
                for _ in phase1(0, h2Ts[0], T4x[0]):
                    pass
                for blk in range(NB):
                    sq = (blk * 2) // NT_L
                    h2T, T4 = h2Ts[blk % 2], T4x[blk % 2]
                    sc = ssbuf[blk % 2]
                    nxt = phase1(blk + 1, h2Ts[(blk + 1) % 2], T4x[(blk + 1) % 2]) if blk + 1 < NB else iter(())
                    pk(3.2)
                    pending = []
                    for sbk in range(256 // NSB):
                        tl = (sbk * NSB) // 128
                        tin = (sbk * NSB) % 128
                        ta = blk * 256 + sbk * NSB
                        tsl = slice(tin, tin + NSB)
                        Srep, Ab, Bb, Eb = Sreps[sbk % NWB], Abs_[sbk % 2], Bbs_[sbk % 2], Ebs_[sbk % 2]
                        fw.dma("sp", Srep[:], ss_d[1, :, ta:ta + NSB, :].unsqueeze(1).to_broadcast([8, 16, NSB, 128]), Srep, sc)
                        for t in range(NSB):
                            V(lambda e, t=t, tin=tin, tl=tl, Ab=Ab: e.tensor_scalar(
                                out=Ab[:, t, :], in0=cstb[:, 2, :], scalar1=T4[:, tl, 0, tin + t:tin + t + 1],
                                scalar2=T4[:, tl, 2, tin + t:tin + t + 1], op0=ALU.is_equal, op1=ALU.mult), [cstb, T4], [Ab])
                        V(lambda e, tsl=tsl, tl=tl, Srep=Srep, Bb=Bb: e.tensor_tensor(out=Bb[:], in0=Srep[:], in1=bk3(T4[:, tl, 1, tsl]), op=ALU.is_ge),
                          [Srep, T4], [Bb])
                        A(lambda e, Srep=Srep, Eb=Eb: e.activation(out=Eb[:], in_=Srep[:], func=AF.Exp), [Srep], [Eb])
                        G(lambda e, Eb=Eb, Bb=Bb: e.tensor_tensor(out=Bb[:], in0=Bb[:], in1=Eb[:], op=ALU.mult), [Bb, Eb], [Bb])
                        for fn_ in pending:
                            fn_()
                        pending = []
                        for t in range(NSB):
                            bk = 2 + 2 * (sbk % 2) + (t // 4) % 2
                            T(lambda e, bk=bk, t=t, Ab=Ab, Bb=Bb: e.matmul(out=pbf(bk)[:, (t % 4) * 128:(t % 4 + 1) * 128], lhsT=Ab[:, t, :],
                                                                           rhs=Bb[:, t, :], start=True, stop=True), [Ab, Bb], [PB[bk]])
                            if t % 4 == 3:
                                t0 = sbk * NSB + t - 3
                                dst = Wsb[:, :, t0:t0 + 4].rearrange("p j t -> p t j")
                                src = pbf(bk).rearrange("p (t j) -> p t j", j=128)
                                if (t // 4) % 2:
                                    pending.append(lambda dst=dst, src=src, bk=bk: A(lambda e: e.copy(out=dst, in_=src), [PB[bk]], [Wsb]))
                                else:
                                    pending.append(lambda dst=dst, src=src, bk=bk: V(lambda e: e.tensor_copy(out=dst, in_=src), [PB[bk]], [Wsb]))
                    for fn_ in pending:
                        fn_()
                    pending = []
                    pk(3.3)
                    pend2 = []
                    for jj in range(64):
                        for _ in range(NXT_PER_JJ):
                            next(nxt, None)
                        ut2, vt2 = utl[jj % 2], vtl[jj % 2]
                        fw.dma("sp", ut2[:], ut_d[2 * jj:2 * jj + 2].rearrange("j p k i -> p j k i"), ut2, utscr)
                        fw.dma("sp", vt2[:], vb_d[2 * jj:2 * jj + 2].rearrange("j p d -> p j d"), vt2, vbscr)
                        for jl in range(2):
                            j = 2 * jj + jl
                            bk = j % 2
                            for k in range(8):
                                T(lambda e, bk=bk, k=k, ut2=ut2, jl=jl: e.matmul(out=pbf(bk)[:, 0:256], lhsT=ut2[:, jl, k, :], rhs=h2T[:, k, :],
                                                                                 start=(k == 0), stop=(k == 7)), [ut2, h2T], [PB[bk]])
                            ge_, gw_ = gef[j % 2], gw[j % 2]
                            A(lambda e, bk=bk, ge_=ge_: e.activation(out=ge_[:], in_=pbf(bk)[:, 0:256], func=AF.Gelu), [PB[bk]], [ge_])
                            V(lambda e, ge_=ge_, gw_=gw_, j=j: e.tensor_tensor(out=gw_[:], in0=ge_[:], in1=Wsb[:, j, :], op=ALU.mult),
                              [ge_, Wsb], [gw_])
                            for fn_ in pend2:
                                fn_()
                            pend2 = []
                            for tl in range(2):
                                for hf in range(2):
                                    ob = 4 + tl * 2 + hf
                                    pend2.append(lambda hf=hf, tl=tl, ob=ob, gw_=gw_, vt2=vt2, jl=jl, j=j: T(lambda e: e.matmul(
                                        out=pbf(ob), lhsT=gw_[:, tl * 128:(tl + 1) * 128], rhs=vt2[:, jl, hf * 512:(hf + 1) * 512],
                                        start=(j == 0), stop=(j == 127)), [gw_, vt2], [PB[ob]]))
                    for fn_ in pend2:
                        fn_()
                    pend2 = []
                    pk(3.4)
                    for _ in nxt:
                        pass
                    if blk % (NT_L // 2) == 0:
                        fw.dma("sp", g2B[:], modrow_d[sq:sq + 1, 5120:6144].partition_broadcast(128), g2B, modrow)
                    for tl in range(2):
                        xt = xts[tl]
                        fw.dma("sp", xt[:], x1_d[blk * 256 + tl * 128:blk * 256 + (tl + 1) * 128, :], xt, x1buf)
                        for hf in range(2):
                            hs = slice(hf * 512, (hf + 1) * 512)
                            ob = 4 + tl * 2 + hf
                            V(lambda e, ob=ob, hs=hs: e.tensor_tensor(out=x2ap[:, hs], in0=pbf(ob), in1=g2B[:, hs], op=ALU.mult),
                              [PB[ob], g2B], [x2])
                        V(lambda e, xt=xt: e.tensor_tensor(out=x2ap, in0=x2ap, in1=xt[:], op=ALU.add), [x2, xt], [x2])
                        junk, ssq, xn = wk
                        A(lambda e: e.activation(out=junk[:], in_=x2ap, func=AF.Square, accum_out=ssq[:]), [x2], [junk, ssq])
                        A(lambda e: e.activation(out=ssq[:], in_=ssq[:], func=AF.Sqrt, scale=1.0 / 1024, bias=EPS), [ssq], [ssq])
                        V(lambda e: e.reciprocal(out=ssq[:], in_=ssq[:]), [ssq], [ssq])
                        V(lambda e: e.scalar_tensor_tensor(out=x2ap, in0=x2ap, scalar=ssq[:, 0:1], in1=fingB[:], op0=ALU.mult, op1=ALU.mult),
                          [x2, ssq, fingB], [x2])
                        ti = (blk * 2 + tl) % NT_L
                        fw.dma("pool", out_d[sq][ti * 128:(ti + 1) * 128, :], x2ap, outb, x2)
                    pk(3.6)
        fw.dead = False
        fw.finish([outb], "sp")
        fw.barrier()
    return nc


def _layout(inp, core):
    b0 = 2 * core
    f = lambda a: np.ascontiguousarray(a, dtype=np.float32)
    csel = np.stack([inp["c"][b0], inp["c"][b0 + 1], inp["c_ctx"]])
    w_in = inp["w_in"][0]
    w_in_r = np.concatenate([w_in[:, 0:2048], w_in[:, 2064:3600], w_in[:, 2048:2064], w_in[:, 3600:3632]], axis=1)
    return {
        "x": f(inp["x"][b0:b0 + 2]), "ctx": f(inp["ctx"][b0:b0 + 2]),
        "cT": f(csel.reshape(3, 8, 128).transpose(2, 1, 0)),
        "w_ada": f(inp["w_ada"][0]), "b_adaT": f(inp["b_ada"][0].reshape(48, 128).T),
        "n1gT": f(inp["norm1_g"][0].reshape(8, 128).T), "n2gT": f(inp["norm2_g"][0].reshape(8, 128).T),
        "final_g": f(inp["final_g"].reshape(1, 1024)), "w_in": f(w_in_r),
        "convT": f(inp["conv_w"][0].T.reshape(12, 128, 5).transpose(1, 0, 2)),
        "a_log": f(inp["dn_a_log"][0].reshape(1, 8)), "dt_bias": f(inp["dn_dt_bias"][0].reshape(1, 8)),
        "dn_g": f(inp["dn_norm_g"][0].reshape(1, 128)), "gla_g": f(inp["gla_norm_g"][0].reshape(1, 128)),
        "wa2": f(inp["gla_wa2"][0]), "ba": f(inp["gla_ba"][0].reshape(2, 1, 256)),
        "w_out": f(inp["w_out"][0]), "wq": f(inp["peer_wq"][0]),
        "keysT": f(inp["peer_keys"][0].reshape(16, 128, 128).transpose(2, 0, 1)),
        "peer_u": f(inp["peer_u"][0]), "peer_v": f(inp["peer_v"][0]),
        "consts": make_consts(),
    }


def kernel(**inputs):
    inp = {k: np.asarray(v) for k, v in inputs.items()}
    nc = build_program(9)
    in_maps = [_layout(inp, c) for c in range(8)]
    res = run_bass_kernel_spmd(nc, in_maps, core_ids=list(range(8)))
    return np.concatenate([r["out"] for r in res.results], axis=0).astype(np.float32)
```
